# Optimizing a Trainium2 kernel written in Bass

```python
import math
import jax, jax.numpy as jnp
from jax import lax
import numpy as np


D_MODEL = 1024
BATCH = 32
SEQ = 2048
DEPTH = 4

D_MIX = D_MODEL
D_S5 = D_MIX // 2
S5_GROUP = 16
S5_GROUPS = D_S5 // S5_GROUP
S5_STATE = 64
DT_MIN = 0.001
DT_MAX = 0.1
D_GLA = D_MIX - D_S5
GLA_HEADS = 4
GLA_DV = D_GLA // GLA_HEADS
GLA_DK = GLA_DV // 2
GLA_KDIM = GLA_HEADS * GLA_DK
GLA_GATE_RANK = 16
GLA_GATE_NORM = 16.0
GLA_CHUNK = 64
D_IN = D_S5 + 2 * GLA_KDIM + 2 * D_GLA + GLA_GATE_RANK
D_FF = 256 * (-(-(8 * D_MODEL) // (3 * 256)))
EPS = 1e-6

kernel_name = "hymba_s5_gla_hybrid_trunk"


def _rmsnorm(x, gain):
    xf = x.astype(jnp.float32)
    y = xf * lax.rsqrt(jnp.mean(xf * xf, axis=-1, keepdims=True) + EPS)
    return (y * gain.astype(jnp.float32)).astype(x.dtype)


def _complex_linear_combine(e1, e2):
    ar1, ai1, br1, bi1 = e1
    ar2, ai2, br2, bi2 = e2
    return (ar1 * ar2 - ai1 * ai2,
            ar1 * ai2 + ai1 * ar2,
            ar2 * br1 - ai2 * bi1 + br2,
            ar2 * bi1 + ai2 * br1 + bi2)


def _s5_mixer(u, lam_re, lam_im, b_re, b_im, c_re, c_im, d_skip, log_step):
    f32 = jnp.float32
    bsz, seq, _ = u.shape
    uf = u.astype(f32).reshape(bsz, seq, S5_GROUPS, S5_GROUP)
    step = jnp.exp(log_step.astype(f32))[:, None]
    lr = lam_re.astype(f32)
    li = lam_im.astype(f32)
    mag = jnp.exp(lr * step)
    ar = mag * jnp.cos(li * step)
    ai = mag * jnp.sin(li * step)
    den = lr * lr + li * li
    fr = ((ar - 1.0) * lr + ai * li) / den
    fi = (ai * lr - (ar - 1.0) * li) / den
    br = b_re.astype(f32)
    bi = b_im.astype(f32)
    bbar_re = fr[..., None] * br - fi[..., None] * bi
    bbar_im = fr[..., None] * bi + fi[..., None] * br
    xr = jnp.einsum('gph,blgh->lbgp', bbar_re, uf)
    xi = jnp.einsum('gph,blgh->lbgp', bbar_im, uf)
    a_re = jnp.broadcast_to(ar, (seq, 1, S5_GROUPS, S5_STATE))
    a_im = jnp.broadcast_to(ai, (seq, 1, S5_GROUPS, S5_STATE))
    _, _, sr, si = lax.associative_scan(_complex_linear_combine, (a_re, a_im, xr, xi), axis=0)
    y = (jnp.einsum('ghp,lbgp->blgh', c_re.astype(f32), sr)
         - jnp.einsum('ghp,lbgp->blgh', c_im.astype(f32), si))
    y = y + d_skip.astype(f32).reshape(S5_GROUPS, S5_GROUP) * uf
    return y.reshape(bsz, seq, D_S5)


def _gla_mixer(q, k, v, g, gate_lr, w_gate, b_gate, out_norm):
    f32 = jnp.float32
    bsz, seq, _ = q.shape
    n_chunks = seq // GLA_CHUNK
    log_a = jax.nn.log_sigmoid(gate_lr.astype(f32) @ w_gate.astype(f32) + b_gate.astype(f32)) / GLA_GATE_NORM

    def chunked(t, dh):
        return t.astype(f32).reshape(bsz, n_chunks, GLA_CHUNK, GLA_HEADS, dh).transpose(0, 3, 1, 2, 4)

    qc = chunked(q, GLA_DK) * (GLA_DK ** -0.5)
    kc = chunked(k, GLA_DK)
    vc = chunked(v, GLA_DV)
    cum = jnp.cumsum(chunked(log_a, GLA_DK), axis=3)
    last = cum[:, :, :, -1:, :]
    q_dec = qc * jnp.exp(cum)
    k_inv = kc * jnp.exp(-cum)
    k_end = kc * jnp.exp(last - cum)
    causal = jnp.tril(jnp.ones((GLA_CHUNK, GLA_CHUNK), dtype=bool))
    scores = jnp.where(causal, jnp.einsum('bhntk,bhnsk->bhnts', q_dec, k_inv), 0.0)
    o_intra = jnp.einsum('bhnts,bhnsv->bhntv', scores, vc)
    upd = jnp.einsum('bhnsk,bhnsv->bhnkv', k_end, vc)
    decay = jnp.exp(last[:, :, :, 0, :])

    def step(state, inp):
        dec, u = inp
        return dec[..., None] * state + u, state

    s0 = jnp.zeros((bsz, GLA_HEADS, GLA_DK, GLA_DV), f32)
    _, s_prev = lax.scan(step, s0, (jnp.moveaxis(decay, 2, 0), jnp.moveaxis(upd, 2, 0)))
    s_prev = jnp.moveaxis(s_prev, 0, 2)
    o = o_intra + jnp.einsum('bhntk,bhnkv->bhntv', q_dec, s_prev)
    o = o.transpose(0, 2, 3, 1, 4).reshape(bsz, seq, GLA_HEADS, GLA_DV)
    o = o * lax.rsqrt(jnp.mean(o * o, axis=-1, keepdims=True) + EPS)
    o = o.reshape(bsz, seq, D_GLA) * out_norm.astype(f32)
    return jax.nn.silu(g.astype(f32)) * o


def _normal(k, shape, scale):
    return scale * jax.random.normal(k, shape, jnp.float32)


def setup_inputs(seed: int = 0) -> dict:
    key = jax.random.key(seed)
    ks = jax.random.split(key, 22)
    n_idx = jnp.arange(S5_STATE, dtype=jnp.float32)
    lam_im = jnp.broadcast_to(jnp.pi * n_idx, (DEPTH, S5_GROUPS, S5_STATE))
    return {
        'x': _normal(ks[0], (BATCH, SEQ, D_MODEL), 1.0),
        'norm_mix': 1.0 + _normal(ks[1], (DEPTH, D_MODEL), 0.02),
        'w_in': _normal(ks[2], (DEPTH, D_MODEL, D_IN), D_MODEL ** -0.5),
        's5_lam_re': -0.5 + _normal(ks[3], (DEPTH, S5_GROUPS, S5_STATE), 0.01),
        's5_lam_im': lam_im + _normal(ks[4], (DEPTH, S5_GROUPS, S5_STATE), 0.01),
        's5_b_re': _normal(ks[5], (DEPTH, S5_GROUPS, S5_STATE, S5_GROUP), (2 * S5_GROUP) ** -0.5),
        's5_b_im': _normal(ks[6], (DEPTH, S5_GROUPS, S5_STATE, S5_GROUP), (2 * S5_GROUP) ** -0.5),
        's5_c_re': _normal(ks[7], (DEPTH, S5_GROUPS, S5_GROUP, S5_STATE), S5_STATE ** -0.5),
        's5_c_im': _normal(ks[8], (DEPTH, S5_GROUPS, S5_GROUP, S5_STATE), S5_STATE ** -0.5),
        's5_d': _normal(ks[9], (DEPTH, D_S5), 1.0),
        's5_log_step': jax.random.uniform(ks[10], (DEPTH, S5_GROUPS), jnp.float32,
                                          minval=math.log(DT_MIN), maxval=math.log(DT_MAX)),
        's5_w_glu': _normal(ks[11], (DEPTH, D_S5, D_S5), D_S5 ** -0.5),
        's5_b_glu': _normal(ks[12], (DEPTH, D_S5), 0.02),
        's5_out_norm': 1.0 + _normal(ks[13], (DEPTH, D_S5), 0.02),
        'gla_w_gate': _normal(ks[14], (DEPTH, GLA_GATE_RANK, GLA_KDIM), GLA_GATE_RANK ** -0.5),
        'gla_b_gate': _normal(ks[15], (DEPTH, GLA_KDIM), 0.1),
        'gla_out_norm': 1.0 + _normal(ks[16], (DEPTH, D_GLA), 0.02),
        'w_out': _normal(ks[17], (DEPTH, D_MIX, D_MODEL), D_MIX ** -0.5),
        'norm_ffn': 1.0 + _normal(ks[18], (DEPTH, D_MODEL), 0.02),
        'w_ffn_in': _normal(ks[19], (DEPTH, D_MODEL, 2 * D_FF), D_MODEL ** -0.5),
        'w_ffn_out': _normal(ks[20], (DEPTH, D_FF, D_MODEL), D_FF ** -0.5),
        'norm_final': 1.0 + _normal(ks[21], (D_MODEL,), 0.02),
    }


def reference(x, norm_mix, w_in, s5_lam_re, s5_lam_im, s5_b_re, s5_b_im, s5_c_re, s5_c_im,
              s5_d, s5_log_step, s5_w_glu, s5_b_glu, s5_out_norm, gla_w_gate, gla_b_gate,
              gla_out_norm, w_out, norm_ffn, w_ffn_in, w_ffn_out, norm_final):
    o_q = D_S5
    o_k = o_q + GLA_KDIM
    o_v = o_k + GLA_KDIM
    o_g = o_v + D_GLA
    o_r = o_g + D_GLA
    for i in range(DEPTH):
        h = _rmsnorm(x, norm_mix[i])
        z = h @ w_in[i]
        u_s5 = z[..., :o_q]
        q = z[..., o_q:o_k]
        k = z[..., o_k:o_v]
        v = z[..., o_v:o_g]
        g = z[..., o_g:o_r]
        gate_lr = z[..., o_r:]
        y = _s5_mixer(u_s5, s5_lam_re[i], s5_lam_im[i], s5_b_re[i], s5_b_im[i],
                      s5_c_re[i], s5_c_im[i], s5_d[i], s5_log_step[i])
        y = jax.nn.gelu(y)
        y = y * jax.nn.sigmoid(y @ s5_w_glu[i].astype(jnp.float32) + s5_b_glu[i].astype(jnp.float32))
        y_s5 = _rmsnorm(y, s5_out_norm[i]).astype(x.dtype)
        y_gla = _gla_mixer(q, k, v, g, gate_lr, gla_w_gate[i], gla_b_gate[i], gla_out_norm[i]).astype(x.dtype)
        x = x + jnp.concatenate([y_s5, y_gla], axis=-1) @ w_out[i]
        h = _rmsnorm(x, norm_ffn[i])
        gu = h @ w_ffn_in[i]
        x = x + (jax.nn.silu(gu[..., :D_FF]) * gu[..., D_FF:]) @ w_ffn_out[i]
    return _rmsnorm(x, norm_final)
```

```python
import contextlib, itertools, math
import numpy as np
import concourse.bass as bass
import concourse.mybir as mybir
from concourse.bass_utils import run_bass_kernel_spmd

F32 = mybir.dt.float32; BF16 = mybir.dt.bfloat16; I32 = mybir.dt.int32
AF = mybir.ActivationFunctionType; ALU = mybir.AluOpType
P = 128; L = 2048; D = 1024; DC = 8; TT = 1024; DIN = 2064; DFF = 2816; FC = 22
NL = 4; G = 32; EPS = 1e-6
TWO_PI = 2.0 * math.pi
C_NMIX = 0; C_NFFN = 32; C_NFIN = 64; C_BGLU = 72; C_S5N = 88; C_GLAN = 104; NCOL = 120
K_ID = 0; K_CM = 128; K_TR = 256; K_TM = 384; K_SW = 512; K_ST = 640; K_NV = 640 + 1920; K_SG = K_NV + 32; NCONST = K_SG + 2
NS5 = 96 + 4 * 512
NVALS = [7, 6, 5, 4, 3, 2, 1, 0, -1, -2, -3, -4, -5, -6, -7, -8, 1, 2, 3, 4, 5, 6, 7, 8, 8, 16, 32, 64, 128, 256, 512, 1024]


class Tok:
    __slots__ = ('name', 'w', 'rd', 'sem', 'semv')

    def __init__(self, name):
        self.name = name; self.w = None; self.rd = {}; self.sem = None; self.semv = 0


class KB:
    def __init__(self, nc, es):
        self.nc = nc; self.es = es
        self.h = {'pe': nc.tensor, 'act': nc.scalar, 'dve': nc.vector, 'pool': nc.gpsimd, 'sp': nc.sync}
        self.sem = {}; self.cnt = {}; self.seen = {}
        self.pesems = set(); self.epoch = 0
        for e in self.h:
            self.seen[e] = {}
        self._new_sems()
        self.nwait = 0; self.nins = 0
        self.dsems = []; self.dlast = {}

    def _new_sems(self):
        for e in self.h:
            self.sem[e] = self.es.enter_context(self.nc.semaphore('s%d_%s' % (self.epoch, e))); self.cnt[e] = 0
        self.pesems.add(self.sem['pe'])
        self.epoch += 1

    def new_epoch(self):
        self.full_barrier()
        self._new_sems()

    def _deps(self, reads, writes):
        need = {}
        for b in reads:
            d = b.w
            if d is not None and need.get(d[0], 0) < d[1]: need[d[0]] = d[1]
        for b in writes:
            d = b.w
            if d is not None and need.get(d[0], 0) < d[1]: need[d[0]] = d[1]
            for k, v in b.rd.items():
                if need.get(k, 0) < v: need[k] = v
        return need

    def _wait(self, eng, need):
        seen = self.seen[eng]
        for k, v in need.items():
            if eng == 'pe' and k in self.pesems: continue
            if seen.get(k, 0) >= v: continue
            self.h[eng].wait_ge(k, v); seen[k] = v; self.nwait += 1

    def op(self, eng, fn, reads=(), writes=()):
        self._wait(eng, self._deps(reads, writes))
        ins = fn(self.h[eng])
        self.cnt[eng] += 1; c = self.cnt[eng]; sm = self.sem[eng]
        ins.then_inc(sm, 1); self.nins += 1
        for b in reads: b.rd[sm] = c
        for b in writes:
            b.w = (sm, c); b.rd = {}
        return ins

    def dma(self, eng, out, in_, reads=(), writes=(), owner=None):
        self._wait(eng, self._deps(reads, writes))
        own = owner or (writes[0] if writes else reads[0])
        if own.sem is None:
            own.sem = self.es.enter_context(self.nc.semaphore('d%d' % len(self.dsems))); own.semv = 0
            self.dsems.append(own.sem)
        ins = self.h[eng].dma_start(out=out, in_=in_)
        own.semv += 16
        ins.then_inc(own.sem, 16); self.nins += 1
        self.dlast[own.sem] = own.semv
        for b in reads: b.rd[own.sem] = own.semv
        for b in writes:
            b.w = (own.sem, own.semv); b.rd = {}
        return ins

    def barrier(self, toks, eng='sp'):
        need = {}
        for b in toks:
            for k, v in ([b.w] if b.w else []) + list(b.rd.items()):
                if need.get(k, 0) < v: need[k] = v
        self._wait(eng, need)

    def full_barrier(self, dmas=True):
        for e in self.h:
            need = {self.sem[k]: self.cnt[k] for k in self.h if self.cnt[k] > 0 and k != e}
            if dmas:
                need.update(self.dlast)
            self._wait(e, need)


class MK:
    def __init__(self, nseq=4, layers=(0, 1, 2, 3), final=True, prologue=True, dbg=None, stop_after=None):
        self.nseq = nseq; self.layers = list(layers); self.final = final; self.dbg = dbg or {}
        self.do_prologue = prologue; self.stop_after = stop_after
        nc = self.nc = bass.Bass("TRN2", target_bir_lowering=False)
        dt = nc.dram_tensor
        self.xT = dt("xT", [nseq, P, DC, L], F32, kind="ExternalInput").ap()
        self.w_in = dt("w_in", [NL, P, DC, DIN], F32, kind="ExternalInput").ap()
        self.w_glu = dt("w_glu", [NL, P, 4, 512], F32, kind="ExternalInput").ap()
        self.w_out = dt("w_out", [NL, P, DC, D], F32, kind="ExternalInput").ap()
        self.w_f1 = dt("w_f1", [NL, P, DC, 2 * DFF], F32, kind="ExternalInput").ap()
        self.w_f2 = dt("w_f2", [NL, P, FC, D], F32, kind="ExternalInput").ap()
        self.cols_d = dt("cols", [P, NCOL], F32, kind="ExternalInput").ap()
        self.wgx_d = dt("wgx", [33, NL, 256], F32, kind="ExternalInput").ap()
        self.s5p_d = dt("s5p", [P, NL, NS5], F32, kind="ExternalInput").ap()
        self.dcol_d = dt("dcol", [P, NL, G], F32, kind="ExternalInput").ap()
        self.consts_d = dt("consts", [P, NCONST], F32, kind="ExternalInput").ap()
        self.yT = dt("yT", [nseq, P, DC, L], F32, kind="ExternalOutput").ap()
        if prologue:
            self.s5m = dt("s5m", [NL, 11, P, 4096], BF16, kind="Internal").ap()
        else:
            self.s5m = dt("s5m", [NL, 11, P, 4096], BF16, kind="ExternalInput").ap()
        self.dbg_out = {}
        for k, shp in self.dbg.items():
            self.dbg_out[k] = dt("dbg_" + k, list(shp), F32, kind="ExternalOutput").ap()
        self.build()

    def sb(self, es, name, shape, dtype):
        self.uid = getattr(self, 'uid', 0) + 1
        return es.enter_context(self.nc.sbuf_tensor("%s_%d" % (name, self.uid), list(shape), dtype))

    def ptok(self, name):
        d = self.__dict__.setdefault('_ptoks', {})
        if name not in d: d[name] = Tok(name)
        return d[name]

    def wing_slot(self, es):
        return self.cur_slot

    def load_wing(self, l):
        TW = self.ptok("WinG")
        for c in range(DC):
            self.kb.dma('pool', self.cur_slot[:, c, :], self.w_in[l, :, c, 512:2064], writes=[TW])
        self.wing_valid = l

    def psum_next(self):
        i = self.ps_i; self.ps_i = (i + 1) % 8
        return self.ps[i], self.Tps[i]

    def evac_eng(self):
        return next(self.ev)

    def build(self):
        nc = self.nc
        with contextlib.ExitStack() as es:
            kb = self.kb = KB(nc, es)
            self.ev = itertools.cycle(['act', 'dve'])
            self.ps = [es.enter_context(nc.psum_tensor("ps%d" % i, [P, 512], F32)) for i in range(8)]
            self.Tps = [Tok("ps%d" % i) for i in range(8)]
            self.ps_i = 0
            self.ident_bf = self.sb(es, "ident_bf", [P, P], BF16)
            self.ones_bf = self.sb(es, "ones_bf", [P, P], BF16)
            self.strips = self.sb(es, "strips", [P, 8, 240], BF16)
            self.cmask = self.sb(es, "cmask", [P, P], F32)
            self.triR = self.sb(es, "triR", [P, P], F32)
            self.cols = self.sb(es, "cols", [P, NCOL], F32)
            self.wgx = self.sb(es, "wgx", [33, NL, 256], BF16)
            self.epsc = self.sb(es, "epsc", [P, 2], F32)
            self.Tconst = Tok("const")
            T = self.Tconst
            with contextlib.ExitStack() as es2:
                cst = self.sb(es2, "cst", [P, NCONST], F32)
                Tc = Tok("cst")
                kb.dma('sp', cst[:], self.consts_d, writes=[Tc])
                kb.dma('sp', self.cols[:], self.cols_d, writes=[T])
                self.Twgx = Tok('wgx'); kb.dma('pool', self.wgx[:], self.wgx_d, writes=[self.Twgx])
                kb.op('dve', lambda e: e.tensor_copy(self.ident_bf[:], cst[:, K_ID:K_ID + 128]), reads=[Tc], writes=[T])
                kb.op('dve', lambda e: e.memset(self.ones_bf[:], 1.0), writes=[T])
                kb.op('dve', lambda e: e.memset(self.epsc[:, 0:1], EPS), writes=[T])
                kb.op('dve', lambda e: e.memset(self.epsc[:, 1:2], 1.0), writes=[T])
                kb.op('dve', lambda e: e.tensor_copy(self.strips[:].rearrange("p a b -> p (a b)"), cst[:, K_ST:K_ST + 1920]), reads=[Tc], writes=[T])
                kb.op('dve', lambda e: e.tensor_copy(self.cmask[:], cst[:, K_CM:K_CM + 128]), reads=[Tc], writes=[T])
                kb.op('dve', lambda e: e.tensor_copy(self.triR[:], cst[:, K_TR:K_TR + 128]), reads=[Tc], writes=[T])
                if self.do_prologue:
                    self.Ts5m = [[self.ptok("s5m%d" % l)] * 11 for l in range(NL)]
                    for l in self.layers:
                        self.prologue(l, cst, Tc)
                else:
                    self.Ts5m = [[self.ptok("s5m%d" % l)] * 11 for l in range(NL)]
                kb.full_barrier()
                allt = [self.Ts5m[l][0] for l in range(NL)]
                for e in ('sp', 'act', 'pool', 'pe', 'dve'):
                    kb.barrier(allt + [Tc, T, self.Twgx], eng=e)
            if self.stop_after != 'prologue':
                self.main(es)

    def prologue(self, l, cst, Tc):
        nc = self.nc; kb = self.kb
        with contextlib.ExitStack() as es:
            sb = lambda n, s, d=F32: self.sb(es, "pl_" + n, s, d)
            sp = sb("sp", [P, NS5]); dc = sb("dc", [P, G])
            Tsp = self.ptok("pl_sp")
            kb.dma('sp', sp[:], self.s5p_d[:, l, :], writes=[Tsp])
            kb.dma('sp', dc[:], self.dcol_d[:, l, :], writes=[Tsp])
            lr2 = sp[:, 0:32]; li2 = sp[:, 32:64]; ls2 = sp[:, 64:96]
            R1 = sp[:, 96:608]; R2 = sp[:, 608:1120]; C1 = sp[:, 1120:1632]; C2 = sp[:, 1632:2144]
            nv = cst[:, K_NV:K_NV + 32]
            sgA = cst[:, K_SG:K_SG + 1]; sgB = cst[:, K_SG + 1:K_SG + 2]
            sm = sb("sm", [P, 16, 32])
            Tsm = Tok("sm")
            tb = {k: sb("tb_" + k, [P, 32, 32]) for k in ("mag", "tr", "tf", "rc", "Cn", "Sn", "SnA", "SnB")}
            ti = sb("ti", [P, 32, 32], I32)
            Ttb = Tok("tb")
            V = lambda e: e
            def dve(fn, r, w): kb.op('dve', fn, reads=r, writes=w)
            def act(fn, r, w): kb.op('act', fn, reads=r, writes=w)
            step = sm[:, 0, :]; lrs = sm[:, 1, :]; lis = sm[:, 2, :]
            act(lambda e: e.activation(out=step, in_=ls2, func=AF.Exp), [Tsp], [Tsm])
            dve(lambda e: e.tensor_tensor(out=lrs, in0=lr2, in1=step, op=ALU.mult), [Tsp, Tsm], [Tsm])
            dve(lambda e: e.tensor_tensor(out=lis, in0=li2, in1=step, op=ALU.mult), [Tsp, Tsm], [Tsm])
            def bc_g(a):
                return a.unsqueeze(1).to_broadcast([P, 32, 32])
            def bc_n(a):
                return a.unsqueeze(2).to_broadcast([P, 32, 32])
            flat = lambda t: t[:].rearrange("p a b -> p (a b)")
            dve(lambda e: e.tensor_tensor(out=tb["mag"][:], in0=bc_g(lrs), in1=bc_n(nv), op=ALU.mult), [Tsm, Tc], [Ttb])
            act(lambda e: e.activation(out=flat(tb["mag"]), in_=flat(tb["mag"]), func=AF.Exp), [Ttb], [Ttb])
            dve(lambda e: e.scalar_tensor_tensor(out=tb["tr"][:], in0=bc_g(lis), scalar=1.0 / TWO_PI, in1=bc_n(nv), op0=ALU.mult, op1=ALU.mult), [Tsm, Tc], [Ttb])
            for name, ph in (("Cn", 0.25), ("Sn", 0.0)):
                dve(lambda e: e.tensor_scalar(out=flat(ti), in0=flat(tb["tr"]), scalar1=ph, scalar2=None, op0=ALU.add), [Ttb], [Ttb])
                dve(lambda e: e.tensor_copy(flat(tb["tf"]), flat(ti)), [Ttb], [Ttb])
                dve(lambda e: e.scalar_tensor_tensor(out=flat(tb["rc"]), in0=flat(tb["tr"]), scalar=ph, in1=flat(tb["tf"]), op0=ALU.add, op1=ALU.subtract), [Ttb], [Ttb])
                act(lambda e: e.activation(out=flat(tb[name]), in_=flat(tb["rc"]), func=AF.Sin, scale=6.283184), [Ttb], [Ttb])
                dve(lambda e: e.tensor_tensor(out=flat(tb[name]), in0=flat(tb[name]), in1=flat(tb["mag"]), op=ALU.mult), [Ttb], [Ttb])
            dve(lambda e: e.tensor_scalar(out=flat(tb["SnA"]), in0=flat(tb["Sn"]), scalar1=sgA, scalar2=None, op0=ALU.mult), [Ttb, Tc], [Ttb])
            dve(lambda e: e.tensor_scalar(out=flat(tb["SnB"]), in0=flat(tb["Sn"]), scalar1=sgB, scalar2=None, op0=ALU.mult), [Ttb, Tc], [Ttb])
            ar = tb["Cn"][:, 16, :]; ai = tb["Sn"][:, 16, :]
            s_ = lambda i: sm[:, i, :]
            tt = lambda o, a, b, op: dve(lambda e: e.tensor_tensor(out=o, in0=a, in1=b, op=op), [Tsm, Ttb, Tsp], [Tsm])
            dve(lambda e: e.tensor_scalar(out=s_(3), in0=ar, scalar1=-1.0, scalar2=None, op0=ALU.add), [Ttb], [Tsm])
            tt(s_(4), s_(3), lr2, ALU.mult); tt(s_(5), ai, li2, ALU.mult); tt(s_(4), s_(4), s_(5), ALU.add)
            tt(s_(5), ai, lr2, ALU.mult); tt(s_(6), s_(3), li2, ALU.mult); tt(s_(5), s_(5), s_(6), ALU.subtract)
            tt(s_(6), lr2, lr2, ALU.mult); tt(s_(7), li2, li2, ALU.mult); tt(s_(6), s_(6), s_(7), ALU.add)
            dve(lambda e: e.reciprocal(s_(6), s_(6)), [Tsm], [Tsm])
            tt(s_(8), s_(4), s_(6), ALU.mult)
            tt(s_(9), s_(5), s_(6), ALU.mult)
            dve(lambda e: e.tensor_scalar(out=s_(10), in0=s_(9), scalar1=sgB, scalar2=None, op0=ALU.mult), [Tsm, Tc], [Tsm])
            dve(lambda e: e.tensor_scalar(out=s_(11), in0=s_(9), scalar1=sgA, scalar2=None, op0=ALU.mult), [Tsm, Tc], [Tsm])
            X1 = sb("X1", [P, 32, 16]); X2 = sb("X2", [P, 32, 16]); Xt = sb("Xt", [P, 32, 16])
            TX = Tok("X")
            bh = lambda a: a.unsqueeze(2).to_broadcast([P, 32, 16])
            r3 = lambda a: a.rearrange("p (g h) -> p g h", h=16)
            dve(lambda e: e.tensor_tensor(out=X1[:], in0=r3(R1), in1=bh(s_(8)), op=ALU.mult), [Tsp, Tsm], [TX])
            dve(lambda e: e.tensor_tensor(out=Xt[:], in0=r3(R2), in1=bh(s_(10)), op=ALU.mult), [Tsp, Tsm], [TX])
            dve(lambda e: e.tensor_tensor(out=X1[:], in0=X1[:], in1=Xt[:], op=ALU.add), [TX], [TX])
            dve(lambda e: e.tensor_tensor(out=X2[:], in0=r3(R2), in1=bh(s_(8)), op=ALU.mult), [Tsp, Tsm], [TX])
            dve(lambda e: e.tensor_tensor(out=Xt[:], in0=r3(R1), in1=bh(s_(11)), op=ALU.mult), [Tsp, Tsm], [TX])
            dve(lambda e: e.tensor_tensor(out=X2[:], in0=X2[:], in1=Xt[:], op=ALU.add), [TX], [TX])
            T3 = sb("T3", [P, 8, 32]); T4 = sb("T4", [P, 8, 32]); TT34 = Tok("T34")
            dve(lambda e: e.tensor_copy(T3[0:64], tb["Cn"][0:64, 16:24, :]), [Ttb], [TT34])
            dve(lambda e: e.tensor_scalar(out=T3[64:128], in0=tb["Sn"][64:128, 16:24, :], scalar1=-1.0, scalar2=None, op0=ALU.mult), [Ttb], [TT34])
            dve(lambda e: e.tensor_scalar(out=T4[0:64], in0=tb["Sn"][0:64, 16:24, :], scalar1=-1.0, scalar2=None, op0=ALU.mult), [Ttb], [TT34])
            dve(lambda e: e.tensor_scalar(out=T4[64:128], in0=tb["Cn"][64:128, 16:24, :], scalar1=-1.0, scalar2=None, op0=ALU.mult), [Ttb], [TT34])
            big = {k: sb("big_" + k, [P, 32, 8, 16]) for k in ("WBe", "WBn", "WC", "m1", "m2")}
            Tbig = {k: Tok("big" + k) for k in big}
            def tab(t, i0):
                return t[:, i0:i0 + 8, :].rearrange("p s g -> p g s").unsqueeze(3).to_broadcast([P, 32, 8, 16])
            def xb(x):
                return x[:].unsqueeze(2).to_broadcast([P, 32, 8, 16])
            def combo(dst, ta, xa, tb_, xb_, i0, extra_r):
                dve(lambda e: e.tensor_tensor(out=big["m1"][:], in0=tab(ta, i0), in1=xb(xa), op=ALU.mult), extra_r, [Tbig["m1"]])
                dve(lambda e: e.tensor_tensor(out=big["m2"][:], in0=tab(tb_, i0), in1=xb(xb_), op=ALU.mult), extra_r, [Tbig["m2"]])
                dve(lambda e: e.tensor_tensor(out=big[dst][:], in0=big["m1"][:], in1=big["m2"][:], op=ALU.add), [Tbig["m1"], Tbig["m2"]], [Tbig[dst]])
            combo("WBe", tb["Cn"], X1, tb["SnB"], X2, 0, [Ttb, TX])
            combo("WBn", tb["Cn"], X1, tb["SnB"], X2, 8, [Ttb, TX])
            C1v = sb("C1v", [P, 32, 16]); C2v = sb("C2v", [P, 32, 16]); TC = Tok("C")
            dve(lambda e: e.tensor_copy(C1v[:], r3(C1)), [Tsp], [TC])
            dve(lambda e: e.tensor_copy(C2v[:], r3(C2)), [Tsp], [TC])
            combo("WC", T3, C1v, T4, C2v, 0, [TT34, TC])
            idf = cst[:, K_ID:K_ID + 128]; tmask = cst[:, K_TM:K_TM + 128]; swapm = cst[:, K_SW:K_SW + 128]
            outb = [sb("outb%d" % i, [P, 32, 128], BF16) for i in range(2)]
            Toutb = [Tok("outb%d" % i) for i in range(2)]
            tmpT = sb("tmpT", [P, 4, 128]); TtmpT = Tok("tmpT")
            g3 = lambda t, g: t[:, g, :, :].rearrange("p s h -> p (s h)")
            for b in range(8):
                ps, Tp = self.psum_next()
                for gi in range(4):
                    g = 4 * b + gi
                    kb.op('pe', lambda e: e.transpose(ps[:, gi * 128:(gi + 1) * 128], g3(big["WBe"], g), idf), reads=[Tbig["WBe"], Tc], writes=[Tp])
                kb.op('act', lambda e: e.activation(out=outb[0][:, 4 * b:4 * b + 4, :].rearrange("p a b -> p (a b)"), in_=ps[:], func=AF.Copy), reads=[Tp], writes=[Toutb[0]])
            kb.dma('sp', self.s5m[l, 0], outb[0][:].rearrange("p a b -> p (a b)"), reads=[Toutb[0]], writes=[self.Ts5m[l][0]], owner=self.Ts5m[l][0])
            for b in range(8):
                ps, Tp = self.psum_next()
                for gi in range(4):
                    g = 4 * b + gi
                    kb.op('pe', lambda e: e.matmul(ps[:, gi * 128:(gi + 1) * 128], g3(big["WBn"], g), g3(big["WC"], g), start=True, stop=True), reads=[Tbig["WBn"], Tbig["WC"]], writes=[Tp])
                dve(lambda e: e.tensor_tensor(out=tmpT[:], in0=ps[:].rearrange("p (a b) -> p a b", b=128), in1=tmask.unsqueeze(1).to_broadcast([P, 4, 128]), op=ALU.mult), [Tp, Tc], [TtmpT])
                for gi in range(4):
                    g = 4 * b + gi
                    dve(lambda e: e.scalar_tensor_tensor(out=outb[1][:, g, :], in0=idf, scalar=dc[:, g:g + 1], in1=tmpT[:, gi, :], op0=ALU.mult, op1=ALU.add), [TtmpT, Tc, Tsp], [Toutb[1]])
            kb.dma('sp', self.s5m[l, 1], outb[1][:].rearrange("p a b -> p (a b)"), reads=[Toutb[1]], writes=[self.Ts5m[l][1]], owner=self.Ts5m[l][1])
            dve(lambda e: e.tensor_copy(outb[0][:].rearrange("p a b -> p (a b)"), big["WC"][:].rearrange("p g s h -> p (g s h)")), [Tbig["WC"]], [Toutb[0]])
            kb.dma('sp', self.s5m[l, 2], outb[0][:].rearrange("p a b -> p (a b)"), reads=[Toutb[0]], writes=[self.Ts5m[l][2]], owner=self.Ts5m[l][2])
            m1 = big["m1"][:].rearrange("p g s h -> p g (s h)"); m2 = big["m2"][:].rearrange("p g s h -> p g (s h)")
            for k in range(8):
                colC = tb["Cn"][:, 24 + k, :].unsqueeze(2).to_broadcast([P, 32, 128])
                colS = tb["SnA"][:, 24 + k, :].unsqueeze(2).to_broadcast([P, 32, 128])
                ob = outb[(k + 1) % 2]; To = Toutb[(k + 1) % 2]
                dve(lambda e: e.tensor_tensor(out=m1, in0=idf.unsqueeze(1).to_broadcast([P, 32, 128]), in1=colC, op=ALU.mult), [Tc, Ttb], [Tbig["m1"]])
                kb.op('pool', lambda e: e.tensor_tensor(out=m2, in0=swapm.unsqueeze(1).to_broadcast([P, 32, 128]), in1=colS, op=ALU.mult), reads=[Tc, Ttb], writes=[Tbig["m2"]])
                dve(lambda e: e.tensor_tensor(out=ob[:], in0=m1, in1=m2, op=ALU.add), [Tbig["m1"], Tbig["m2"]], [To])
                kb.dma('sp', self.s5m[l, 3 + k], ob[:].rearrange("p a b -> p (a b)"), reads=[To], writes=[self.Ts5m[l][3 + k]], owner=self.Ts5m[l][3 + k])
            kb.full_barrier()
            for e_ in ('sp', 'dve', 'act', 'pool', 'pe'):
                kb.barrier(Toutb + [Tsp], eng=e_)

    def main(self, es):
        nc = self.nc; kb = self.kb
        self.xres = self.sb(es, "xres", [P, DC, L], F32)
        self.Tx = [[Tok("x%d_%d" % (b, c)) for c in range(DC)] for b in range(4)]
        self.hbuf = self.sb(es, "hbuf", [P, DC, TT], BF16)
        self.Th = [Tok("h0"), Tok("h1")]
        self.ycat = self.sb(es, "ycat", [P, DC, TT], BF16)
        self.Tyc = [[Tok("yc%d_%d" % (hb, c)) for c in range(DC)] for hb in range(2)]
        self.cur_slot = self.sb(es, "WinGslot", [P, DC, 1552], BF16)
        self.carry = self.sb(es, "carry", [P, G], F32); self.Tcarry = Tok("carry")
        self.gst_f = self.sb(es, "gst_f", [P, 2, 128], F32)
        self.gst_b = [self.sb(es, "gst_b%d" % i, [P, 2, 128], BF16) for i in range(2)]
        self.Tgf = Tok("gst_f"); self.Tgb = [Tok("gst_b0"), Tok("gst_b1")]
        for s in range(self.nseq):
            kb.new_epoch()
            for b in range(4):
                kb.dma('sp', self.xres[:, :, b * 512:(b + 1) * 512], self.xT[s, :, :, b * 512:(b + 1) * 512], writes=self.Tx[b], owner=self.Tx[b][0])
            self.stopped = (self.stop_after == 'load')
            for li, l in enumerate(self.layers):
                if li + 1 < len(self.layers): self.next_layer = self.layers[li + 1]
                elif s + 1 < self.nseq: self.next_layer = self.layers[0]
                else: self.next_layer = None
                for tile in range(2):
                    if not self.stopped: self.mixer(l, tile)
                for tile in range(2):
                    if not self.stopped: self.ffn(l, tile)
            if 'xres' in self.dbg_out:
                kb.dma('sp', self.dbg_out['xres'], self.xres[:], reads=[t for b in range(4) for t in self.Tx[b]], owner=self.Tx[0][0])
            if self.final:
                self.final_norm(s)
            else:
                for b in range(4):
                    kb.dma('sp', self.yT[s, :, :, b * 512:(b + 1) * 512], self.xres[:, :, b * 512:(b + 1) * 512], reads=self.Tx[b], owner=self.Tx[b][1])
        allt = [t for b in range(4) for t in self.Tx[b]] + getattr(self, 'Tout', [])
        kb.full_barrier()
        for e_ in ('sp', 'act', 'pool'):
            kb.barrier(allt, eng=e_)

    def rmsnorm_block(self, es_scr, blk, col0, dst_fn, Tdst):
        kb = self.kb
        sq, Tsq, rs, Trs = es_scr
        t0 = blk * 512
        Tx = self.Tx[blk]
        kb.op('act', lambda e: e.activation(out=sq[:], in_=self.xres[:, :, t0:t0 + 512], func=AF.Square), reads=Tx, writes=[Tsq])
        ps, Tp = self.psum_next()
        for c in range(DC):
            kb.op('pe', lambda e: e.matmul(ps[:], self.ones_bf[:], sq[:, c, :], start=(c == 0), stop=(c == DC - 1)), reads=[Tsq, self.Tconst], writes=[Tp])
        kb.op('act', lambda e: e.activation(out=rs[:], in_=ps[:], func=AF.Sqrt, scale=1.0 / D, bias=self.epsc[:, 0:1]), reads=[Tp, self.Tconst], writes=[Trs])
        kb.op('dve', lambda e: e.reciprocal(rs[:], rs[:]), reads=[Trs], writes=[Trs])
        for c in range(DC):
            kb.op('dve', lambda e: e.scalar_tensor_tensor(out=dst_fn(c), in0=self.xres[:, c, t0:t0 + 512], scalar=self.cols[:, col0 + c:col0 + c + 1], in1=rs[:], op0=ALU.mult, op1=ALU.mult), reads=[Tx[c], Trs, self.Tconst], writes=Tdst)

    def norm_scratch(self, es):
        sq = self.sb(es, "nsq", [P, DC, 512], BF16); rs = self.sb(es, "nrs", [P, 512], F32)
        return (sq, Tok("nsq"), rs, Tok("nrs"))

    def mixer(self, l, tile):
        nc = self.nc; kb = self.kb
        with contextlib.ExitStack() as es:
            self.wing_slot(es)
            if getattr(self, 'wing_valid', None) != l:
                self.load_wing(l)
            scr = self.norm_scratch(es)
            for hb in range(2):
                self.rmsnorm_block(scr, tile * 2 + hb, C_NMIX + l * 8, lambda c: self.hbuf[:, c, hb * 512:(hb + 1) * 512], [self.Th[hb]])
            kb.full_barrier()
        if self.stop_after == 'norm': self.stopped = True; return
        with contextlib.ExitStack() as esm:
            self.WSs = self.sb(esm, "WSs", [P, 6144], BF16)
            with contextlib.ExitStack() as es:
                self.gla(es, l, tile)
                kb.full_barrier()
            with contextlib.ExitStack() as es:
                self.s5(es, l, tile)
                kb.full_barrier()
        if self.stop_after == 's5': self.stopped = True; return
        with contextlib.ExitStack() as es:
            self.wout(es, l, tile)
            kb.full_barrier()
        if self.stop_after == 'wout': self.stopped = True; return

    def gla(self, es, l, tile):
        nc = self.nc; kb = self.kb
        sb = lambda n, s, d: self.sb(es, "g_" + n, s, d)
        WinG = self.wing_slot(es); TW = self.ptok("WinG")
        if getattr(self, 'wing_valid', None) != l:
            self.load_wing(l)
        WSs = self.WSs
        kb.dma('pool', WSs[:, 0:4096].rearrange("p (c n) -> p c n", c=DC), self.w_in[l, :, :, 0:512], writes=[self.ptok("WinS")])
        kb.dma('pool', WSs[:, 4096:6144].rearrange("p (c n) -> p c n", c=4), self.w_glu[l], writes=[self.ptok("Wglu")])
        glx = sb("glx", [33, TT], BF16); Tglx = Tok("glx")
        Epos = sb("Epos", [P, 2, TT], F32); Eneg = sb("Eneg", [P, 2, TT], F32)
        TEp = [Tok("Ep%d" % i) for i in range(8)]
        qd = sb("qd", [P, 2, TT], BF16); ki = sb("ki", [P, 2, TT], BF16); Tqk = [Tok("qk0"), Tok("qk1")]
        gs = sb("gs", [P, 4, TT], BF16); Tgs = [Tok("gs0"), Tok("gs1")]
        vt = sb("vt", [P, 8, 512], BF16); Tvt = [Tok("vt%d" % i) for i in range(8)]
        ke = sb("ke", [P, 8, 256], BF16); Tke = [Tok("ke%d" % i) for i in range(8)]
        Erc = [sb("Erc%d" % i, [P, 256], F32) for i in range(2)]; TErc = [Tok("Erc0"), Tok("Erc1")]
        e1 = [sb("e1_%d" % i, [P, 256], F32) for i in range(2)]; Te1 = [Tok("e1_0"), Tok("e1_1")]
        nl = [sb("nl_%d" % i, [P, 256], F32) for i in range(2)]; Tnl = [Tok("nl0"), Tok("nl1")]
        sT = [sb("sT%d" % i, [P, 4, 128], BF16) for i in range(2)]; TsT = [Tok("sT0"), Tok("sT1")]
        on = [sb("on%d" % i, [P, 4, 128], BF16) for i in range(2)]; Ton = [Tok("on0"), Tok("on1")]
        junk = sb("junk", [P, 128], BF16); Tjunk = Tok("junk")
        ss = [sb("ss%d" % i, [P, 4], F32) for i in range(2)]; Tss = [Tok("ss0"), Tok("ss1")]
        Tc = self.Tconst
        if tile == 0:
            kb.op('dve', lambda e: e.memset(self.gst_f[:], 0.0), writes=[self.Tgf])
            kb.op('dve', lambda e: e.memset(self.gst_b[0][:], 0.0), writes=[self.Tgb[0]])
        kb.op('dve', lambda e: e.memset(glx[:], 0.0), writes=[Tglx])
        kb.op('dve', lambda e: e.memset(glx[32:33, :], 1.0), writes=[Tglx])
        for hb in range(2):
            ps, Tp = self.psum_next()
            for c in range(DC):
                kb.op('pe', lambda e: e.matmul(ps[0:16, :], WinG[:, c, 1536:1552], self.hbuf[:, c, hb * 512:(hb + 1) * 512], start=(c == 0), stop=(c == DC - 1)), reads=[TW, self.Th[hb]], writes=[Tp])
            kb.op('act', lambda e: e.activation(out=glx[0:16, hb * 512:(hb + 1) * 512], in_=ps[0:16, :], func=AF.Copy), reads=[Tp], writes=[Tglx])
        for st in range(8):
            tk = slice(st * 128, (st + 1) * 128); hb = st // 4; r = st % 2
            ps, Tp = self.psum_next()
            kb.op('pe', lambda e: e.matmul(ps[:, 0:256], glx[0:33, tk], self.wgx[0:33, l, :], start=True, stop=True), reads=[Tglx, self.Twgx], writes=[Tp])
            kb.op('act', lambda e: e.activation(out=e1[r][:], in_=ps[:, 0:256], func=AF.Exp, scale=-1.0), reads=[Tp], writes=[Te1[r]])
            ps, Tp = self.psum_next()
            for c in range(DC):
                kb.op('pe', lambda e: e.matmul(ps[:], self.hbuf[:, c, tk], WinG[:, c, 512:1024], start=(c == 0), stop=(c == DC - 1)), reads=[TW, self.Th[hb]], writes=[Tp])
            kb.op('act', lambda e: e.activation(out=vt[:, st, :], in_=ps[:], func=AF.Copy), reads=[Tp], writes=[Tvt[st]])
            kb.op('act', lambda e: e.activation(out=nl[r][:], in_=e1[r][:], func=AF.Ln, bias=self.epsc[:, 1:2]), reads=[Te1[r], Tc], writes=[Tnl[r]])
            ps, Tp = self.psum_next()
            for c in range(2):
                kb.op('pe', lambda e: e.matmul(ps[:, c * 128:(c + 1) * 128], nl[r][:, c * 128:(c + 1) * 128], self.cmask[:], start=True, stop=True), reads=[Tnl[r], Tc], writes=[Tp])
            kb.op('act', lambda e: e.activation(out=Epos[:, :, tk], in_=ps[:, 0:256].rearrange("p (c t) -> p c t", c=2), func=AF.Exp, scale=-1.0 / 16), reads=[Tp], writes=[TEp[st]])
            kb.op('act', lambda e: e.activation(out=Eneg[:, :, tk], in_=ps[:, 0:256].rearrange("p (c t) -> p c t", c=2), func=AF.Exp, scale=1.0 / 16), reads=[Tp], writes=[TEp[st]])
            ps, Tp = self.psum_next()
            kb.op('pe', lambda e: e.matmul(ps[:, 0:256], self.triR[:], nl[r][:], start=True, stop=True), reads=[Tnl[r], Tc], writes=[Tp])
            kb.op('act', lambda e: e.activation(out=Erc[r][:], in_=ps[:, 0:256], func=AF.Exp, scale=-1.0 / 16), reads=[Tp], writes=[TErc[r]])
            ps, Tp = self.psum_next()
            for c in range(DC):
                kb.op('pe', lambda e: e.matmul(ps[:, 0:256], self.hbuf[:, c, tk], WinG[:, c, 256:512], start=(c == 0), stop=(c == DC - 1)), reads=[TW, self.Th[hb]], writes=[Tp])
            kb.op('dve', lambda e: e.tensor_tensor(out=ke[:, st, :], in0=ps[:, 0:256], in1=Erc[r][:], op=ALU.mult), reads=[Tp, TErc[r]], writes=[Tke[st]])
        for hb in range(2):
            hs = slice(hb * 512, (hb + 1) * 512)
            TE = TEp[hb * 4:(hb + 1) * 4]
            for c in range(2):
                ps, Tp = self.psum_next()
                for kc in range(DC):
                    kb.op('pe', lambda e: e.matmul(ps[:], WinG[:, kc, c * 128:(c + 1) * 128], self.hbuf[:, kc, hs], start=(kc == 0), stop=(kc == DC - 1)), reads=[TW, self.Th[hb]], writes=[Tp])
                kb.op('dve', lambda e: e.scalar_tensor_tensor(out=qd[:, c, hs], in0=ps[:], scalar=0.125, in1=Epos[:, c, hs], op0=ALU.mult, op1=ALU.mult), reads=[Tp] + TE, writes=[Tqk[hb]])
                ps, Tp = self.psum_next()
                for kc in range(DC):
                    kb.op('pe', lambda e: e.matmul(ps[:], WinG[:, kc, 256 + c * 128:256 + (c + 1) * 128], self.hbuf[:, kc, hs], start=(kc == 0), stop=(kc == DC - 1)), reads=[TW, self.Th[hb]], writes=[Tp])
                kb.op('dve', lambda e: e.tensor_tensor(out=ki[:, c, hs], in0=ps[:], in1=Eneg[:, c, hs], op=ALU.mult), reads=[Tp] + TE, writes=[Tqk[hb]])
            for c in range(4):
                ps, Tp = self.psum_next()
                for kc in range(DC):
                    kb.op('pe', lambda e: e.matmul(ps[:], WinG[:, kc, 1024 + c * 128:1024 + (c + 1) * 128], self.hbuf[:, kc, hs], start=(kc == 0), stop=(kc == DC - 1)), reads=[TW, self.Th[hb]], writes=[Tp])
                kb.op('act', lambda e: e.activation(out=gs[:, c, hs], in_=ps[:], func=AF.Silu), reads=[Tp], writes=[Tgs[hb]])
        cur = 0
        for st in range(8):
            tk = slice(st * 128, (st + 1) * 128); hb = st // 4; r = st % 2
            psb = [self.psum_next(), self.psum_next()]
            for hd in range(4):
                c = hd // 2; par = hd % 2; pr = slice(par * 64, par * 64 + 64)
                ps, Tp = psb[par]
                kb.op('pe', lambda e: e.matmul(ps[:, c * 128:(c + 1) * 128], ki[pr, c, tk], qd[pr, c, tk], start=True, stop=True), reads=[Tqk[hb]], writes=[Tp])
            for par in range(2):
                ps, Tp = psb[par]
                dst = sT[r][:].rearrange("p (c two) t -> p two c t", two=2)[:, par, :, :]
                kb.op('dve', lambda e: e.tensor_tensor(out=dst, in0=ps[:, 0:256].rearrange("p (a b) -> p a b", b=128), in1=self.cmask[:].unsqueeze(1).to_broadcast([P, 2, 128]), op=ALU.mult), reads=[Tp, Tc], writes=[TsT[r]])
            def upd_state(half, dst):
                rows = slice(half * 64, half * 64 + 64)
                psu, Tpu = self.psum_next()
                for hd in range(4):
                    c = hd // 2; pr = slice((hd % 2) * 64, (hd % 2) * 64 + 64)
                    kb.op('pe', lambda e: e.matmul(psu[pr, c * 128:(c + 1) * 128], ke[rows, st, hd * 64:(hd + 1) * 64], vt[rows, st, hd * 128:(hd + 1) * 128], start=True, stop=True), reads=[Tke[st], Tvt[st]], writes=[Tpu])
                tend = st * 128 + half * 64 + 63
                for c in range(2):
                    kb.op('dve', lambda e: e.scalar_tensor_tensor(out=self.gst_f[:, c, :], in0=self.gst_f[:, c, :], scalar=Epos[:, c, tend:tend + 1], in1=psu[:, c * 128:(c + 1) * 128], op0=ALU.mult, op1=ALU.add), reads=[self.Tgf, TEp[st], Tpu], writes=[self.Tgf])
                kb.op('act', lambda e: e.activation(out=self.gst_b[dst][:].rearrange("p a b -> p (a b)"), in_=self.gst_f[:].rearrange("p a b -> p (a b)"), func=AF.Copy), reads=[self.Tgf], writes=[self.Tgb[dst]])
            upd_state(0, 1)
            pob = [self.psum_next(), self.psum_next()]
            for hd in range(4):
                c = hd // 2; par = hd % 2; pr = slice(par * 64, par * 64 + 64)
                pso, Tpo = pob[par]
                oc = slice(c * 128, (c + 1) * 128)
                kb.op('pe', lambda e: e.matmul(pso[:, oc], sT[r][:, hd, :], vt[:, st, hd * 128:(hd + 1) * 128], start=True, stop=False, skip_group_check=True), reads=[TsT[r], Tvt[st]], writes=[Tpo])
                kb.op('pe', lambda e: e.matmul(pso[0:64, oc], qd[pr, c, st * 128:st * 128 + 64], self.gst_b[0][pr, c, :], start=False, stop=False, skip_group_check=True), reads=[Tqk[hb], self.Tgb[0]], writes=[Tpo])
                kb.op('pe', lambda e: e.matmul(pso[64:128, oc], qd[pr, c, st * 128 + 64:st * 128 + 128], self.gst_b[1][pr, c, :], start=False, stop=True, skip_group_check=True), reads=[Tqk[hb], self.Tgb[1]], writes=[Tpo])
            upd_state(1, 0)
            for hd in range(4):
                c = hd // 2; par = hd % 2; pso, Tpo = pob[par]
                kb.op('act', lambda e: e.activation(out=junk[:], in_=pso[:, c * 128:(c + 1) * 128], func=AF.Square, accum_out=ss[r][:, hd:hd + 1]), reads=[Tpo], writes=[Tjunk, Tss[r]])
            kb.op('act', lambda e: e.activation(out=ss[r][:], in_=ss[r][:], func=AF.Sqrt, scale=1.0 / 128, bias=self.epsc[:, 0:1]), reads=[Tss[r], Tc], writes=[Tss[r]])
            kb.op('dve', lambda e: e.reciprocal(ss[r][:], ss[r][:]), reads=[Tss[r]], writes=[Tss[r]])
            for par in range(2):
                pso, Tpo = pob[par]
                dst = on[r][:].rearrange("p (c two) t -> p two c t", two=2)[:, par, :, :]
                sc = ss[r][:].rearrange("p (c two) -> p two c", two=2)[:, par, :].unsqueeze(2).to_broadcast([P, 2, 128])
                kb.op('dve', lambda e: e.tensor_tensor(out=dst, in0=pso[:, 0:256].rearrange("p (a b) -> p a b", b=128), in1=sc, op=ALU.mult), reads=[Tpo, Tss[r]], writes=[Ton[r]])
            pst, Tpt = self.psum_next()
            pstb = pst[:].bitcast(BF16)
            for hd in range(4):
                kb.op('pe', lambda e: e.transpose(pstb[:, hd * 128:(hd + 1) * 128], on[r][:, hd, :], self.ident_bf[:]), reads=[Ton[r], Tc], writes=[Tpt])
            for hd in range(4):
                kb.op('dve', lambda e: e.scalar_tensor_tensor(out=self.ycat[:, 4 + hd, tk], in0=pstb[:, hd * 128:(hd + 1) * 128], scalar=self.cols[:, C_GLAN + l * 4 + hd:C_GLAN + l * 4 + hd + 1], in1=gs[:, hd, tk], op0=ALU.mult, op1=ALU.mult), reads=[Tpt, Tc, Tgs[hb]], writes=[self.Tyc[hb][4 + hd]])

    def s5(self, es, l, tile):
        nc = self.nc; kb = self.kb
        sb = lambda n, s, d: self.sb(es, "s_" + n, s, d)
        Tc = self.Tconst
        slot = self.cur_slot
        self.wing_valid = None
        flat = slot[:].rearrange("p a b -> p (a b)")
        WSs = self.WSs
        WinS = WSs[:, 0:4096].rearrange("p (c n) -> p c n", c=DC); TW = self.ptok("WinS")
        Wglu = WSs[:, 4096:6144].rearrange("p (c n) -> p c n", c=4); TWg = self.ptok("Wglu")
        mats = [sb("mat%d" % i, [P, G, 128], BF16) for i in range(3)]; Tm = [self.ptok("mat%d" % i) for i in range(3)]
        for i in range(3):
            kb.dma('sp', mats[i][:].rearrange("p a b -> p (a b)"), self.s5m[l, i], reads=[self.Ts5m[l][i]], writes=[Tm[i]], owner=Tm[i])
        WB, Toep, WC = mats
        ring = [sb("ring%d" % i, [P, G, 128], BF16) for i in range(2)]; Tring = [self.ptok("ring0"), self.ptok("ring1")]
        bufA = sb("bufA", [P, 4, TT], BF16); TA = [Tok("bufA%d" % i) for i in range(4)]
        UT = sb("UT", [P, G, 128], BF16); TUT = [Tok("UT%d" % i) for i in range(8)]
        Sf = flat[:, 0:8256].bitcast(F32).rearrange("p (g j) -> p g j", j=129)
        Sb_ = flat[:, 8256:12384].rearrange("p (g j) -> p g j", j=129)
        TSf = [Tok("Sf%d" % i) for i in range(8)]; TSb = [Tok("Sb%d" % i) for i in range(8)]
        yg = ring[0]; Tyg = [Tok("yg%d" % i) for i in range(4)]
        y2 = self.hbuf
        if tile == 0:
            kb.op('dve', lambda e: e.memset(self.carry[:], 0.0), writes=[self.Tcarry])
        for cc in range(4):
            for hb in range(2):
                ps, Tp = self.psum_next()
                for c in range(DC):
                    kb.op('pe', lambda e: e.matmul(ps[:], WinS[:, c, cc * 128:(cc + 1) * 128], self.hbuf[:, c, hb * 512:(hb + 1) * 512], start=(c == 0), stop=(c == DC - 1)), reads=[TW, self.Th[hb]], writes=[Tp])
                eng = self.evac_eng()
                if eng == 'act':
                    kb.op('act', lambda e: e.activation(out=bufA[:, cc, hb * 512:(hb + 1) * 512], in_=ps[:], func=AF.Copy), reads=[Tp], writes=[TA[cc]])
                else:
                    kb.op('dve', lambda e: e.tensor_copy(bufA[:, cc, hb * 512:(hb + 1) * 512], ps[:]), reads=[Tp], writes=[TA[cc]])
        for b in range(8):
            ps, Tp = self.psum_next()
            for gi in range(4):
                g = 4 * b + gi; cc = g // 8; gl = g % 8
                for s_ in range(8):
                    mv = bufA[:, cc, :].rearrange("p (j s) -> p s j", s=8)[:, s_, :]
                    kb.op('pe', lambda e: e.matmul(ps[:, gi * 128:(gi + 1) * 128], self.strips[:, gl, 112 - 16 * s_:240 - 16 * s_], mv, start=(s_ == 0), stop=(s_ == 7)), reads=[TA[cc], Tc], writes=[Tp])
            eng = self.evac_eng()
            dst = UT[:, 4 * b:4 * b + 4, :].rearrange("p a b -> p (a b)")
            if eng == 'act':
                kb.op('act', lambda e: e.activation(out=dst, in_=ps[:], func=AF.Copy), reads=[Tp], writes=[TUT[b]])
            else:
                kb.op('dve', lambda e: e.tensor_copy(dst, ps[:]), reads=[Tp], writes=[TUT[b]])
        for b in range(8):
            ps, Tp = self.psum_next()
            for gi in range(4):
                g = 4 * b + gi
                kb.op('pe', lambda e: e.matmul(ps[:, gi * 128:(gi + 1) * 128], WB[:, g, :], UT[:, g, :], start=True, stop=True), reads=[Tm[0], TUT[b]], writes=[Tp])
            g4 = slice(4 * b, 4 * b + 4)
            kb.op('dve', lambda e: e.tensor_copy(Sf[:, g4, 1:129], ps[:].rearrange("p (a b) -> p a b", b=128)), reads=[Tp], writes=[TSf[b]])
            kb.op('dve', lambda e: e.tensor_copy(Sf[:, g4, 0:1], self.carry[:, g4].unsqueeze(2)), reads=[self.Tcarry], writes=[TSf[b]])
            kb.op('act', lambda e: e.activation(out=Sb_[:, g4, :], in_=Sf[:, g4, :], func=AF.Copy), reads=[TSf[b]], writes=[TSb[b]])
        for k in range(8):
            d = 1 << k; n = 129 - d
            rg = ring[k % 2]; Tr = Tring[k % 2]
            kb.dma('sp', rg[:].rearrange("p a b -> p (a b)"), self.s5m[l, 3 + k], reads=[self.Ts5m[l][3 + k]], writes=[Tr], owner=Tr)
            for b in range(8):
                ps, Tp = self.psum_next()
                g4 = slice(4 * b, 4 * b + 4)
                for gi in range(4):
                    g = 4 * b + gi
                    kb.op('pe', lambda e: e.matmul(ps[:, gi * 128:gi * 128 + n], rg[:, g, :], Sb_[:, g, 0:n], start=True, stop=True), reads=[Tr, TSb[b]], writes=[Tp])
                kb.op('dve', lambda e: e.tensor_tensor(out=Sf[:, g4, d:129], in0=Sf[:, g4, d:129], in1=ps[:].rearrange("p (a b) -> p a b", b=128)[:, :, 0:n], op=ALU.add), reads=[Tp, TSf[b]], writes=[TSf[b]])
                kb.op('act', lambda e: e.activation(out=Sb_[:, g4, d:129], in_=Sf[:, g4, d:129], func=AF.Copy), reads=[TSf[b]], writes=[TSb[b]])
        kb.op('dve', lambda e: e.tensor_copy(self.carry[:].unsqueeze(2), Sf[:, :, 128:129]), reads=TSf, writes=[self.Tcarry])
        for b in range(8):
            ps, Tp = self.psum_next()
            for gi in range(4):
                g = 4 * b + gi
                kb.op('pe', lambda e: e.matmul(ps[:, gi * 128:(gi + 1) * 128], Toep[:, g, :], UT[:, g, :], start=True, stop=False), reads=[Tm[1], TUT[b]], writes=[Tp])
                kb.op('pe', lambda e: e.matmul(ps[:, gi * 128:(gi + 1) * 128], WC[:, g, :], Sb_[:, g, 0:128], start=False, stop=True), reads=[Tm[2], TSb[b]], writes=[Tp])
            kb.op('act', lambda e: e.activation(out=yg[:, 4 * b:4 * b + 4, :].rearrange("p a b -> p (a b)"), in_=ps[:], func=AF.Gelu_apprx_tanh), reads=[Tp], writes=[Tyg[b // 2], Tring[0]])
        for cc in range(4):
            for th in range(2):
                ps, Tp = self.psum_next()
                for ti in range(4):
                    t0 = th * 4 + ti
                    for gl in range(8):
                        kb.op('pe', lambda e: e.matmul(ps[:, ti * 128:(ti + 1) * 128], self.strips[:, t0, 112 - 16 * gl:240 - 16 * gl], yg[:, cc * 8 + gl, :], start=(gl == 0), stop=(gl == 7)), reads=[Tyg[cc], Tc], writes=[Tp])
                dst = bufA[:, cc, :].rearrange("p (j s) -> p s j", s=8)[:, th * 4:th * 4 + 4, :]
                eng = self.evac_eng()
                if eng == 'act':
                    kb.op('act', lambda e: e.activation(out=dst, in_=ps[:].rearrange("p (a b) -> p a b", b=128), func=AF.Copy), reads=[Tp], writes=[TA[cc]])
                else:
                    kb.op('dve', lambda e: e.tensor_copy(dst, ps[:].rearrange("p (a b) -> p a b", b=128)), reads=[Tp], writes=[TA[cc]])
        if 'yg' in self.dbg_out and tile == 0:
            kb.dma('pool', self.dbg_out['yg'], bufA[:], reads=TA, owner=TA[0])
        sq = UT[:, 0:16, :].rearrange("p (a b) c -> p a (b c)", a=4); Tsq = TUT[0:4]
        rs = UT[:, 16:24, :].rearrange("p a b -> p (a b)").bitcast(F32); Trs = TUT[4:6]
        gt = [UT[:, 24:28, :].rearrange("p a b -> p (a b)"), UT[:, 28:32, :].rearrange("p a b -> p (a b)")]; Tgt = [TUT[6], TUT[7]]
        for hb in range(2):
            hs = slice(hb * 512, (hb + 1) * 512)
            for co in range(4):
                ps, Tp = self.psum_next()
                for ci in range(4):
                    kb.op('pe', lambda e: e.matmul(ps[:], Wglu[:, ci, co * 128:(co + 1) * 128], bufA[:, ci, hs], start=(ci == 0), stop=(ci == 3)), reads=[TWg] + TA, writes=[Tp])
                r = co % 2
                kb.op('act', lambda e: e.activation(out=gt[r], in_=ps[:], func=AF.Sigmoid, bias=self.cols[:, C_BGLU + l * 4 + co:C_BGLU + l * 4 + co + 1]), reads=[Tp, Tc], writes=[Tgt[r]])
                kb.op('dve', lambda e: e.tensor_tensor(out=y2[:, co, hs], in0=bufA[:, co, hs], in1=gt[r], op=ALU.mult), reads=[TA[co], Tgt[r]], writes=[self.Th[hb]])
        for hb in range(2):
            hs = slice(hb * 512, (hb + 1) * 512)
            kb.op('act', lambda e: e.activation(out=sq, in_=y2[:, 0:4, hs], func=AF.Square), reads=[self.Th[hb]], writes=Tsq)
            ps, Tp = self.psum_next()
            for c in range(4):
                kb.op('pe', lambda e: e.matmul(ps[:], self.ones_bf[:], sq[:, c, :], start=(c == 0), stop=(c == 3)), reads=Tsq + [Tc], writes=[Tp])
            kb.op('act', lambda e: e.activation(out=rs, in_=ps[:], func=AF.Sqrt, scale=1.0 / 512, bias=self.epsc[:, 0:1]), reads=[Tp, Tc], writes=Trs)
            kb.op('dve', lambda e: e.reciprocal(rs, rs), reads=Trs, writes=Trs)
            for c in range(4):
                kb.op('dve', lambda e: e.scalar_tensor_tensor(out=self.ycat[:, c, hs], in0=y2[:, c, hs], scalar=self.cols[:, C_S5N + l * 4 + c:C_S5N + l * 4 + c + 1], in1=rs, op0=ALU.mult, op1=ALU.mult), reads=[self.Th[hb], Tc] + Trs, writes=[self.Tyc[hb][c]])

    def wout(self, es, l, tile):
        kb = self.kb
        self.wing_slot(es)
        Wout = self.sb(es, "Wout", [P, DC, D], BF16); TWo = [self.ptok("Wout%d" % c) for c in range(DC)]
        for co in range(DC):
            kb.dma('pool', Wout[:, :, co * 128:(co + 1) * 128], self.w_out[l, :, :, co * 128:(co + 1) * 128], writes=[TWo[co]])
        if tile == 0:
            self.load_wing(l)
        if 'ycat' in self.dbg_out and tile == 0:
            dtmp = self.sb(es, "dtmp2", [P, DC, TT], F32); Td = Tok("dtmp2")
            kb.op('dve', lambda e: e.tensor_copy(dtmp[:], self.ycat[:]), reads=[t for hb in range(2) for t in self.Tyc[hb]], writes=[Td])
            kb.dma('sp', self.dbg_out['ycat'], dtmp[:], reads=[Td])
            kb.barrier([Td], eng='dve')
        for hb in range(2):
            blk = tile * 2 + hb; t0 = blk * 512
            for co in range(DC):
                ps, Tp = self.psum_next()
                for ci in range(DC):
                    kb.op('pe', lambda e: e.matmul(ps[:], Wout[:, ci, co * 128:(co + 1) * 128], self.ycat[:, ci, hb * 512:(hb + 1) * 512], start=(ci == 0), stop=(ci == DC - 1)), reads=[TWo[co], self.Tyc[hb][ci]], writes=[Tp])
                kb.op('dve', lambda e: e.tensor_tensor(out=self.xres[:, co, t0:t0 + 512], in0=ps[:], in1=self.xres[:, co, t0:t0 + 512], op=ALU.add), reads=[Tp, self.Tx[blk][co]], writes=[self.Tx[blk][co]])

    def ffn(self, l, tile):
        kb = self.kb
        with contextlib.ExitStack() as es:
            self.wing_slot(es)
            scr = self.norm_scratch(es)
            for hb in range(2):
                self.rmsnorm_block(scr, tile * 2 + hb, C_NFFN + l * 8, lambda c: self.hbuf[:, c, hb * 512:(hb + 1) * 512], [self.Th[hb]])
            kb.full_barrier()
        with contextlib.ExitStack() as es:
            sb = lambda n, s, d: self.sb(es, "f_" + n, s, d)
            self.wing_slot(es)
            if tile == 1 and self.next_layer is not None:
                self.load_wing(self.next_layer)
            act = sb("act", [P, FC, TT], BF16); Tact = [[Tok("act%d_%d" % (f, hb)) for hb in range(2)] for f in range(FC)]
            NR1 = 4; NR2 = 2
            W1 = [sb("W1_%d" % i, [P, DC, 2, 128], BF16) for i in range(NR1)]; TW1 = [self.ptok("W1_%d" % i) for i in range(NR1)]
            W2 = [sb("W2_%d" % i, [P, FC, 128], BF16) for i in range(NR2)]; TW2 = [self.ptok("W2_%d" % i) for i in range(NR2)]
            sg = [sb("sg%d" % i, [P, 512], BF16) for i in range(2)]; Tsg = [Tok("sg0"), Tok("sg1")]
            for f in range(FC):
                w = W1[f % NR1]; Tw = TW1[f % NR1]
                kb.dma('pool', w[:, :, 0, :], self.w_f1[l, :, :, f * 128:(f + 1) * 128], writes=[Tw])
                kb.dma('pool', w[:, :, 1, :], self.w_f1[l, :, :, DFF + f * 128:DFF + (f + 1) * 128], writes=[Tw])
                for hb in range(2):
                    hs = slice(hb * 512, (hb + 1) * 512)
                    psg, Tpg = self.psum_next()
                    for c in range(DC):
                        kb.op('pe', lambda e: e.matmul(psg[:], w[:, c, 0, :], self.hbuf[:, c, hs], start=(c == 0), stop=(c == DC - 1)), reads=[Tw, self.Th[hb]], writes=[Tpg])
                    psu, Tpu = self.psum_next()
                    for c in range(DC):
                        kb.op('pe', lambda e: e.matmul(psu[:], w[:, c, 1, :], self.hbuf[:, c, hs], start=(c == 0), stop=(c == DC - 1)), reads=[Tw, self.Th[hb]], writes=[Tpu])
                    r = (2 * f + hb) % 2
                    kb.op('act', lambda e: e.activation(out=sg[r][:], in_=psg[:], func=AF.Silu), reads=[Tpg], writes=[Tsg[r]])
                    kb.op('dve', lambda e: e.tensor_tensor(out=act[:, f, hs], in0=psu[:], in1=sg[r][:], op=ALU.mult), reads=[Tpu, Tsg[r]], writes=[Tact[f][hb]])
            for co in range(DC):
                w = W2[co % NR2]; Tw = TW2[co % NR2]
                kb.dma('pool', w[:], self.w_f2[l, :, :, co * 128:(co + 1) * 128], writes=[Tw])
                for hb in range(2):
                    blk = tile * 2 + hb; t0 = blk * 512
                    ps, Tp = self.psum_next()
                    for f in range(FC):
                        kb.op('pe', lambda e: e.matmul(ps[:], w[:, f, :], act[:, f, hb * 512:(hb + 1) * 512], start=(f == 0), stop=(f == FC - 1)), reads=[Tw, Tact[f][hb]], writes=[Tp])
                    kb.op('dve', lambda e: e.tensor_tensor(out=self.xres[:, co, t0:t0 + 512], in0=ps[:], in1=self.xres[:, co, t0:t0 + 512], op=ALU.add), reads=[Tp, self.Tx[blk][co]], writes=[self.Tx[blk][co]])
            kb.full_barrier()
            for e_ in ('pool',):
                kb.barrier(TW1 + TW2, eng=e_)

    def final_norm(self, s):
        kb = self.kb
        with contextlib.ExitStack() as es:
            self.wing_slot(es)
            scr = self.norm_scratch(es)
            ob = [self.sb(es, "fo%d" % i, [P, DC, 512], F32) for i in range(2)]
            To = [self.ptok("fo0"), self.ptok("fo1")]
            self.Tout = To
            for blk in range(4):
                r = blk % 2
                self.rmsnorm_block(scr, blk, C_NFIN, lambda c: ob[r][:, c, :], [To[r]])
                kb.dma('sp', self.yT[s, :, :, blk * 512:(blk + 1) * 512], ob[r][:], reads=[To[r]], owner=To[r])
            kb.full_barrier()
            for e_ in ('sp', 'act', 'dve', 'pool', 'pe'):
                kb.barrier(To, eng=e_)


def _consts():
    c = np.zeros((P, NCONST), np.float32)
    idx = np.arange(P)
    c[:, K_ID:K_ID + 128] = np.eye(P, dtype=np.float32)
    s = idx[:, None]; t = idx[None, :]
    same = (s // 64) == (t // 64)
    c[:, K_CM:K_CM + 128] = (same & (s <= t)).astype(np.float32)
    c[:, K_TR:K_TR + 128] = (same & (s > t)).astype(np.float32)
    c[:, K_TM:K_TM + 128] = ((t // 16) >= (s // 16)).astype(np.float32)
    c[:, K_SW:K_SW + 128] = (((s % 64) == (t % 64)) & ((s // 64) != (t // 64))).astype(np.float32)
    st = np.zeros((P, 8, 240), np.float32)
    for gl in range(8):
        for h in range(16):
            st[gl * 16 + h, gl, 112 + h] = 1.0
    c[:, K_ST:K_ST + 1920] = st.reshape(P, 1920)
    c[:, K_NV:K_NV + 32] = np.array(NVALS, np.float32)[None, :]
    c[0:64, K_SG] = 1.0; c[64:128, K_SG] = -1.0
    c[0:64, K_SG + 1] = -1.0; c[64:128, K_SG + 1] = 1.0
    return c


def prep_shared(inp):
    f = lambda a: np.ascontiguousarray(np.asarray(a, dtype=np.float32))
    sh = {}
    sh["w_in"] = f(inp["w_in"].reshape(NL, DC, P, DIN).transpose(0, 2, 1, 3))
    sh["w_glu"] = f(inp["s5_w_glu"].reshape(NL, 4, P, 512).transpose(0, 2, 1, 3))
    sh["w_out"] = f(inp["w_out"].reshape(NL, DC, P, D).transpose(0, 2, 1, 3))
    sh["w_f1"] = f(inp["w_ffn_in"].reshape(NL, DC, P, 2 * DFF).transpose(0, 2, 1, 3))
    sh["w_f2"] = f(inp["w_ffn_out"].reshape(NL, FC, P, D).transpose(0, 2, 1, 3))
    cols = np.zeros((P, NCOL), np.float32)
    cols[:, C_NMIX:C_NMIX + 32] = inp["norm_mix"].reshape(NL, DC, P).transpose(2, 0, 1).reshape(P, 32)
    cols[:, C_NFFN:C_NFFN + 32] = inp["norm_ffn"].reshape(NL, DC, P).transpose(2, 0, 1).reshape(P, 32)
    cols[:, C_NFIN:C_NFIN + 8] = inp["norm_final"].reshape(DC, P).T
    cols[:, C_BGLU:C_BGLU + 16] = inp["s5_b_glu"].reshape(NL, 4, P).transpose(2, 0, 1).reshape(P, 16)
    cols[:, C_S5N:C_S5N + 16] = inp["s5_out_norm"].reshape(NL, 4, P).transpose(2, 0, 1).reshape(P, 16)
    cols[:, C_GLAN:C_GLAN + 16] = inp["gla_out_norm"].reshape(NL, 4, P).transpose(2, 0, 1).reshape(P, 16)
    sh["cols"] = cols
    wgx = np.zeros((33, NL, 256), np.float32)
    wgx[0:16] = inp["gla_w_gate"].transpose(1, 0, 2)
    wgx[32] = inp["gla_b_gate"]
    sh["wgx"] = wgx
    s5p = np.zeros((P, NL, NS5), np.float32)
    dup = lambda a: np.concatenate([a, a], 0)
    lre = inp["s5_lam_re"].transpose(2, 0, 1); lim = inp["s5_lam_im"].transpose(2, 0, 1)
    s5p[:, :, 0:32] = dup(lre); s5p[:, :, 32:64] = dup(lim)
    s5p[:, :, 64:96] = np.broadcast_to(inp["s5_log_step"][None], (P, NL, G))
    bre = inp["s5_b_re"].transpose(2, 0, 1, 3).reshape(64, NL, 512); bim = inp["s5_b_im"].transpose(2, 0, 1, 3).reshape(64, NL, 512)
    s5p[:, :, 96:608] = np.concatenate([bre, bim], 0); s5p[:, :, 608:1120] = np.concatenate([bim, bre], 0)
    cre = inp["s5_c_re"].transpose(3, 0, 1, 2).reshape(64, NL, 512); cim = inp["s5_c_im"].transpose(3, 0, 1, 2).reshape(64, NL, 512)
    s5p[:, :, 1120:1632] = dup(cre); s5p[:, :, 1632:2144] = dup(cim)
    sh["s5p"] = s5p
    dd = inp["s5_d"].reshape(NL, G, 16).transpose(2, 0, 1)
    sh["dcol"] = f(np.tile(dd, (8, 1, 1)))
    sh["consts"] = _consts()
    return sh


_PROG = {}


def kernel(**inputs):
    x = np.asarray(inputs["x"], dtype=np.float32)
    B = x.shape[0]; ncores = 8; nseq = B // ncores
    sh = prep_shared(inputs)
    if "full" not in _PROG:
        _PROG["full"] = MK(nseq=nseq)
    mk = _PROG["full"]
    in_maps = []
    for c in range(ncores):
        xs = x[c * nseq:(c + 1) * nseq]
        xT = np.ascontiguousarray(xs.reshape(nseq, L, DC, P).transpose(0, 3, 2, 1))
        m = dict(sh); m["xT"] = xT
        in_maps.append(m)
    res = run_bass_kernel_spmd(mk.nc, in_maps, core_ids=list(range(ncores)))
    out = np.empty((B, L, D), np.float32)
    for c in range(ncores):
        yT = res.results[c]["yT"]
        out[c * nseq:(c + 1) * nseq] = yT.transpose(0, 3, 2, 1).reshape(nseq, L, D)
    return out
```

```python
import contextlib, itertools, math
import numpy as np
import concourse.bass as bass
import concourse.mybir as mybir
from concourse.bass_utils import run_bass_kernel_spmd

F32 = mybir.dt.float32; BF16 = mybir.dt.bfloat16; I32 = mybir.dt.int32
AF = mybir.ActivationFunctionType; ALU = mybir.AluOpType
P = 128; L = 2048; D = 1024; DC = 8; TT = 1024; DIN = 2064; DFF = 2816; FC = 22
NL = 4; G = 32; EPS = 1e-6
TWO_PI = 2.0 * math.pi
C_NMIX = 0; C_NFFN = 32; C_NFIN = 64; C_BGLU = 72; C_S5N = 88; C_GLAN = 104; NCOL = 120
K_ID = 0; K_CM = 128; K_TR = 256; K_TM = 384; K_SW = 512; K_ST = 640; K_NV = 640 + 1920; K_SG = K_NV + 32; NCONST = K_SG + 2
NS5 = 96 + 4 * 512
NVALS = [7, 6, 5, 4, 3, 2, 1, 0, -1, -2, -3, -4, -5, -6, -7, -8, 1, 2, 3, 4, 5, 6, 7, 8, 8, 16, 32, 64, 128, 256, 512, 1024]


class Tok:
    __slots__ = ('name', 'w', 'rd', 'sem', 'semv')

    def __init__(self, name):
        self.name = name; self.w = None; self.rd = {}; self.sem = None; self.semv = 0


class KB:
    def __init__(self, nc, es):
        self.nc = nc; self.es = es
        self.h = {'pe': nc.tensor, 'act': nc.scalar, 'dve': nc.vector, 'pool': nc.gpsimd, 'sp': nc.sync}
        self.sem = {}; self.cnt = {}; self.seen = {}
        self.pesems = set(); self.epoch = 0
        for e in self.h:
            self.seen[e] = {}
        self._new_sems()
        self.nwait = 0; self.nins = 0
        self.dsems = []; self.dlast = {}

    def _new_sems(self):
        for e in self.h:
            self.sem[e] = self.es.enter_context(self.nc.semaphore('s%d_%s' % (self.epoch, e))); self.cnt[e] = 0
        self.pesems.add(self.sem['pe'])
        self.epoch += 1

    def new_epoch(self):
        self.full_barrier()
        self._new_sems()

    def _deps(self, reads, writes):
        need = {}
        for b in reads:
            d = b.w
            if d is not None and need.get(d[0], 0) < d[1]: need[d[0]] = d[1]
        for b in writes:
            d = b.w
            if d is not None and need.get(d[0], 0) < d[1]: need[d[0]] = d[1]
            for k, v in b.rd.items():
                if need.get(k, 0) < v: need[k] = v
        return need

    def _wait(self, eng, need):
        seen = self.seen[eng]
        for k, v in need.items():
            if eng == 'pe' and k in self.pesems: continue
            if seen.get(k, 0) >= v: continue
            self.h[eng].wait_ge(k, v); seen[k] = v; self.nwait += 1

    def op(self, eng, fn, reads=(), writes=()):
        self._wait(eng, self._deps(reads, writes))
        ins = fn(self.h[eng])
        self.cnt[eng] += 1; c = self.cnt[eng]; sm = self.sem[eng]
        ins.then_inc(sm, 1); self.nins += 1
        for b in reads: b.rd[sm] = c
        for b in writes:
            b.w = (sm, c); b.rd = {}
        return ins

    def dma(self, eng, out, in_, reads=(), writes=(), owner=None):
        self._wait(eng, self._deps(reads, writes))
        own = owner or (writes[0] if writes else reads[0])
        if own.sem is None:
            own.sem = self.es.enter_context(self.nc.semaphore('d%d' % len(self.dsems))); own.semv = 0
            self.dsems.append(own.sem)
        ins = self.h[eng].dma_start(out=out, in_=in_)
        own.semv += 16
        ins.then_inc(own.sem, 16); self.nins += 1
        self.dlast[own.sem] = own.semv
        for b in reads: b.rd[own.sem] = own.semv
        for b in writes:
            b.w = (own.sem, own.semv); b.rd = {}
        return ins

    def barrier(self, toks, eng='sp'):
        need = {}
        for b in toks:
            for k, v in ([b.w] if b.w else []) + list(b.rd.items()):
                if need.get(k, 0) < v: need[k] = v
        self._wait(eng, need)

    def full_barrier(self, dmas=True):
        for e in self.h:
            need = {self.sem[k]: self.cnt[k] for k in self.h if self.cnt[k] > 0 and k != e}
            if dmas:
                need.update(self.dlast)
            self._wait(e, need)


class MK:
    def __init__(self, nseq=4, layers=(0, 1, 2, 3), final=True, prologue=True, dbg=None, stop_after=None):
        self.nseq = nseq; self.layers = list(layers); self.final = final; self.dbg = dbg or {}
        self.do_prologue = prologue; self.stop_after = stop_after
        nc = self.nc = bass.Bass("TRN2", target_bir_lowering=False)
        dt = nc.dram_tensor
        self.xT = dt("xT", [nseq, P, DC, L], F32, kind="ExternalInput").ap()
        self.w_in = dt("w_in", [NL, P, DC, DIN], F32, kind="ExternalInput").ap()
        self.w_glu = dt("w_glu", [NL, P, 4, 512], F32, kind="ExternalInput").ap()
        self.w_out = dt("w_out", [NL, DC, P, DC, 128], F32, kind="ExternalInput").ap()
        self.w_f1 = dt("w_f1", [NL, FC, P, DC, 2, 128], F32, kind="ExternalInput").ap()
        self.w_f2 = dt("w_f2", [NL, DC, P, FC, 128], F32, kind="ExternalInput").ap()
        self.cols_d = dt("cols", [P, NCOL], F32, kind="ExternalInput").ap()
        self.wgx_d = dt("wgx", [33, NL, 256], F32, kind="ExternalInput").ap()
        self.s5p_d = dt("s5p", [P, NL, NS5], F32, kind="ExternalInput").ap()
        self.dcol_d = dt("dcol", [P, NL, G], F32, kind="ExternalInput").ap()
        self.consts_d = dt("consts", [P, NCONST], F32, kind="ExternalInput").ap()
        self.yT = dt("yT", [nseq, P, DC, L], F32, kind="ExternalOutput").ap()
        if prologue:
            self.s5m = dt("s5m", [NL, 11, P, 4096], BF16, kind="Internal").ap()
        else:
            self.s5m = dt("s5m", [NL, 11, P, 4096], BF16, kind="ExternalInput").ap()
        self.dbg_out = {}
        for k, shp in self.dbg.items():
            self.dbg_out[k] = dt("dbg_" + k, list(shp), F32, kind="ExternalOutput").ap()
        self.build()

    def sb(self, es, name, shape, dtype):
        self.uid = getattr(self, 'uid', 0) + 1
        return es.enter_context(self.nc.sbuf_tensor("%s_%d" % (name, self.uid), list(shape), dtype))

    def ptok(self, name):
        d = self.__dict__.setdefault('_ptoks', {})
        if name not in d: d[name] = Tok(name)
        return d[name]

    def wing_slot(self, es):
        return self.cur_slot

    def load_wing(self, l):
        TW = self.ptok("WinG")
        for c in range(DC):
            self.kb.dma('pool', self.cur_slot[:, c, :], self.w_in[l, :, c, 512:2064], writes=[TW])
        self.wing_valid = l

    def psum_next(self):
        i = self.ps_i; self.ps_i = (i + 1) % 8
        return self.ps[i], self.Tps[i]

    def evac_eng(self):
        return next(self.ev)

    def build(self):
        nc = self.nc
        with contextlib.ExitStack() as es:
            kb = self.kb = KB(nc, es)
            self.ev = itertools.cycle(['act', 'dve'])
            self.ps = [es.enter_context(nc.psum_tensor("ps%d" % i, [P, 512], F32)) for i in range(8)]
            self.Tps = [Tok("ps%d" % i) for i in range(8)]
            self.ps_i = 0
            self.ident_bf = self.sb(es, "ident_bf", [P, P], BF16)
            self.ones_bf = self.sb(es, "ones_bf", [P, P], BF16)
            self.strips = self.sb(es, "strips", [P, 8, 240], BF16)
            self.cmask = self.sb(es, "cmask", [P, P], F32)
            self.triR = self.sb(es, "triR", [P, P], F32)
            self.cols = self.sb(es, "cols", [P, NCOL], F32)
            self.wgx = self.sb(es, "wgx", [33, NL, 256], BF16)
            self.epsc = self.sb(es, "epsc", [P, 2], F32)
            self.Tconst = Tok("const")
            T = self.Tconst
            with contextlib.ExitStack() as es2:
                cst = self.sb(es2, "cst", [P, NCONST], F32)
                Tc = Tok("cst")
                kb.dma('sp', cst[:], self.consts_d, writes=[Tc])
                kb.dma('sp', self.cols[:], self.cols_d, writes=[T])
                self.Twgx = Tok('wgx'); kb.dma('pool', self.wgx[:], self.wgx_d, writes=[self.Twgx])
                kb.op('dve', lambda e: e.tensor_copy(self.ident_bf[:], cst[:, K_ID:K_ID + 128]), reads=[Tc], writes=[T])
                kb.op('dve', lambda e: e.memset(self.ones_bf[:], 1.0), writes=[T])
                kb.op('dve', lambda e: e.memset(self.epsc[:, 0:1], EPS), writes=[T])
                kb.op('dve', lambda e: e.memset(self.epsc[:, 1:2], 1.0), writes=[T])
                kb.op('dve', lambda e: e.tensor_copy(self.strips[:].rearrange("p a b -> p (a b)"), cst[:, K_ST:K_ST + 1920]), reads=[Tc], writes=[T])
                kb.op('dve', lambda e: e.tensor_copy(self.cmask[:], cst[:, K_CM:K_CM + 128]), reads=[Tc], writes=[T])
                kb.op('dve', lambda e: e.tensor_copy(self.triR[:], cst[:, K_TR:K_TR + 128]), reads=[Tc], writes=[T])
                if self.do_prologue:
                    self.Ts5m = [[self.ptok("s5m%d" % l)] * 11 for l in range(NL)]
                    for l in self.layers:
                        self.prologue(l, cst, Tc)
                else:
                    self.Ts5m = [[self.ptok("s5m%d" % l)] * 11 for l in range(NL)]
                kb.full_barrier()
                allt = [self.Ts5m[l][0] for l in range(NL)]
                for e in ('sp', 'act', 'pool', 'pe', 'dve'):
                    kb.barrier(allt + [Tc, T, self.Twgx], eng=e)
            if self.stop_after != 'prologue':
                self.main(es)

    def prologue(self, l, cst, Tc):
        nc = self.nc; kb = self.kb
        with contextlib.ExitStack() as es:
            sb = lambda n, s, d=F32: self.sb(es, "pl_" + n, s, d)
            sp = sb("sp", [P, NS5]); dc = sb("dc", [P, G])
            Tsp = self.ptok("pl_sp")
            kb.dma('sp', sp[:], self.s5p_d[:, l, :], writes=[Tsp])
            kb.dma('sp', dc[:], self.dcol_d[:, l, :], writes=[Tsp])
            lr2 = sp[:, 0:32]; li2 = sp[:, 32:64]; ls2 = sp[:, 64:96]
            R1 = sp[:, 96:608]; R2 = sp[:, 608:1120]; C1 = sp[:, 1120:1632]; C2 = sp[:, 1632:2144]
            nv = cst[:, K_NV:K_NV + 32]
            sgA = cst[:, K_SG:K_SG + 1]; sgB = cst[:, K_SG + 1:K_SG + 2]
            sm = sb("sm", [P, 16, 32])
            Tsm = Tok("sm")
            tb = {k: sb("tb_" + k, [P, 32, 32]) for k in ("mag", "tr", "tf", "rc", "Cn", "Sn", "SnA", "SnB")}
            ti = sb("ti", [P, 32, 32], I32)
            Ttb = Tok("tb")
            V = lambda e: e
            def dve(fn, r, w): kb.op('dve', fn, reads=r, writes=w)
            def act(fn, r, w): kb.op('act', fn, reads=r, writes=w)
            step = sm[:, 0, :]; lrs = sm[:, 1, :]; lis = sm[:, 2, :]
            act(lambda e: e.activation(out=step, in_=ls2, func=AF.Exp), [Tsp], [Tsm])
            dve(lambda e: e.tensor_tensor(out=lrs, in0=lr2, in1=step, op=ALU.mult), [Tsp, Tsm], [Tsm])
            dve(lambda e: e.tensor_tensor(out=lis, in0=li2, in1=step, op=ALU.mult), [Tsp, Tsm], [Tsm])
            def bc_g(a):
                return a.unsqueeze(1).to_broadcast([P, 32, 32])
            def bc_n(a):
                return a.unsqueeze(2).to_broadcast([P, 32, 32])
            flat = lambda t: t[:].rearrange("p a b -> p (a b)")
            dve(lambda e: e.tensor_tensor(out=tb["mag"][:], in0=bc_g(lrs), in1=bc_n(nv), op=ALU.mult), [Tsm, Tc], [Ttb])
            act(lambda e: e.activation(out=flat(tb["mag"]), in_=flat(tb["mag"]), func=AF.Exp), [Ttb], [Ttb])
            dve(lambda e: e.scalar_tensor_tensor(out=tb["tr"][:], in0=bc_g(lis), scalar=1.0 / TWO_PI, in1=bc_n(nv), op0=ALU.mult, op1=ALU.mult), [Tsm, Tc], [Ttb])
            for name, ph in (("Cn", 0.25), ("Sn", 0.0)):
                dve(lambda e: e.tensor_scalar(out=flat(ti), in0=flat(tb["tr"]), scalar1=ph, scalar2=None, op0=ALU.add), [Ttb], [Ttb])
                dve(lambda e: e.tensor_copy(flat(tb["tf"]), flat(ti)), [Ttb], [Ttb])
                dve(lambda e: e.scalar_tensor_tensor(out=flat(tb["rc"]), in0=flat(tb["tr"]), scalar=ph, in1=flat(tb["tf"]), op0=ALU.add, op1=ALU.subtract), [Ttb], [Ttb])
                act(lambda e: e.activation(out=flat(tb[name]), in_=flat(tb["rc"]), func=AF.Sin, scale=6.283184), [Ttb], [Ttb])
                dve(lambda e: e.tensor_tensor(out=flat(tb[name]), in0=flat(tb[name]), in1=flat(tb["mag"]), op=ALU.mult), [Ttb], [Ttb])
            dve(lambda e: e.tensor_scalar(out=flat(tb["SnA"]), in0=flat(tb["Sn"]), scalar1=sgA, scalar2=None, op0=ALU.mult), [Ttb, Tc], [Ttb])
            dve(lambda e: e.tensor_scalar(out=flat(tb["SnB"]), in0=flat(tb["Sn"]), scalar1=sgB, scalar2=None, op0=ALU.mult), [Ttb, Tc], [Ttb])
            ar = tb["Cn"][:, 16, :]; ai = tb["Sn"][:, 16, :]
            s_ = lambda i: sm[:, i, :]
            tt = lambda o, a, b, op: dve(lambda e: e.tensor_tensor(out=o, in0=a, in1=b, op=op), [Tsm, Ttb, Tsp], [Tsm])
            dve(lambda e: e.tensor_scalar(out=s_(3), in0=ar, scalar1=-1.0, scalar2=None, op0=ALU.add), [Ttb], [Tsm])
            tt(s_(4), s_(3), lr2, ALU.mult); tt(s_(5), ai, li2, ALU.mult); tt(s_(4), s_(4), s_(5), ALU.add)
            tt(s_(5), ai, lr2, ALU.mult); tt(s_(6), s_(3), li2, ALU.mult); tt(s_(5), s_(5), s_(6), ALU.subtract)
            tt(s_(6), lr2, lr2, ALU.mult); tt(s_(7), li2, li2, ALU.mult); tt(s_(6), s_(6), s_(7), ALU.add)
            dve(lambda e: e.reciprocal(s_(6), s_(6)), [Tsm], [Tsm])
            tt(s_(8), s_(4), s_(6), ALU.mult)
            tt(s_(9), s_(5), s_(6), ALU.mult)
            dve(lambda e: e.tensor_scalar(out=s_(10), in0=s_(9), scalar1=sgB, scalar2=None, op0=ALU.mult), [Tsm, Tc], [Tsm])
            dve(lambda e: e.tensor_scalar(out=s_(11), in0=s_(9), scalar1=sgA, scalar2=None, op0=ALU.mult), [Tsm, Tc], [Tsm])
            X1 = sb("X1", [P, 32, 16]); X2 = sb("X2", [P, 32, 16]); Xt = sb("Xt", [P, 32, 16])
            TX = Tok("X")
            bh = lambda a: a.unsqueeze(2).to_broadcast([P, 32, 16])
            r3 = lambda a: a.rearrange("p (g h) -> p g h", h=16)
            dve(lambda e: e.tensor_tensor(out=X1[:], in0=r3(R1), in1=bh(s_(8)), op=ALU.mult), [Tsp, Tsm], [TX])
            dve(lambda e: e.tensor_tensor(out=Xt[:], in0=r3(R2), in1=bh(s_(10)), op=ALU.mult), [Tsp, Tsm], [TX])
            dve(lambda e: e.tensor_tensor(out=X1[:], in0=X1[:], in1=Xt[:], op=ALU.add), [TX], [TX])
            dve(lambda e: e.tensor_tensor(out=X2[:], in0=r3(R2), in1=bh(s_(8)), op=ALU.mult), [Tsp, Tsm], [TX])
            dve(lambda e: e.tensor_tensor(out=Xt[:], in0=r3(R1), in1=bh(s_(11)), op=ALU.mult), [Tsp, Tsm], [TX])
            dve(lambda e: e.tensor_tensor(out=X2[:], in0=X2[:], in1=Xt[:], op=ALU.add), [TX], [TX])
            T3 = sb("T3", [P, 8, 32]); T4 = sb("T4", [P, 8, 32]); TT34 = Tok("T34")
            dve(lambda e: e.tensor_copy(T3[0:64], tb["Cn"][0:64, 16:24, :]), [Ttb], [TT34])
            dve(lambda e: e.tensor_scalar(out=T3[64:128], in0=tb["Sn"][64:128, 16:24, :], scalar1=-1.0, scalar2=None, op0=ALU.mult), [Ttb], [TT34])
            dve(lambda e: e.tensor_scalar(out=T4[0:64], in0=tb["Sn"][0:64, 16:24, :], scalar1=-1.0, scalar2=None, op0=ALU.mult), [Ttb], [TT34])
            dve(lambda e: e.tensor_scalar(out=T4[64:128], in0=tb["Cn"][64:128, 16:24, :], scalar1=-1.0, scalar2=None, op0=ALU.mult), [Ttb], [TT34])
            big = {k: sb("big_" + k, [P, 32, 8, 16]) for k in ("WBe", "WBn", "WC", "m1", "m2")}
            Tbig = {k: Tok("big" + k) for k in big}
            def tab(t, i0):
                return t[:, i0:i0 + 8, :].rearrange("p s g -> p g s").unsqueeze(3).to_broadcast([P, 32, 8, 16])
            def xb(x):
                return x[:].unsqueeze(2).to_broadcast([P, 32, 8, 16])
            def combo(dst, ta, xa, tb_, xb_, i0, extra_r):
                dve(lambda e: e.tensor_tensor(out=big["m1"][:], in0=tab(ta, i0), in1=xb(xa), op=ALU.mult), extra_r, [Tbig["m1"]])
                dve(lambda e: e.tensor_tensor(out=big["m2"][:], in0=tab(tb_, i0), in1=xb(xb_), op=ALU.mult), extra_r, [Tbig["m2"]])
                dve(lambda e: e.tensor_tensor(out=big[dst][:], in0=big["m1"][:], in1=big["m2"][:], op=ALU.add), [Tbig["m1"], Tbig["m2"]], [Tbig[dst]])
            combo("WBe", tb["Cn"], X1, tb["SnB"], X2, 0, [Ttb, TX])
            combo("WBn", tb["Cn"], X1, tb["SnB"], X2, 8, [Ttb, TX])
            C1v = sb("C1v", [P, 32, 16]); C2v = sb("C2v", [P, 32, 16]); TC = Tok("C")
            dve(lambda e: e.tensor_copy(C1v[:], r3(C1)), [Tsp], [TC])
            dve(lambda e: e.tensor_copy(C2v[:], r3(C2)), [Tsp], [TC])
            combo("WC", T3, C1v, T4, C2v, 0, [TT34, TC])
            idf = cst[:, K_ID:K_ID + 128]; tmask = cst[:, K_TM:K_TM + 128]; swapm = cst[:, K_SW:K_SW + 128]
            outb = [sb("outb%d" % i, [P, 32, 128], BF16) for i in range(2)]
            Toutb = [Tok("outb%d" % i) for i in range(2)]
            tmpT = sb("tmpT", [P, 4, 128]); TtmpT = Tok("tmpT")
            g3 = lambda t, g: t[:, g, :, :].rearrange("p s h -> p (s h)")
            for b in range(8):
                ps, Tp = self.psum_next()
                for gi in range(4):
                    g = 4 * b + gi
                    kb.op('pe', lambda e: e.transpose(ps[:, gi * 128:(gi + 1) * 128], g3(big["WBe"], g), idf), reads=[Tbig["WBe"], Tc], writes=[Tp])
                kb.op('act', lambda e: e.activation(out=outb[0][:, 4 * b:4 * b + 4, :].rearrange("p a b -> p (a b)"), in_=ps[:], func=AF.Copy), reads=[Tp], writes=[Toutb[0]])
            kb.dma('sp', self.s5m[l, 0], outb[0][:].rearrange("p a b -> p (a b)"), reads=[Toutb[0]], writes=[self.Ts5m[l][0]], owner=self.Ts5m[l][0])
            for b in range(8):
                ps, Tp = self.psum_next()
                for gi in range(4):
                    g = 4 * b + gi
                    kb.op('pe', lambda e: e.matmul(ps[:, gi * 128:(gi + 1) * 128], g3(big["WBn"], g), g3(big["WC"], g), start=True, stop=True), reads=[Tbig["WBn"], Tbig["WC"]], writes=[Tp])
                dve(lambda e: e.tensor_tensor(out=tmpT[:], in0=ps[:].rearrange("p (a b) -> p a b", b=128), in1=tmask.unsqueeze(1).to_broadcast([P, 4, 128]), op=ALU.mult), [Tp, Tc], [TtmpT])
                for gi in range(4):
                    g = 4 * b + gi
                    dve(lambda e: e.scalar_tensor_tensor(out=outb[1][:, g, :], in0=idf, scalar=dc[:, g:g + 1], in1=tmpT[:, gi, :], op0=ALU.mult, op1=ALU.add), [TtmpT, Tc, Tsp], [Toutb[1]])
            kb.dma('sp', self.s5m[l, 1], outb[1][:].rearrange("p a b -> p (a b)"), reads=[Toutb[1]], writes=[self.Ts5m[l][1]], owner=self.Ts5m[l][1])
            dve(lambda e: e.tensor_copy(outb[0][:].rearrange("p a b -> p (a b)"), big["WC"][:].rearrange("p g s h -> p (g s h)")), [Tbig["WC"]], [Toutb[0]])
            kb.dma('sp', self.s5m[l, 2], outb[0][:].rearrange("p a b -> p (a b)"), reads=[Toutb[0]], writes=[self.Ts5m[l][2]], owner=self.Ts5m[l][2])
            m1 = big["m1"][:].rearrange("p g s h -> p g (s h)"); m2 = big["m2"][:].rearrange("p g s h -> p g (s h)")
            for k in range(8):
                colC = tb["Cn"][:, 24 + k, :].unsqueeze(2).to_broadcast([P, 32, 128])
                colS = tb["SnA"][:, 24 + k, :].unsqueeze(2).to_broadcast([P, 32, 128])
                ob = outb[(k + 1) % 2]; To = Toutb[(k + 1) % 2]
                dve(lambda e: e.tensor_tensor(out=m1, in0=idf.unsqueeze(1).to_broadcast([P, 32, 128]), in1=colC, op=ALU.mult), [Tc, Ttb], [Tbig["m1"]])
                kb.op('pool', lambda e: e.tensor_tensor(out=m2, in0=swapm.unsqueeze(1).to_broadcast([P, 32, 128]), in1=colS, op=ALU.mult), reads=[Tc, Ttb], writes=[Tbig["m2"]])
                dve(lambda e: e.tensor_tensor(out=ob[:], in0=m1, in1=m2, op=ALU.add), [Tbig["m1"], Tbig["m2"]], [To])
                kb.dma('sp', self.s5m[l, 3 + k], ob[:].rearrange("p a b -> p (a b)"), reads=[To], writes=[self.Ts5m[l][3 + k]], owner=self.Ts5m[l][3 + k])
            kb.full_barrier()
            for e_ in ('sp', 'dve', 'act', 'pool', 'pe'):
                kb.barrier(Toutb + [Tsp], eng=e_)

    def main(self, es):
        nc = self.nc; kb = self.kb
        self.xres = self.sb(es, "xres", [P, DC, L], F32)
        self.Tx = [[Tok("x%d_%d" % (b, c)) for c in range(DC)] for b in range(4)]
        self.hbuf = self.sb(es, "hbuf", [P, DC, TT], BF16)
        self.Th = [Tok("h0"), Tok("h1")]
        self.ycat = self.sb(es, "ycat", [P, DC, TT], BF16)
        self.Tyc = [[Tok("yc%d_%d" % (hb, c)) for c in range(DC)] for hb in range(2)]
        self.cur_slot = self.sb(es, "WinGslot", [P, DC, 1552], BF16)
        self.carry = self.sb(es, "carry", [P, G], F32); self.Tcarry = Tok("carry")
        self.gst_f = self.sb(es, "gst_f", [P, 2, 128], F32)
        self.gst_b = [self.sb(es, "gst_b%d" % i, [P, 2, 128], BF16) for i in range(2)]
        self.Tgf = Tok("gst_f"); self.Tgb = [Tok("gst_b0"), Tok("gst_b1")]
        for s in range(self.nseq):
            kb.new_epoch()
            for b in range(4):
                kb.dma('sp', self.xres[:, :, b * 512:(b + 1) * 512], self.xT[s, :, :, b * 512:(b + 1) * 512], writes=self.Tx[b], owner=self.Tx[b][0])
            self.stopped = (self.stop_after == 'load')
            for li, l in enumerate(self.layers):
                if li + 1 < len(self.layers): self.next_layer = self.layers[li + 1]
                elif s + 1 < self.nseq: self.next_layer = self.layers[0]
                else: self.next_layer = None
                for tile in range(2):
                    if not self.stopped: self.mixer(l, tile)
                for tile in range(2):
                    if not self.stopped: self.ffn(l, tile)
            if 'xres' in self.dbg_out:
                kb.dma('sp', self.dbg_out['xres'], self.xres[:], reads=[t for b in range(4) for t in self.Tx[b]], owner=self.Tx[0][0])
            if self.final:
                self.final_norm(s)
            else:
                for b in range(4):
                    kb.dma('sp', self.yT[s, :, :, b * 512:(b + 1) * 512], self.xres[:, :, b * 512:(b + 1) * 512], reads=self.Tx[b], owner=self.Tx[b][1])
        allt = [t for b in range(4) for t in self.Tx[b]] + getattr(self, 'Tout', [])
        kb.full_barrier()
        for e_ in ('sp', 'act', 'pool'):
            kb.barrier(allt, eng=e_)

    def rmsnorm_block(self, es_scr, blk, col0, dst_fn, Tdst):
        kb = self.kb
        sq, Tsq, rs, Trs = es_scr
        t0 = blk * 512
        Tx = self.Tx[blk]
        kb.op('act', lambda e: e.activation(out=sq[:], in_=self.xres[:, :, t0:t0 + 512], func=AF.Square), reads=Tx, writes=[Tsq])
        ps, Tp = self.psum_next()
        for c in range(DC):
            kb.op('pe', lambda e: e.matmul(ps[:], self.ones_bf[:], sq[:, c, :], start=(c == 0), stop=(c == DC - 1)), reads=[Tsq, self.Tconst], writes=[Tp])
        kb.op('act', lambda e: e.activation(out=rs[:], in_=ps[:], func=AF.Sqrt, scale=1.0 / D, bias=self.epsc[:, 0:1]), reads=[Tp, self.Tconst], writes=[Trs])
        kb.op('dve', lambda e: e.reciprocal(rs[:], rs[:]), reads=[Trs], writes=[Trs])
        for c in range(DC):
            kb.op('dve', lambda e: e.scalar_tensor_tensor(out=dst_fn(c), in0=self.xres[:, c, t0:t0 + 512], scalar=self.cols[:, col0 + c:col0 + c + 1], in1=rs[:], op0=ALU.mult, op1=ALU.mult), reads=[Tx[c], Trs, self.Tconst], writes=Tdst)

    def norm_scratch(self, es):
        sq = self.sb(es, "nsq", [P, DC, 512], BF16); rs = self.sb(es, "nrs", [P, 512], F32)
        return (sq, Tok("nsq"), rs, Tok("nrs"))

    def mixer(self, l, tile):
        nc = self.nc; kb = self.kb
        with contextlib.ExitStack() as es:
            self.wing_slot(es)
            if getattr(self, 'wing_valid', None) != l:
                self.load_wing(l)
            scr = self.norm_scratch(es)
            for hb in range(2):
                self.rmsnorm_block(scr, tile * 2 + hb, C_NMIX + l * 8, lambda c: self.hbuf[:, c, hb * 512:(hb + 1) * 512], [self.Th[hb]])
            kb.full_barrier()
        if self.stop_after == 'norm': self.stopped = True; return
        with contextlib.ExitStack() as esm:
            self.WSs = self.sb(esm, "WSs", [P, 6144], BF16)
            with contextlib.ExitStack() as es:
                self.gla(es, l, tile)
                kb.full_barrier()
            with contextlib.ExitStack() as es:
                self.s5(es, l, tile)
                kb.full_barrier()
        if self.stop_after == 's5': self.stopped = True; return
        with contextlib.ExitStack() as es:
            self.wout(es, l, tile)
            kb.full_barrier()
        if self.stop_after == 'wout': self.stopped = True; return

    def gla(self, es, l, tile):
        nc = self.nc; kb = self.kb
        sb = lambda n, s, d: self.sb(es, "g_" + n, s, d)
        WinG = self.wing_slot(es); TW = self.ptok("WinG")
        if getattr(self, 'wing_valid', None) != l:
            self.load_wing(l)
        WSs = self.WSs
        kb.dma('pool', WSs[:, 0:4096].rearrange("p (c n) -> p c n", c=DC), self.w_in[l, :, :, 0:512], writes=[self.ptok("WinS")])
        kb.dma('pool', WSs[:, 4096:6144].rearrange("p (c n) -> p c n", c=4), self.w_glu[l], writes=[self.ptok("Wglu")])
        glx = sb("glx", [33, TT], BF16); Tglx = Tok("glx")
        Epos = sb("Epos", [P, 2, TT], F32); Eneg = sb("Eneg", [P, 2, TT], F32)
        TEp = [Tok("Ep%d" % i) for i in range(8)]
        qd = sb("qd", [P, 2, TT], BF16); ki = sb("ki", [P, 2, TT], BF16); Tqk = [Tok("qk0"), Tok("qk1")]
        gs = sb("gs", [P, 4, TT], BF16); Tgs = [Tok("gs0"), Tok("gs1")]
        vt = sb("vt", [P, 8, 512], BF16); Tvt = [Tok("vt%d" % i) for i in range(8)]
        ke = sb("ke", [P, 8, 256], BF16); Tke = [Tok("ke%d" % i) for i in range(8)]
        Erc = [sb("Erc%d" % i, [P, 256], F32) for i in range(2)]; TErc = [Tok("Erc0"), Tok("Erc1")]
        e1 = [sb("e1_%d" % i, [P, 256], F32) for i in range(2)]; Te1 = [Tok("e1_0"), Tok("e1_1")]
        nl = [sb("nl_%d" % i, [P, 256], F32) for i in range(2)]; Tnl = [Tok("nl0"), Tok("nl1")]
        sT = [sb("sT%d" % i, [P, 4, 128], BF16) for i in range(2)]; TsT = [Tok("sT0"), Tok("sT1")]
        on = [sb("on%d" % i, [P, 4, 128], BF16) for i in range(2)]; Ton = [Tok("on0"), Tok("on1")]
        junk = sb("junk", [P, 128], BF16); Tjunk = Tok("junk")
        ss = [sb("ss%d" % i, [P, 4], F32) for i in range(2)]; Tss = [Tok("ss0"), Tok("ss1")]
        Tc = self.Tconst
        if tile == 0:
            kb.op('dve', lambda e: e.memset(self.gst_f[:], 0.0), writes=[self.Tgf])
            kb.op('dve', lambda e: e.memset(self.gst_b[0][:], 0.0), writes=[self.Tgb[0]])
        kb.op('dve', lambda e: e.memset(glx[:], 0.0), writes=[Tglx])
        kb.op('dve', lambda e: e.memset(glx[32:33, :], 1.0), writes=[Tglx])
        for hb in range(2):
            ps, Tp = self.psum_next()
            for c in range(DC):
                kb.op('pe', lambda e: e.matmul(ps[0:16, :], WinG[:, c, 1536:1552], self.hbuf[:, c, hb * 512:(hb + 1) * 512], start=(c == 0), stop=(c == DC - 1)), reads=[TW, self.Th[hb]], writes=[Tp])
            kb.op('act', lambda e: e.activation(out=glx[0:16, hb * 512:(hb + 1) * 512], in_=ps[0:16, :], func=AF.Copy), reads=[Tp], writes=[Tglx])
        for st in range(8):
            tk = slice(st * 128, (st + 1) * 128); hb = st // 4; r = st % 2
            ps, Tp = self.psum_next()
            kb.op('pe', lambda e: e.matmul(ps[:, 0:256], glx[0:33, tk], self.wgx[0:33, l, :], start=True, stop=True), reads=[Tglx, self.Twgx], writes=[Tp])
            kb.op('act', lambda e: e.activation(out=e1[r][:], in_=ps[:, 0:256], func=AF.Exp, scale=-1.0), reads=[Tp], writes=[Te1[r]])
            ps, Tp = self.psum_next()
            for c in range(DC):
                kb.op('pe', lambda e: e.matmul(ps[:], self.hbuf[:, c, tk], WinG[:, c, 512:1024], start=(c == 0), stop=(c == DC - 1)), reads=[TW, self.Th[hb]], writes=[Tp])
            kb.op('act', lambda e: e.activation(out=vt[:, st, :], in_=ps[:], func=AF.Copy), reads=[Tp], writes=[Tvt[st]])
            kb.op('act', lambda e: e.activation(out=nl[r][:], in_=e1[r][:], func=AF.Ln, bias=self.epsc[:, 1:2]), reads=[Te1[r], Tc], writes=[Tnl[r]])
            ps, Tp = self.psum_next()
            for c in range(2):
                kb.op('pe', lambda e: e.matmul(ps[:, c * 128:(c + 1) * 128], nl[r][:, c * 128:(c + 1) * 128], self.cmask[:], start=True, stop=True), reads=[Tnl[r], Tc], writes=[Tp])
            kb.op('act', lambda e: e.activation(out=Epos[:, :, tk], in_=ps[:, 0:256].rearrange("p (c t) -> p c t", c=2), func=AF.Exp, scale=-1.0 / 16), reads=[Tp], writes=[TEp[st]])
            kb.op('act', lambda e: e.activation(out=Eneg[:, :, tk], in_=ps[:, 0:256].rearrange("p (c t) -> p c t", c=2), func=AF.Exp, scale=1.0 / 16), reads=[Tp], writes=[TEp[st]])
            ps, Tp = self.psum_next()
            kb.op('pe', lambda e: e.matmul(ps[:, 0:256], self.triR[:], nl[r][:], start=True, stop=True), reads=[Tnl[r], Tc], writes=[Tp])
            kb.op('act', lambda e: e.activation(out=Erc[r][:], in_=ps[:, 0:256], func=AF.Exp, scale=-1.0 / 16), reads=[Tp], writes=[TErc[r]])
            ps, Tp = self.psum_next()
            for c in range(DC):
                kb.op('pe', lambda e: e.matmul(ps[:, 0:256], self.hbuf[:, c, tk], WinG[:, c, 256:512], start=(c == 0), stop=(c == DC - 1)), reads=[TW, self.Th[hb]], writes=[Tp])
            kb.op('dve', lambda e: e.tensor_tensor(out=ke[:, st, :], in0=ps[:, 0:256], in1=Erc[r][:], op=ALU.mult), reads=[Tp, TErc[r]], writes=[Tke[st]])
        for hb in range(2):
            hs = slice(hb * 512, (hb + 1) * 512)
            TE = TEp[hb * 4:(hb + 1) * 4]
            for c in range(2):
                ps, Tp = self.psum_next()
                for kc in range(DC):
                    kb.op('pe', lambda e: e.matmul(ps[:], WinG[:, kc, c * 128:(c + 1) * 128], self.hbuf[:, kc, hs], start=(kc == 0), stop=(kc == DC - 1)), reads=[TW, self.Th[hb]], writes=[Tp])
                kb.op('dve', lambda e: e.scalar_tensor_tensor(out=qd[:, c, hs], in0=ps[:], scalar=0.125, in1=Epos[:, c, hs], op0=ALU.mult, op1=ALU.mult), reads=[Tp] + TE, writes=[Tqk[hb]])
                ps, Tp = self.psum_next()
                for kc in range(DC):
                    kb.op('pe', lambda e: e.matmul(ps[:], WinG[:, kc, 256 + c * 128:256 + (c + 1) * 128], self.hbuf[:, kc, hs], start=(kc == 0), stop=(kc == DC - 1)), reads=[TW, self.Th[hb]], writes=[Tp])
                kb.op('dve', lambda e: e.tensor_tensor(out=ki[:, c, hs], in0=ps[:], in1=Eneg[:, c, hs], op=ALU.mult), reads=[Tp] + TE, writes=[Tqk[hb]])
            for c in range(4):
                ps, Tp = self.psum_next()
                for kc in range(DC):
                    kb.op('pe', lambda e: e.matmul(ps[:], WinG[:, kc, 1024 + c * 128:1024 + (c + 1) * 128], self.hbuf[:, kc, hs], start=(kc == 0), stop=(kc == DC - 1)), reads=[TW, self.Th[hb]], writes=[Tp])
                kb.op('act', lambda e: e.activation(out=gs[:, c, hs], in_=ps[:], func=AF.Silu), reads=[Tp], writes=[Tgs[hb]])
        pending = None
        for st in range(8):
            tk = slice(st * 128, (st + 1) * 128); hb = st // 4; r = st % 2
            psb = [self.psum_next(), self.psum_next()]
            for hd in range(4):
                c = hd // 2; par = hd % 2; pr = slice(par * 64, par * 64 + 64)
                ps, Tp = psb[par]
                kb.op('pe', lambda e: e.matmul(ps[:, c * 128:(c + 1) * 128], ki[pr, c, tk], qd[pr, c, tk], start=True, stop=True), reads=[Tqk[hb]], writes=[Tp])
            for par in range(2):
                ps, Tp = psb[par]
                dst = sT[r][:].rearrange("p (c two) t -> p two c t", two=2)[:, par, :, :]
                kb.op('dve', lambda e: e.tensor_tensor(out=dst, in0=ps[:, 0:256].rearrange("p (a b) -> p a b", b=128), in1=self.cmask[:].unsqueeze(1).to_broadcast([P, 2, 128]), op=ALU.mult), reads=[Tp, Tc], writes=[TsT[r]])
            def upd_state(half, dst):
                rows = slice(half * 64, half * 64 + 64)
                psu, Tpu = self.psum_next()
                for hd in range(4):
                    c = hd // 2; pr = slice((hd % 2) * 64, (hd % 2) * 64 + 64)
                    kb.op('pe', lambda e: e.matmul(psu[pr, c * 128:(c + 1) * 128], ke[rows, st, hd * 64:(hd + 1) * 64], vt[rows, st, hd * 128:(hd + 1) * 128], start=True, stop=True), reads=[Tke[st], Tvt[st]], writes=[Tpu])
                tend = st * 128 + half * 64 + 63
                for c in range(2):
                    kb.op('dve', lambda e: e.scalar_tensor_tensor(out=self.gst_f[:, c, :], in0=self.gst_f[:, c, :], scalar=Epos[:, c, tend:tend + 1], in1=psu[:, c * 128:(c + 1) * 128], op0=ALU.mult, op1=ALU.add), reads=[self.Tgf, TEp[st], Tpu], writes=[self.Tgf])
                kb.op('act', lambda e: e.activation(out=self.gst_b[dst][:].rearrange("p a b -> p (a b)"), in_=self.gst_f[:].rearrange("p a b -> p (a b)"), func=AF.Copy), reads=[self.Tgf], writes=[self.Tgb[dst]])
            upd_state(0, 1)
            pob = [self.psum_next(), self.psum_next()]
            for hd in range(4):
                c = hd // 2; par = hd % 2; pr = slice(par * 64, par * 64 + 64)
                pso, Tpo = pob[par]
                oc = slice(c * 128, (c + 1) * 128)
                kb.op('pe', lambda e: e.matmul(pso[:, oc], sT[r][:, hd, :], vt[:, st, hd * 128:(hd + 1) * 128], start=True, stop=False, skip_group_check=True), reads=[TsT[r], Tvt[st]], writes=[Tpo])
                kb.op('pe', lambda e: e.matmul(pso[0:64, oc], qd[pr, c, st * 128:st * 128 + 64], self.gst_b[0][pr, c, :], start=False, stop=False, skip_group_check=True), reads=[Tqk[hb], self.Tgb[0]], writes=[Tpo])
                kb.op('pe', lambda e: e.matmul(pso[64:128, oc], qd[pr, c, st * 128 + 64:st * 128 + 128], self.gst_b[1][pr, c, :], start=False, stop=True, skip_group_check=True), reads=[Tqk[hb], self.Tgb[1]], writes=[Tpo])
            upd_state(1, 0)
            for hd in range(4):
                c = hd // 2; par = hd % 2; pso, Tpo = pob[par]
                kb.op('act', lambda e: e.activation(out=junk[:], in_=pso[:, c * 128:(c + 1) * 128], func=AF.Square, accum_out=ss[r][:, hd:hd + 1]), reads=[Tpo], writes=[Tjunk, Tss[r]])
            kb.op('act', lambda e: e.activation(out=ss[r][:], in_=ss[r][:], func=AF.Sqrt, scale=1.0 / 128, bias=self.epsc[:, 0:1]), reads=[Tss[r], Tc], writes=[Tss[r]])
            kb.op('dve', lambda e: e.reciprocal(ss[r][:], ss[r][:]), reads=[Tss[r]], writes=[Tss[r]])
            for par in range(2):
                pso, Tpo = pob[par]
                dst = on[r][:].rearrange("p (c two) t -> p two c t", two=2)[:, par, :, :]
                sc = ss[r][:].rearrange("p (c two) -> p two c", two=2)[:, par, :].unsqueeze(2).to_broadcast([P, 2, 128])
                kb.op('dve', lambda e: e.tensor_tensor(out=dst, in0=pso[:, 0:256].rearrange("p (a b) -> p a b", b=128), in1=sc, op=ALU.mult), reads=[Tpo, Tss[r]], writes=[Ton[r]])
            def tail(st=st, tk=tk, hb=hb, r=r):
                pst, Tpt = self.psum_next()
                pstb = pst[:].bitcast(BF16)
                for hd in range(4):
                    kb.op('pe', lambda e: e.transpose(pstb[:, hd * 128:(hd + 1) * 128], on[r][:, hd, :], self.ident_bf[:]), reads=[Ton[r], Tc], writes=[Tpt])
                for hd in range(4):
                    kb.op('dve', lambda e: e.scalar_tensor_tensor(out=self.ycat[:, 4 + hd, tk], in0=pstb[:, hd * 128:(hd + 1) * 128], scalar=self.cols[:, C_GLAN + l * 4 + hd:C_GLAN + l * 4 + hd + 1], in1=gs[:, hd, tk], op0=ALU.mult, op1=ALU.mult), reads=[Tpt, Tc, Tgs[hb]], writes=[self.Tyc[hb][4 + hd]])
            if pending is not None:
                pending()
            pending = tail
        pending()

    def s5(self, es, l, tile):
        nc = self.nc; kb = self.kb
        sb = lambda n, s, d: self.sb(es, "s_" + n, s, d)
        Tc = self.Tconst
        slot = self.cur_slot
        self.wing_valid = None
        flat = slot[:].rearrange("p a b -> p (a b)")
        WSs = self.WSs
        WinS = WSs[:, 0:4096].rearrange("p (c n) -> p c n", c=DC); TW = self.ptok("WinS")
        Wglu = WSs[:, 4096:6144].rearrange("p (c n) -> p c n", c=4); TWg = self.ptok("Wglu")
        mats = [sb("mat%d" % i, [P, G, 128], BF16) for i in range(3)]; Tm = [self.ptok("mat%d" % i) for i in range(3)]
        for i in range(3):
            kb.dma('sp', mats[i][:].rearrange("p a b -> p (a b)"), self.s5m[l, i], reads=[self.Ts5m[l][i]], writes=[Tm[i]], owner=Tm[i])
        WB, Toep, WC = mats
        ring = [sb("ring%d" % i, [P, G, 128], BF16) for i in range(2)]; Tring = [self.ptok("ring0"), self.ptok("ring1")]
        bufA = sb("bufA", [P, 4, TT], BF16); TA = [Tok("bufA%d" % i) for i in range(4)]
        UT = sb("UT", [P, G, 128], BF16); TUT = [Tok("UT%d" % i) for i in range(8)]
        Sf = flat[:, 0:8256].bitcast(F32).rearrange("p (g j) -> p g j", j=129)
        Sb_ = flat[:, 8256:12384].rearrange("p (g j) -> p g j", j=129)
        TSf = [Tok("Sf%d" % i) for i in range(8)]; TSb = [Tok("Sb%d" % i) for i in range(8)]
        yg = ring[0]; Tyg = [Tok("yg%d" % i) for i in range(4)]
        y2 = self.hbuf
        if tile == 0:
            kb.op('dve', lambda e: e.memset(self.carry[:], 0.0), writes=[self.Tcarry])
        for cc in range(4):
            for hb in range(2):
                ps, Tp = self.psum_next()
                for c in range(DC):
                    kb.op('pe', lambda e: e.matmul(ps[:], WinS[:, c, cc * 128:(cc + 1) * 128], self.hbuf[:, c, hb * 512:(hb + 1) * 512], start=(c == 0), stop=(c == DC - 1)), reads=[TW, self.Th[hb]], writes=[Tp])
                eng = self.evac_eng()
                if eng == 'act':
                    kb.op('act', lambda e: e.activation(out=bufA[:, cc, hb * 512:(hb + 1) * 512], in_=ps[:], func=AF.Copy), reads=[Tp], writes=[TA[cc]])
                else:
                    kb.op('dve', lambda e: e.tensor_copy(bufA[:, cc, hb * 512:(hb + 1) * 512], ps[:]), reads=[Tp], writes=[TA[cc]])
        for b in range(8):
            ps, Tp = self.psum_next()
            for gi in range(4):
                g = 4 * b + gi; cc = g // 8; gl = g % 8
                for s_ in range(8):
                    mv = bufA[:, cc, :].rearrange("p (j s) -> p s j", s=8)[:, s_, :]
                    kb.op('pe', lambda e: e.matmul(ps[:, gi * 128:(gi + 1) * 128], self.strips[:, gl, 112 - 16 * s_:240 - 16 * s_], mv, start=(s_ == 0), stop=(s_ == 7)), reads=[TA[cc], Tc], writes=[Tp])
            eng = self.evac_eng()
            dst = UT[:, 4 * b:4 * b + 4, :].rearrange("p a b -> p (a b)")
            if eng == 'act':
                kb.op('act', lambda e: e.activation(out=dst, in_=ps[:], func=AF.Copy), reads=[Tp], writes=[TUT[b]])
            else:
                kb.op('dve', lambda e: e.tensor_copy(dst, ps[:]), reads=[Tp], writes=[TUT[b]])
        for b in range(8):
            ps, Tp = self.psum_next()
            for gi in range(4):
                g = 4 * b + gi
                kb.op('pe', lambda e: e.matmul(ps[:, gi * 128:(gi + 1) * 128], WB[:, g, :], UT[:, g, :], start=True, stop=True), reads=[Tm[0], TUT[b]], writes=[Tp])
            g4 = slice(4 * b, 4 * b + 4)
            kb.op('dve', lambda e: e.tensor_copy(Sf[:, g4, 1:129], ps[:].rearrange("p (a b) -> p a b", b=128)), reads=[Tp], writes=[TSf[b]])
            kb.op('dve', lambda e: e.tensor_copy(Sf[:, g4, 0:1], self.carry[:, g4].unsqueeze(2)), reads=[self.Tcarry], writes=[TSf[b]])
            kb.op('act', lambda e: e.activation(out=Sb_[:, g4, :], in_=Sf[:, g4, :], func=AF.Copy), reads=[TSf[b]], writes=[TSb[b]])
        for k in range(8):
            d = 1 << k; n = 129 - d
            rg = ring[k % 2]; Tr = Tring[k % 2]
            kb.dma('sp', rg[:].rearrange("p a b -> p (a b)"), self.s5m[l, 3 + k], reads=[self.Ts5m[l][3 + k]], writes=[Tr], owner=Tr)
            for b in range(8):
                ps, Tp = self.psum_next()
                g4 = slice(4 * b, 4 * b + 4)
                for gi in range(4):
                    g = 4 * b + gi
                    kb.op('pe', lambda e: e.matmul(ps[:, gi * 128:gi * 128 + n], rg[:, g, :], Sb_[:, g, 0:n], start=True, stop=True), reads=[Tr, TSb[b]], writes=[Tp])
                kb.op('dve', lambda e: e.tensor_tensor(out=Sf[:, g4, d:129], in0=Sf[:, g4, d:129], in1=ps[:].rearrange("p (a b) -> p a b", b=128)[:, :, 0:n], op=ALU.add), reads=[Tp, TSf[b]], writes=[TSf[b]])
                kb.op('act', lambda e: e.activation(out=Sb_[:, g4, d:129], in_=Sf[:, g4, d:129], func=AF.Copy), reads=[TSf[b]], writes=[TSb[b]])
        kb.op('dve', lambda e: e.tensor_copy(self.carry[:].unsqueeze(2), Sf[:, :, 128:129]), reads=TSf, writes=[self.Tcarry])
        for b in range(8):
            ps, Tp = self.psum_next()
            for gi in range(4):
                g = 4 * b + gi
                kb.op('pe', lambda e: e.matmul(ps[:, gi * 128:(gi + 1) * 128], Toep[:, g, :], UT[:, g, :], start=True, stop=False), reads=[Tm[1], TUT[b]], writes=[Tp])
                kb.op('pe', lambda e: e.matmul(ps[:, gi * 128:(gi + 1) * 128], WC[:, g, :], Sb_[:, g, 0:128], start=False, stop=True), reads=[Tm[2], TSb[b]], writes=[Tp])
            kb.op('act', lambda e: e.activation(out=yg[:, 4 * b:4 * b + 4, :].rearrange("p a b -> p (a b)"), in_=ps[:], func=AF.Gelu_apprx_tanh), reads=[Tp], writes=[Tyg[b // 2], Tring[0]])
        for cc in range(4):
            for th in range(2):
                ps, Tp = self.psum_next()
                for ti in range(4):
                    t0 = th * 4 + ti
                    for gl in range(8):
                        kb.op('pe', lambda e: e.matmul(ps[:, ti * 128:(ti + 1) * 128], self.strips[:, t0, 112 - 16 * gl:240 - 16 * gl], yg[:, cc * 8 + gl, :], start=(gl == 0), stop=(gl == 7)), reads=[Tyg[cc], Tc], writes=[Tp])
                dst = bufA[:, cc, :].rearrange("p (j s) -> p s j", s=8)[:, th * 4:th * 4 + 4, :]
                eng = self.evac_eng()
                if eng == 'act':
                    kb.op('act', lambda e: e.activation(out=dst, in_=ps[:].rearrange("p (a b) -> p a b", b=128), func=AF.Copy), reads=[Tp], writes=[TA[cc]])
                else:
                    kb.op('dve', lambda e: e.tensor_copy(dst, ps[:].rearrange("p (a b) -> p a b", b=128)), reads=[Tp], writes=[TA[cc]])
        if 'yg' in self.dbg_out and tile == 0:
            kb.dma('pool', self.dbg_out['yg'], bufA[:], reads=TA, owner=TA[0])
        sq = UT[:, 0:16, :].rearrange("p (a b) c -> p a (b c)", a=4); Tsq = TUT[0:4]
        rs = UT[:, 16:24, :].rearrange("p a b -> p (a b)").bitcast(F32); Trs = TUT[4:6]
        gt = [UT[:, 24:28, :].rearrange("p a b -> p (a b)"), UT[:, 28:32, :].rearrange("p a b -> p (a b)")]; Tgt = [TUT[6], TUT[7]]
        for hb in range(2):
            hs = slice(hb * 512, (hb + 1) * 512)
            for co in range(4):
                ps, Tp = self.psum_next()
                for ci in range(4):
                    kb.op('pe', lambda e: e.matmul(ps[:], Wglu[:, ci, co * 128:(co + 1) * 128], bufA[:, ci, hs], start=(ci == 0), stop=(ci == 3)), reads=[TWg] + TA, writes=[Tp])
                r = co % 2
                kb.op('act', lambda e: e.activation(out=gt[r], in_=ps[:], func=AF.Sigmoid, bias=self.cols[:, C_BGLU + l * 4 + co:C_BGLU + l * 4 + co + 1]), reads=[Tp, Tc], writes=[Tgt[r]])
                kb.op('dve', lambda e: e.tensor_tensor(out=y2[:, co, hs], in0=bufA[:, co, hs], in1=gt[r], op=ALU.mult), reads=[TA[co], Tgt[r]], writes=[self.Th[hb]])
        for hb in range(2):
            hs = slice(hb * 512, (hb + 1) * 512)
            kb.op('act', lambda e: e.activation(out=sq, in_=y2[:, 0:4, hs], func=AF.Square), reads=[self.Th[hb]], writes=Tsq)
            ps, Tp = self.psum_next()
            for c in range(4):
                kb.op('pe', lambda e: e.matmul(ps[:], self.ones_bf[:], sq[:, c, :], start=(c == 0), stop=(c == 3)), reads=Tsq + [Tc], writes=[Tp])
            kb.op('act', lambda e: e.activation(out=rs, in_=ps[:], func=AF.Sqrt, scale=1.0 / 512, bias=self.epsc[:, 0:1]), reads=[Tp, Tc], writes=Trs)
            kb.op('dve', lambda e: e.reciprocal(rs, rs), reads=Trs, writes=Trs)
            for c in range(4):
                kb.op('dve', lambda e: e.scalar_tensor_tensor(out=self.ycat[:, c, hs], in0=y2[:, c, hs], scalar=self.cols[:, C_S5N + l * 4 + c:C_S5N + l * 4 + c + 1], in1=rs, op0=ALU.mult, op1=ALU.mult), reads=[self.Th[hb], Tc] + Trs, writes=[self.Tyc[hb][c]])

    def wout(self, es, l, tile):
        kb = self.kb
        self.wing_slot(es)
        Wout = self.sb(es, "Wout", [P, DC, DC, 128], BF16)
        TWo = [self.ptok("Wout%d" % c) for c in range(DC)]
        for co in range(DC):
            kb.dma('pool', Wout[:, co, :, :], self.w_out[l, co], writes=[TWo[co]])
        if tile == 0:
            self.load_wing(l)
        if 'ycat' in self.dbg_out and tile == 0:
            dtmp = self.sb(es, "dtmp2", [P, DC, TT], F32); Td = Tok("dtmp2")
            kb.op('dve', lambda e: e.tensor_copy(dtmp[:], self.ycat[:]), reads=[t for hb in range(2) for t in self.Tyc[hb]], writes=[Td])
            kb.dma('sp', self.dbg_out['ycat'], dtmp[:], reads=[Td])
            kb.barrier([Td], eng='dve')
        for hb in range(2):
            blk = tile * 2 + hb; t0 = blk * 512
            for co in range(DC):
                ps, Tp = self.psum_next()
                for ci in range(DC):
                    kb.op('pe', lambda e: e.matmul(ps[:], Wout[:, co, ci, :], self.ycat[:, ci, hb * 512:(hb + 1) * 512], start=(ci == 0), stop=(ci == DC - 1)), reads=[TWo[co], self.Tyc[hb][ci]], writes=[Tp])
                kb.op('dve', lambda e: e.tensor_tensor(out=self.xres[:, co, t0:t0 + 512], in0=ps[:], in1=self.xres[:, co, t0:t0 + 512], op=ALU.add), reads=[Tp, self.Tx[blk][co]], writes=[self.Tx[blk][co]])

    def ffn(self, l, tile):
        kb = self.kb
        with contextlib.ExitStack() as es:
            self.wing_slot(es)
            scr = self.norm_scratch(es)
            for hb in range(2):
                self.rmsnorm_block(scr, tile * 2 + hb, C_NFFN + l * 8, lambda c: self.hbuf[:, c, hb * 512:(hb + 1) * 512], [self.Th[hb]])
            kb.full_barrier()
        with contextlib.ExitStack() as es:
            sb = lambda n, s, d: self.sb(es, "f_" + n, s, d)
            self.wing_slot(es)
            if tile == 1 and self.next_layer is not None:
                self.load_wing(self.next_layer)
            act = sb("act", [P, FC, TT], BF16); Tact = [[Tok("act%d_%d" % (f, hb)) for hb in range(2)] for f in range(FC)]
            NR1 = 4; NR2 = 2
            W1 = [sb("W1_%d" % i, [P, DC, 2, 128], BF16) for i in range(NR1)]; TW1 = [self.ptok("W1_%d" % i) for i in range(NR1)]
            W2 = [sb("W2_%d" % i, [P, FC, 128], BF16) for i in range(NR2)]; TW2 = [self.ptok("W2_%d" % i) for i in range(NR2)]
            sg = [sb("sg%d" % i, [P, 512], BF16) for i in range(2)]; Tsg = [Tok("sg0"), Tok("sg1")]
            for f in range(FC):
                w = W1[f % NR1]; Tw = TW1[f % NR1]
                kb.dma('pool', w[:], self.w_f1[l, f], writes=[Tw])
                for hb in range(2):
                    hs = slice(hb * 512, (hb + 1) * 512)
                    psg, Tpg = self.psum_next()
                    for c in range(DC):
                        kb.op('pe', lambda e: e.matmul(psg[:], w[:, c, 0, :], self.hbuf[:, c, hs], start=(c == 0), stop=(c == DC - 1)), reads=[Tw, self.Th[hb]], writes=[Tpg])
                    psu, Tpu = self.psum_next()
                    for c in range(DC):
                        kb.op('pe', lambda e: e.matmul(psu[:], w[:, c, 1, :], self.hbuf[:, c, hs], start=(c == 0), stop=(c == DC - 1)), reads=[Tw, self.Th[hb]], writes=[Tpu])
                    r = (2 * f + hb) % 2
                    kb.op('act', lambda e: e.activation(out=sg[r][:], in_=psg[:], func=AF.Silu), reads=[Tpg], writes=[Tsg[r]])
                    kb.op('dve', lambda e: e.tensor_tensor(out=act[:, f, hs], in0=psu[:], in1=sg[r][:], op=ALU.mult), reads=[Tpu, Tsg[r]], writes=[Tact[f][hb]])
            for co in range(DC):
                w = W2[co % NR2]; Tw = TW2[co % NR2]
                kb.dma('pool', w[:], self.w_f2[l, co], writes=[Tw])
                for hb in range(2):
                    blk = tile * 2 + hb; t0 = blk * 512
                    ps, Tp = self.psum_next()
                    for f in range(FC):
                        kb.op('pe', lambda e: e.matmul(ps[:], w[:, f, :], act[:, f, hb * 512:(hb + 1) * 512], start=(f == 0), stop=(f == FC - 1)), reads=[Tw, Tact[f][hb]], writes=[Tp])
                    kb.op('dve', lambda e: e.tensor_tensor(out=self.xres[:, co, t0:t0 + 512], in0=ps[:], in1=self.xres[:, co, t0:t0 + 512], op=ALU.add), reads=[Tp, self.Tx[blk][co]], writes=[self.Tx[blk][co]])
            kb.full_barrier()
            for e_ in ('pool',):
                kb.barrier(TW1 + TW2, eng=e_)

    def final_norm(self, s):
        kb = self.kb
        with contextlib.ExitStack() as es:
            self.wing_slot(es)
            scr = self.norm_scratch(es)
            ob = [self.sb(es, "fo%d" % i, [P, DC, 512], F32) for i in range(2)]
            To = [self.ptok("fo0"), self.ptok("fo1")]
            self.Tout = To
            for blk in range(4):
                r = blk % 2
                self.rmsnorm_block(scr, blk, C_NFIN, lambda c: ob[r][:, c, :], [To[r]])
                kb.dma('sp', self.yT[s, :, :, blk * 512:(blk + 1) * 512], ob[r][:], reads=[To[r]], owner=To[r])
            kb.full_barrier()
            for e_ in ('sp', 'act', 'dve', 'pool', 'pe'):
                kb.barrier(To, eng=e_)


def _consts():
    c = np.zeros((P, NCONST), np.float32)
    idx = np.arange(P)
    c[:, K_ID:K_ID + 128] = np.eye(P, dtype=np.float32)
    s = idx[:, None]; t = idx[None, :]
    same = (s // 64) == (t // 64)
    c[:, K_CM:K_CM + 128] = (same & (s <= t)).astype(np.float32)
    c[:, K_TR:K_TR + 128] = (same & (s > t)).astype(np.float32)
    c[:, K_TM:K_TM + 128] = ((t // 16) >= (s // 16)).astype(np.float32)
    c[:, K_SW:K_SW + 128] = (((s % 64) == (t % 64)) & ((s // 64) != (t // 64))).astype(np.float32)
    st = np.zeros((P, 8, 240), np.float32)
    for gl in range(8):
        for h in range(16):
            st[gl * 16 + h, gl, 112 + h] = 1.0
    c[:, K_ST:K_ST + 1920] = st.reshape(P, 1920)
    c[:, K_NV:K_NV + 32] = np.array(NVALS, np.float32)[None, :]
    c[0:64, K_SG] = 1.0; c[64:128, K_SG] = -1.0
    c[0:64, K_SG + 1] = -1.0; c[64:128, K_SG + 1] = 1.0
    return c


def prep_shared(inp):
    f = lambda a: np.ascontiguousarray(np.asarray(a, dtype=np.float32))
    sh = {}
    sh["w_in"] = f(inp["w_in"].reshape(NL, DC, P, DIN).transpose(0, 2, 1, 3))
    sh["w_glu"] = f(inp["s5_w_glu"].reshape(NL, 4, P, 512).transpose(0, 2, 1, 3))
    sh["w_out"] = f(inp["w_out"].reshape(NL, DC, P, DC, 128).transpose(0, 3, 2, 1, 4))
    sh["w_f1"] = f(inp["w_ffn_in"].reshape(NL, DC, P, 2, FC, 128).transpose(0, 4, 2, 1, 3, 5))
    sh["w_f2"] = f(inp["w_ffn_out"].reshape(NL, FC, P, DC, 128).transpose(0, 3, 2, 1, 4))
    cols = np.zeros((P, NCOL), np.float32)
    cols[:, C_NMIX:C_NMIX + 32] = inp["norm_mix"].reshape(NL, DC, P).transpose(2, 0, 1).reshape(P, 32)
    cols[:, C_NFFN:C_NFFN + 32] = inp["norm_ffn"].reshape(NL, DC, P).transpose(2, 0, 1).reshape(P, 32)
    cols[:, C_NFIN:C_NFIN + 8] = inp["norm_final"].reshape(DC, P).T
    cols[:, C_BGLU:C_BGLU + 16] = inp["s5_b_glu"].reshape(NL, 4, P).transpose(2, 0, 1).reshape(P, 16)
    cols[:, C_S5N:C_S5N + 16] = inp["s5_out_norm"].reshape(NL, 4, P).transpose(2, 0, 1).reshape(P, 16)
    cols[:, C_GLAN:C_GLAN + 16] = inp["gla_out_norm"].reshape(NL, 4, P).transpose(2, 0, 1).reshape(P, 16)
    sh["cols"] = cols
    wgx = np.zeros((33, NL, 256), np.float32)
    wgx[0:16] = inp["gla_w_gate"].transpose(1, 0, 2)
    wgx[32] = inp["gla_b_gate"]
    sh["wgx"] = wgx
    s5p = np.zeros((P, NL, NS5), np.float32)
    dup = lambda a: np.concatenate([a, a], 0)
    lre = inp["s5_lam_re"].transpose(2, 0, 1); lim = inp["s5_lam_im"].transpose(2, 0, 1)
    s5p[:, :, 0:32] = dup(lre); s5p[:, :, 32:64] = dup(lim)
    s5p[:, :, 64:96] = np.broadcast_to(inp["s5_log_step"][None], (P, NL, G))
    bre = inp["s5_b_re"].transpose(2, 0, 1, 3).reshape(64, NL, 512); bim = inp["s5_b_im"].transpose(2, 0, 1, 3).reshape(64, NL, 512)
    s5p[:, :, 96:608] = np.concatenate([bre, bim], 0); s5p[:, :, 608:1120] = np.concatenate([bim, bre], 0)
    cre = inp["s5_c_re"].transpose(3, 0, 1, 2).reshape(64, NL, 512); cim = inp["s5_c_im"].transpose(3, 0, 1, 2).reshape(64, NL, 512)
    s5p[:, :, 1120:1632] = dup(cre); s5p[:, :, 1632:2144] = dup(cim)
    sh["s5p"] = s5p
    dd = inp["s5_d"].reshape(NL, G, 16).transpose(2, 0, 1)
    sh["dcol"] = f(np.tile(dd, (8, 1, 1)))
    sh["consts"] = _consts()
    return sh


_PROG = {}


def kernel(**inputs):
    x = np.asarray(inputs["x"], dtype=np.float32)
    B = x.shape[0]; ncores = 8; nseq = B // ncores
    sh = prep_shared(inputs)
    if "full" not in _PROG:
        _PROG["full"] = MK(nseq=nseq)
    mk = _PROG["full"]
    in_maps = []
    for c in range(ncores):
        xs = x[c * nseq:(c + 1) * nseq]
        xT = np.ascontiguousarray(xs.reshape(nseq, L, DC, P).transpose(0, 3, 2, 1))
        m = dict(sh); m["xT"] = xT
        in_maps.append(m)
    res = run_bass_kernel_spmd(mk.nc, in_maps, core_ids=list(range(ncores)))
    out = np.empty((B, L, D), np.float32)
    for c in range(ncores):
        yT = res.results[c]["yT"]
        out[c * nseq:(c + 1) * nseq] = yT.transpose(0, 3, 2, 1).reshape(nseq, L, D)
    return out
```

```python
import contextlib, itertools, math
import numpy as np
import concourse.bass as bass
import concourse.mybir as mybir
from concourse.bass_utils import run_bass_kernel_spmd

F32 = mybir.dt.float32; BF16 = mybir.dt.bfloat16; I32 = mybir.dt.int32
AF = mybir.ActivationFunctionType; ALU = mybir.AluOpType
P = 128; L = 2048; D = 1024; DC = 8; TT = 1024; DIN = 2064; DFF = 2816; FC = 22
NL = 4; G = 32; EPS = 1e-6
TWO_PI = 2.0 * math.pi
C_NMIX = 0; C_NFFN = 32; C_NFIN = 64; C_BGLU = 72; C_S5N = 88; C_GLAN = 104; NCOL = 120
K_ID = 0; K_CM = 128; K_TR = 256; K_TM = 384; K_SW = 512; K_ST = 640; K_NV = 640 + 1920; K_SG = K_NV + 32; NCONST = K_SG + 2
NS5 = 96 + 4 * 512
NVALS = [7, 6, 5, 4, 3, 2, 1, 0, -1, -2, -3, -4, -5, -6, -7, -8, 1, 2, 3, 4, 5, 6, 7, 8, 8, 16, 32, 64, 128, 256, 512, 1024]


class Tok:
    __slots__ = ('name', 'w', 'rd', 'sem', 'semv')

    def __init__(self, name):
        self.name = name; self.w = None; self.rd = {}; self.sem = None; self.semv = 0


class KB:
    def __init__(self, nc, es):
        self.nc = nc; self.es = es
        self.h = {'pe': nc.tensor, 'act': nc.scalar, 'dve': nc.vector, 'pool': nc.gpsimd, 'sp': nc.sync}
        self.sem = {}; self.cnt = {}; self.seen = {}
        self.pesems = set(); self.epoch = 0
        for e in self.h:
            self.seen[e] = {}
        self._new_sems()
        self.nwait = 0; self.nins = 0
        self.dsems = []; self.dlast = {}

    def _new_sems(self):
        for e in self.h:
            self.sem[e] = self.es.enter_context(self.nc.semaphore('s%d_%s' % (self.epoch, e))); self.cnt[e] = 0
        self.pesems.add(self.sem['pe'])
        self.epoch += 1

    def new_epoch(self):
        self.full_barrier()
        self._new_sems()

    def _deps(self, reads, writes):
        need = {}
        for b in reads:
            d = b.w
            if d is not None and need.get(d[0], 0) < d[1]: need[d[0]] = d[1]
        for b in writes:
            d = b.w
            if d is not None and need.get(d[0], 0) < d[1]: need[d[0]] = d[1]
            for k, v in b.rd.items():
                if need.get(k, 0) < v: need[k] = v
        return need

    def _wait(self, eng, need):
        seen = self.seen[eng]
        for k, v in need.items():
            if eng == 'pe' and k in self.pesems: continue
            if seen.get(k, 0) >= v: continue
            self.h[eng].wait_ge(k, v); seen[k] = v; self.nwait += 1

    def op(self, eng, fn, reads=(), writes=()):
        self._wait(eng, self._deps(reads, writes))
        ins = fn(self.h[eng])
        self.cnt[eng] += 1; c = self.cnt[eng]; sm = self.sem[eng]
        ins.then_inc(sm, 1); self.nins += 1
        for b in reads: b.rd[sm] = c
        for b in writes:
            b.w = (sm, c); b.rd = {}
        return ins

    def dma(self, eng, out, in_, reads=(), writes=(), owner=None):
        self._wait(eng, self._deps(reads, writes))
        own = owner or (writes[0] if writes else reads[0])
        if own.sem is None:
            own.sem = self.es.enter_context(self.nc.semaphore('d%d' % len(self.dsems))); own.semv = 0
            self.dsems.append(own.sem)
        ins = self.h[eng].dma_start(out=out, in_=in_)
        own.semv += 16
        ins.then_inc(own.sem, 16); self.nins += 1
        self.dlast[own.sem] = own.semv
        for b in reads: b.rd[own.sem] = own.semv
        for b in writes:
            b.w = (own.sem, own.semv); b.rd = {}
        return ins

    def barrier(self, toks, eng='sp'):
        need = {}
        for b in toks:
            for k, v in ([b.w] if b.w else []) + list(b.rd.items()):
                if need.get(k, 0) < v: need[k] = v
        self._wait(eng, need)

    def full_barrier(self, dmas=True):
        for e in self.h:
            need = {self.sem[k]: self.cnt[k] for k in self.h if self.cnt[k] > 0 and k != e}
            if dmas:
                need.update(self.dlast)
            self._wait(e, need)


class MK:
    def __init__(self, nseq=4, layers=(0, 1, 2, 3), final=True, prologue=True, dbg=None, stop_after=None):
        self.nseq = nseq; self.layers = list(layers); self.final = final; self.dbg = dbg or {}
        self.do_prologue = prologue; self.stop_after = stop_after
        nc = self.nc = bass.Bass("TRN2", target_bir_lowering=False)
        dt = nc.dram_tensor
        self.xT = dt("xT", [nseq, P, DC, L], F32, kind="ExternalInput").ap()
        self.w_in = dt("w_in", [NL, P, DC, DIN], F32, kind="ExternalInput").ap()
        self.w_glu = dt("w_glu", [NL, P, 4, 512], F32, kind="ExternalInput").ap()
        self.w_out = dt("w_out", [NL, DC, P, DC, 128], F32, kind="ExternalInput").ap()
        self.w_f1 = dt("w_f1", [NL, FC, P, DC, 2, 128], F32, kind="ExternalInput").ap()
        self.w_f2 = dt("w_f2", [NL, DC, P, FC, 128], F32, kind="ExternalInput").ap()
        self.cols_d = dt("cols", [P, NCOL], F32, kind="ExternalInput").ap()
        self.wgx_d = dt("wgx", [33, NL, 256], F32, kind="ExternalInput").ap()
        self.s5p_d = dt("s5p", [P, NL, NS5], F32, kind="ExternalInput").ap()
        self.dcol_d = dt("dcol", [P, NL, G], F32, kind="ExternalInput").ap()
        self.consts_d = dt("consts", [P, NCONST], F32, kind="ExternalInput").ap()
        self.yT = dt("yT", [nseq, P, DC, L], F32, kind="ExternalOutput").ap()
        if prologue:
            self.s5m = dt("s5m", [NL, 11, P, 4096], BF16, kind="Internal").ap()
        else:
            self.s5m = dt("s5m", [NL, 11, P, 4096], BF16, kind="ExternalInput").ap()
        self.dbg_out = {}
        for k, shp in self.dbg.items():
            self.dbg_out[k] = dt("dbg_" + k, list(shp), F32, kind="ExternalOutput").ap()
        self.build()

    def sb(self, es, name, shape, dtype):
        self.uid = getattr(self, 'uid', 0) + 1
        return es.enter_context(self.nc.sbuf_tensor("%s_%d" % (name, self.uid), list(shape), dtype))

    def ptok(self, name):
        d = self.__dict__.setdefault('_ptoks', {})
        if name not in d: d[name] = Tok(name)
        return d[name]

    def wing_slot(self, es):
        return self.cur_slot

    def load_wing(self, l):
        TW = self.ptok("WinG")
        for c in range(DC):
            self.kb.dma('pool', self.cur_slot[:, c, :], self.w_in[l, :, c, 512:2064], writes=[TW])
        self.wing_valid = l

    def psum_next(self):
        i = self.ps_i; self.ps_i = (i + 1) % 8
        return self.ps[i], self.Tps[i]

    def evac_eng(self):
        return next(self.ev)

    def build(self):
        nc = self.nc
        with contextlib.ExitStack() as es:
            kb = self.kb = KB(nc, es)
            self.ev = itertools.cycle(['act', 'dve'])
            self.ps = [es.enter_context(nc.psum_tensor("ps%d" % i, [P, 512], F32)) for i in range(8)]
            self.Tps = [Tok("ps%d" % i) for i in range(8)]
            self.ps_i = 0
            self.ident_bf = self.sb(es, "ident_bf", [P, P], BF16)
            self.ones_bf = self.sb(es, "ones_bf", [P, P], BF16)
            self.strips = self.sb(es, "strips", [P, 8, 240], BF16)
            self.cmask = self.sb(es, "cmask", [P, P], F32)
            self.triR = self.sb(es, "triR", [P, P], F32)
            self.cols = self.sb(es, "cols", [P, NCOL], F32)
            self.wgx = self.sb(es, "wgx", [33, NL, 256], BF16)
            self.epsc = self.sb(es, "epsc", [P, 2], F32)
            self.Tconst = Tok("const")
            T = self.Tconst
            with contextlib.ExitStack() as es2:
                cst = self.sb(es2, "cst", [P, NCONST], F32)
                Tc = Tok("cst")
                kb.dma('sp', cst[:], self.consts_d, writes=[Tc])
                kb.dma('sp', self.cols[:], self.cols_d, writes=[T])
                self.Twgx = Tok('wgx'); kb.dma('pool', self.wgx[:], self.wgx_d, writes=[self.Twgx])
                kb.op('dve', lambda e: e.tensor_copy(self.ident_bf[:], cst[:, K_ID:K_ID + 128]), reads=[Tc], writes=[T])
                kb.op('dve', lambda e: e.memset(self.ones_bf[:], 1.0), writes=[T])
                kb.op('dve', lambda e: e.memset(self.epsc[:, 0:1], EPS), writes=[T])
                kb.op('dve', lambda e: e.memset(self.epsc[:, 1:2], 1.0), writes=[T])
                kb.op('dve', lambda e: e.tensor_copy(self.strips[:].rearrange("p a b -> p (a b)"), cst[:, K_ST:K_ST + 1920]), reads=[Tc], writes=[T])
                kb.op('dve', lambda e: e.tensor_copy(self.cmask[:], cst[:, K_CM:K_CM + 128]), reads=[Tc], writes=[T])
                kb.op('dve', lambda e: e.tensor_copy(self.triR[:], cst[:, K_TR:K_TR + 128]), reads=[Tc], writes=[T])
                if self.do_prologue:
                    self.Ts5m = [[self.ptok("s5m%d" % l)] * 11 for l in range(NL)]
                    for l in self.layers:
                        self.prologue(l, cst, Tc)
                else:
                    self.Ts5m = [[self.ptok("s5m%d" % l)] * 11 for l in range(NL)]
                kb.full_barrier()
                allt = [self.Ts5m[l][0] for l in range(NL)]
                for e in ('sp', 'act', 'pool', 'pe', 'dve'):
                    kb.barrier(allt + [Tc, T, self.Twgx], eng=e)
            if self.stop_after != 'prologue':
                self.main(es)

    def prologue(self, l, cst, Tc):
        nc = self.nc; kb = self.kb
        with contextlib.ExitStack() as es:
            sb = lambda n, s, d=F32: self.sb(es, "pl_" + n, s, d)
            sp = sb("sp", [P, NS5]); dc = sb("dc", [P, G])
            Tsp = self.ptok("pl_sp")
            kb.dma('sp', sp[:], self.s5p_d[:, l, :], writes=[Tsp])
            kb.dma('sp', dc[:], self.dcol_d[:, l, :], writes=[Tsp])
            lr2 = sp[:, 0:32]; li2 = sp[:, 32:64]; ls2 = sp[:, 64:96]
            R1 = sp[:, 96:608]; R2 = sp[:, 608:1120]; C1 = sp[:, 1120:1632]; C2 = sp[:, 1632:2144]
            nv = cst[:, K_NV:K_NV + 32]
            sgA = cst[:, K_SG:K_SG + 1]; sgB = cst[:, K_SG + 1:K_SG + 2]
            sm = sb("sm", [P, 16, 32])
            Tsm = Tok("sm")
            tb = {k: sb("tb_" + k, [P, 32, 32]) for k in ("mag", "tr", "tf", "rc", "Cn", "Sn", "SnA", "SnB")}
            ti = sb("ti", [P, 32, 32], I32)
            Ttb = Tok("tb")
            V = lambda e: e
            def dve(fn, r, w): kb.op('dve', fn, reads=r, writes=w)
            def act(fn, r, w): kb.op('act', fn, reads=r, writes=w)
            step = sm[:, 0, :]; lrs = sm[:, 1, :]; lis = sm[:, 2, :]
            act(lambda e: e.activation(out=step, in_=ls2, func=AF.Exp), [Tsp], [Tsm])
            dve(lambda e: e.tensor_tensor(out=lrs, in0=lr2, in1=step, op=ALU.mult), [Tsp, Tsm], [Tsm])
            dve(lambda e: e.tensor_tensor(out=lis, in0=li2, in1=step, op=ALU.mult), [Tsp, Tsm], [Tsm])
            def bc_g(a):
                return a.unsqueeze(1).to_broadcast([P, 32, 32])
            def bc_n(a):
                return a.unsqueeze(2).to_broadcast([P, 32, 32])
            flat = lambda t: t[:].rearrange("p a b -> p (a b)")
            dve(lambda e: e.tensor_tensor(out=tb["mag"][:], in0=bc_g(lrs), in1=bc_n(nv), op=ALU.mult), [Tsm, Tc], [Ttb])
            act(lambda e: e.activation(out=flat(tb["mag"]), in_=flat(tb["mag"]), func=AF.Exp), [Ttb], [Ttb])
            dve(lambda e: e.scalar_tensor_tensor(out=tb["tr"][:], in0=bc_g(lis), scalar=1.0 / TWO_PI, in1=bc_n(nv), op0=ALU.mult, op1=ALU.mult), [Tsm, Tc], [Ttb])
            for name, ph in (("Cn", 0.25), ("Sn", 0.0)):
                dve(lambda e: e.tensor_scalar(out=flat(ti), in0=flat(tb["tr"]), scalar1=ph, scalar2=None, op0=ALU.add), [Ttb], [Ttb])
                dve(lambda e: e.tensor_copy(flat(tb["tf"]), flat(ti)), [Ttb], [Ttb])
                dve(lambda e: e.scalar_tensor_tensor(out=flat(tb["rc"]), in0=flat(tb["tr"]), scalar=ph, in1=flat(tb["tf"]), op0=ALU.add, op1=ALU.subtract), [Ttb], [Ttb])
                act(lambda e: e.activation(out=flat(tb[name]), in_=flat(tb["rc"]), func=AF.Sin, scale=6.283184), [Ttb], [Ttb])
                dve(lambda e: e.tensor_tensor(out=flat(tb[name]), in0=flat(tb[name]), in1=flat(tb["mag"]), op=ALU.mult), [Ttb], [Ttb])
            dve(lambda e: e.tensor_scalar(out=flat(tb["SnA"]), in0=flat(tb["Sn"]), scalar1=sgA, scalar2=None, op0=ALU.mult), [Ttb, Tc], [Ttb])
            dve(lambda e: e.tensor_scalar(out=flat(tb["SnB"]), in0=flat(tb["Sn"]), scalar1=sgB, scalar2=None, op0=ALU.mult), [Ttb, Tc], [Ttb])
            ar = tb["Cn"][:, 16, :]; ai = tb["Sn"][:, 16, :]
            s_ = lambda i: sm[:, i, :]
            tt = lambda o, a, b, op: dve(lambda e: e.tensor_tensor(out=o, in0=a, in1=b, op=op), [Tsm, Ttb, Tsp], [Tsm])
            dve(lambda e: e.tensor_scalar(out=s_(3), in0=ar, scalar1=-1.0, scalar2=None, op0=ALU.add), [Ttb], [Tsm])
            tt(s_(4), s_(3), lr2, ALU.mult); tt(s_(5), ai, li2, ALU.mult); tt(s_(4), s_(4), s_(5), ALU.add)
            tt(s_(5), ai, lr2, ALU.mult); tt(s_(6), s_(3), li2, ALU.mult); tt(s_(5), s_(5), s_(6), ALU.subtract)
            tt(s_(6), lr2, lr2, ALU.mult); tt(s_(7), li2, li2, ALU.mult); tt(s_(6), s_(6), s_(7), ALU.add)
            dve(lambda e: e.reciprocal(s_(6), s_(6)), [Tsm], [Tsm])
            tt(s_(8), s_(4), s_(6), ALU.mult)
            tt(s_(9), s_(5), s_(6), ALU.mult)
            dve(lambda e: e.tensor_scalar(out=s_(10), in0=s_(9), scalar1=sgB, scalar2=None, op0=ALU.mult), [Tsm, Tc], [Tsm])
            dve(lambda e: e.tensor_scalar(out=s_(11), in0=s_(9), scalar1=sgA, scalar2=None, op0=ALU.mult), [Tsm, Tc], [Tsm])
            X1 = sb("X1", [P, 32, 16]); X2 = sb("X2", [P, 32, 16]); Xt = sb("Xt", [P, 32, 16])
            TX = Tok("X")
            bh = lambda a: a.unsqueeze(2).to_broadcast([P, 32, 16])
            r3 = lambda a: a.rearrange("p (g h) -> p g h", h=16)
            dve(lambda e: e.tensor_tensor(out=X1[:], in0=r3(R1), in1=bh(s_(8)), op=ALU.mult), [Tsp, Tsm], [TX])
            dve(lambda e: e.tensor_tensor(out=Xt[:], in0=r3(R2), in1=bh(s_(10)), op=ALU.mult), [Tsp, Tsm], [TX])
            dve(lambda e: e.tensor_tensor(out=X1[:], in0=X1[:], in1=Xt[:], op=ALU.add), [TX], [TX])
            dve(lambda e: e.tensor_tensor(out=X2[:], in0=r3(R2), in1=bh(s_(8)), op=ALU.mult), [Tsp, Tsm], [TX])
            dve(lambda e: e.tensor_tensor(out=Xt[:], in0=r3(R1), in1=bh(s_(11)), op=ALU.mult), [Tsp, Tsm], [TX])
            dve(lambda e: e.tensor_tensor(out=X2[:], in0=X2[:], in1=Xt[:], op=ALU.add), [TX], [TX])
            T3 = sb("T3", [P, 8, 32]); T4 = sb("T4", [P, 8, 32]); TT34 = Tok("T34")
            dve(lambda e: e.tensor_copy(T3[0:64], tb["Cn"][0:64, 16:24, :]), [Ttb], [TT34])
            dve(lambda e: e.tensor_scalar(out=T3[64:128], in0=tb["Sn"][64:128, 16:24, :], scalar1=-1.0, scalar2=None, op0=ALU.mult), [Ttb], [TT34])
            dve(lambda e: e.tensor_scalar(out=T4[0:64], in0=tb["Sn"][0:64, 16:24, :], scalar1=-1.0, scalar2=None, op0=ALU.mult), [Ttb], [TT34])
            dve(lambda e: e.tensor_scalar(out=T4[64:128], in0=tb["Cn"][64:128, 16:24, :], scalar1=-1.0, scalar2=None, op0=ALU.mult), [Ttb], [TT34])
            big = {k: sb("big_" + k, [P, 32, 8, 16]) for k in ("WBe", "WBn", "WC", "m1", "m2")}
            Tbig = {k: Tok("big" + k) for k in big}
            def tab(t, i0):
                return t[:, i0:i0 + 8, :].rearrange("p s g -> p g s").unsqueeze(3).to_broadcast([P, 32, 8, 16])
            def xb(x):
                return x[:].unsqueeze(2).to_broadcast([P, 32, 8, 16])
            def combo(dst, ta, xa, tb_, xb_, i0, extra_r):
                dve(lambda e: e.tensor_tensor(out=big["m1"][:], in0=tab(ta, i0), in1=xb(xa), op=ALU.mult), extra_r, [Tbig["m1"]])
                dve(lambda e: e.tensor_tensor(out=big["m2"][:], in0=tab(tb_, i0), in1=xb(xb_), op=ALU.mult), extra_r, [Tbig["m2"]])
                dve(lambda e: e.tensor_tensor(out=big[dst][:], in0=big["m1"][:], in1=big["m2"][:], op=ALU.add), [Tbig["m1"], Tbig["m2"]], [Tbig[dst]])
            combo("WBe", tb["Cn"], X1, tb["SnB"], X2, 0, [Ttb, TX])
            combo("WBn", tb["Cn"], X1, tb["SnB"], X2, 8, [Ttb, TX])
            C1v = sb("C1v", [P, 32, 16]); C2v = sb("C2v", [P, 32, 16]); TC = Tok("C")
            dve(lambda e: e.tensor_copy(C1v[:], r3(C1)), [Tsp], [TC])
            dve(lambda e: e.tensor_copy(C2v[:], r3(C2)), [Tsp], [TC])
            combo("WC", T3, C1v, T4, C2v, 0, [TT34, TC])
            idf = cst[:, K_ID:K_ID + 128]; tmask = cst[:, K_TM:K_TM + 128]; swapm = cst[:, K_SW:K_SW + 128]
            outb = [sb("outb%d" % i, [P, 32, 128], BF16) for i in range(2)]
            Toutb = [Tok("outb%d" % i) for i in range(2)]
            tmpT = sb("tmpT", [P, 4, 128]); TtmpT = Tok("tmpT")
            g3 = lambda t, g: t[:, g, :, :].rearrange("p s h -> p (s h)")
            for b in range(8):
                ps, Tp = self.psum_next()
                for gi in range(4):
                    g = 4 * b + gi
                    kb.op('pe', lambda e: e.transpose(ps[:, gi * 128:(gi + 1) * 128], g3(big["WBe"], g), idf), reads=[Tbig["WBe"], Tc], writes=[Tp])
                kb.op('act', lambda e: e.activation(out=outb[0][:, 4 * b:4 * b + 4, :].rearrange("p a b -> p (a b)"), in_=ps[:], func=AF.Copy), reads=[Tp], writes=[Toutb[0]])
            kb.dma('sp', self.s5m[l, 0], outb[0][:].rearrange("p a b -> p (a b)"), reads=[Toutb[0]], writes=[self.Ts5m[l][0]], owner=self.Ts5m[l][0])
            for b in range(8):
                ps, Tp = self.psum_next()
                for gi in range(4):
                    g = 4 * b + gi
                    kb.op('pe', lambda e: e.matmul(ps[:, gi * 128:(gi + 1) * 128], g3(big["WBn"], g), g3(big["WC"], g), start=True, stop=True), reads=[Tbig["WBn"], Tbig["WC"]], writes=[Tp])
                dve(lambda e: e.tensor_tensor(out=tmpT[:], in0=ps[:].rearrange("p (a b) -> p a b", b=128), in1=tmask.unsqueeze(1).to_broadcast([P, 4, 128]), op=ALU.mult), [Tp, Tc], [TtmpT])
                for gi in range(4):
                    g = 4 * b + gi
                    dve(lambda e: e.scalar_tensor_tensor(out=outb[1][:, g, :], in0=idf, scalar=dc[:, g:g + 1], in1=tmpT[:, gi, :], op0=ALU.mult, op1=ALU.add), [TtmpT, Tc, Tsp], [Toutb[1]])
            kb.dma('sp', self.s5m[l, 1], outb[1][:].rearrange("p a b -> p (a b)"), reads=[Toutb[1]], writes=[self.Ts5m[l][1]], owner=self.Ts5m[l][1])
            dve(lambda e: e.tensor_copy(outb[0][:].rearrange("p a b -> p (a b)"), big["WC"][:].rearrange("p g s h -> p (g s h)")), [Tbig["WC"]], [Toutb[0]])
            kb.dma('sp', self.s5m[l, 2], outb[0][:].rearrange("p a b -> p (a b)"), reads=[Toutb[0]], writes=[self.Ts5m[l][2]], owner=self.Ts5m[l][2])
            m1 = big["m1"][:].rearrange("p g s h -> p g (s h)"); m2 = big["m2"][:].rearrange("p g s h -> p g (s h)")
            for k in range(8):
                colC = tb["Cn"][:, 24 + k, :].unsqueeze(2).to_broadcast([P, 32, 128])
                colS = tb["SnA"][:, 24 + k, :].unsqueeze(2).to_broadcast([P, 32, 128])
                ob = outb[(k + 1) % 2]; To = Toutb[(k + 1) % 2]
                dve(lambda e: e.tensor_tensor(out=m1, in0=idf.unsqueeze(1).to_broadcast([P, 32, 128]), in1=colC, op=ALU.mult), [Tc, Ttb], [Tbig["m1"]])
                kb.op('pool', lambda e: e.tensor_tensor(out=m2, in0=swapm.unsqueeze(1).to_broadcast([P, 32, 128]), in1=colS, op=ALU.mult), reads=[Tc, Ttb], writes=[Tbig["m2"]])
                dve(lambda e: e.tensor_tensor(out=ob[:], in0=m1, in1=m2, op=ALU.add), [Tbig["m1"], Tbig["m2"]], [To])
                kb.dma('sp', self.s5m[l, 3 + k], ob[:].rearrange("p a b -> p (a b)"), reads=[To], writes=[self.Ts5m[l][3 + k]], owner=self.Ts5m[l][3 + k])
            kb.full_barrier()
            for e_ in ('sp', 'dve', 'act', 'pool', 'pe'):
                kb.barrier(Toutb + [Tsp], eng=e_)

    def main(self, es):
        nc = self.nc; kb = self.kb
        self.xres = self.sb(es, "xres", [P, DC, L], F32)
        self.Tx = [[Tok("x%d_%d" % (b, c)) for c in range(DC)] for b in range(4)]
        self.hbuf = self.sb(es, "hbuf", [P, DC, TT], BF16)
        self.Th = [Tok("h0"), Tok("h1")]
        self.ycat = self.sb(es, "ycat", [P, DC, TT], BF16)
        self.Tyc = [[Tok("yc%d_%d" % (hb, c)) for c in range(DC)] for hb in range(2)]
        self.cur_slot = self.sb(es, "WinGslot", [P, DC, 1552], BF16)
        self.carry = self.sb(es, "carry", [P, G], F32); self.Tcarry = Tok("carry")
        self.gst_f = self.sb(es, "gst_f", [P, 2, 128], F32)
        self.gst_b = [self.sb(es, "gst_b%d" % i, [P, 2, 128], BF16) for i in range(2)]
        self.Tgf = Tok("gst_f"); self.Tgb = [Tok("gst_b0"), Tok("gst_b1")]
        for s in range(self.nseq):
            kb.new_epoch()
            for b in range(4):
                kb.dma('sp', self.xres[:, :, b * 512:(b + 1) * 512], self.xT[s, :, :, b * 512:(b + 1) * 512], writes=self.Tx[b], owner=self.Tx[b][0])
            self.stopped = (self.stop_after == 'load')
            for li, l in enumerate(self.layers):
                if li + 1 < len(self.layers): self.next_layer = self.layers[li + 1]
                elif s + 1 < self.nseq: self.next_layer = self.layers[0]
                else: self.next_layer = None
                for tile in range(2):
                    if not self.stopped: self.mixer(l, tile)
                for tile in range(2):
                    if not self.stopped: self.ffn(l, tile)
            if 'xres' in self.dbg_out:
                kb.dma('sp', self.dbg_out['xres'], self.xres[:], reads=[t for b in range(4) for t in self.Tx[b]], owner=self.Tx[0][0])
            if self.final:
                self.final_norm(s)
            else:
                for b in range(4):
                    kb.dma('sp', self.yT[s, :, :, b * 512:(b + 1) * 512], self.xres[:, :, b * 512:(b + 1) * 512], reads=self.Tx[b], owner=self.Tx[b][1])
        allt = [t for b in range(4) for t in self.Tx[b]] + getattr(self, 'Tout', [])
        kb.full_barrier()
        for e_ in ('sp', 'act', 'pool'):
            kb.barrier(allt, eng=e_)

    def rmsnorm_block(self, es_scr, blk, col0, dst_fn, Tdst):
        kb = self.kb
        sq, Tsq, rs, Trs = es_scr
        t0 = blk * 512
        Tx = self.Tx[blk]
        kb.op('act', lambda e: e.activation(out=sq[:], in_=self.xres[:, :, t0:t0 + 512], func=AF.Square), reads=Tx, writes=[Tsq])
        ps, Tp = self.psum_next()
        for c in range(DC):
            kb.op('pe', lambda e: e.matmul(ps[:], self.ones_bf[:], sq[:, c, :], start=(c == 0), stop=(c == DC - 1)), reads=[Tsq, self.Tconst], writes=[Tp])
        kb.op('act', lambda e: e.activation(out=rs[:], in_=ps[:], func=AF.Sqrt, scale=1.0 / D, bias=self.epsc[:, 0:1]), reads=[Tp, self.Tconst], writes=[Trs])
        kb.op('dve', lambda e: e.reciprocal(rs[:], rs[:]), reads=[Trs], writes=[Trs])
        for c in range(DC):
            kb.op('dve', lambda e: e.scalar_tensor_tensor(out=dst_fn(c), in0=self.xres[:, c, t0:t0 + 512], scalar=self.cols[:, col0 + c:col0 + c + 1], in1=rs[:], op0=ALU.mult, op1=ALU.mult), reads=[Tx[c], Trs, self.Tconst], writes=Tdst)

    def norm_scratch(self, es):
        sq = self.sb(es, "nsq", [P, DC, 512], BF16); rs = self.sb(es, "nrs", [P, 512], F32)
        return (sq, Tok("nsq"), rs, Tok("nrs"))

    def mixer(self, l, tile):
        nc = self.nc; kb = self.kb
        with contextlib.ExitStack() as es:
            self.wing_slot(es)
            if getattr(self, 'wing_valid', None) != l:
                self.load_wing(l)
            scr = self.norm_scratch(es)
            for hb in range(2):
                self.rmsnorm_block(scr, tile * 2 + hb, C_NMIX + l * 8, lambda c: self.hbuf[:, c, hb * 512:(hb + 1) * 512], [self.Th[hb]])
            kb.full_barrier()
        if self.stop_after == 'norm': self.stopped = True; return
        with contextlib.ExitStack() as esm:
            self.WSs = self.sb(esm, "WSs", [P, 6144], BF16)
            with contextlib.ExitStack() as es:
                self.gla(es, l, tile)
                kb.full_barrier()
            with contextlib.ExitStack() as es:
                self.s5(es, l, tile)
                kb.full_barrier()
        if self.stop_after == 's5': self.stopped = True; return
        with contextlib.ExitStack() as es:
            self.wout(es, l, tile)
            kb.full_barrier()
        if self.stop_after == 'wout': self.stopped = True; return

    def gla(self, es, l, tile):
        nc = self.nc; kb = self.kb
        sb = lambda n, s, d: self.sb(es, "g_" + n, s, d)
        WinG = self.wing_slot(es); TW = self.ptok("WinG")
        if getattr(self, 'wing_valid', None) != l:
            self.load_wing(l)
        WSs = self.WSs
        kb.dma('pool', WSs[:, 0:4096].rearrange("p (c n) -> p c n", c=DC), self.w_in[l, :, :, 0:512], writes=[self.ptok("WinS")])
        kb.dma('pool', WSs[:, 4096:6144].rearrange("p (c n) -> p c n", c=4), self.w_glu[l], writes=[self.ptok("Wglu")])
        glx = sb("glx", [33, TT], BF16); Tglx = Tok("glx")
        Epos = sb("Epos", [P, 2, TT], F32); Eneg = sb("Eneg", [P, 2, TT], F32)
        TEp = [Tok("Ep%d" % i) for i in range(8)]
        qd = sb("qd", [P, 2, TT], BF16); ki = sb("ki", [P, 2, TT], BF16); Tqk = [Tok("qk0"), Tok("qk1")]
        gs = sb("gs", [P, 4, TT], BF16); Tgs = [Tok("gs0"), Tok("gs1")]
        vt = sb("vt", [P, 8, 512], BF16); Tvt = [Tok("vt%d" % i) for i in range(8)]
        ke = sb("ke", [P, 8, 256], BF16); Tke = [Tok("ke%d" % i) for i in range(8)]
        Erc = [sb("Erc%d" % i, [P, 256], F32) for i in range(2)]; TErc = [Tok("Erc0"), Tok("Erc1")]
        e1 = [sb("e1_%d" % i, [P, 256], F32) for i in range(2)]; Te1 = [Tok("e1_0"), Tok("e1_1")]
        nl = [sb("nl_%d" % i, [P, 256], F32) for i in range(2)]; Tnl = [Tok("nl0"), Tok("nl1")]
        sT = [sb("sT%d" % i, [P, 4, 128], BF16) for i in range(2)]; TsT = [Tok("sT0"), Tok("sT1")]
        on = [sb("on%d" % i, [P, 4, 128], BF16) for i in range(2)]; Ton = [Tok("on0"), Tok("on1")]
        junk = sb("junk", [P, 128], BF16); Tjunk = Tok("junk")
        ss = [sb("ss%d" % i, [P, 4], F32) for i in range(2)]; Tss = [Tok("ss0"), Tok("ss1")]
        Tc = self.Tconst
        if tile == 0:
            kb.op('dve', lambda e: e.memset(self.gst_f[:], 0.0), writes=[self.Tgf])
            kb.op('dve', lambda e: e.memset(self.gst_b[0][:], 0.0), writes=[self.Tgb[0]])
        kb.op('dve', lambda e: e.memset(glx[:], 0.0), writes=[Tglx])
        kb.op('dve', lambda e: e.memset(glx[32:33, :], 1.0), writes=[Tglx])
        for hb in range(2):
            ps, Tp = self.psum_next()
            for c in range(DC):
                kb.op('pe', lambda e: e.matmul(ps[0:16, :], WinG[:, c, 1536:1552], self.hbuf[:, c, hb * 512:(hb + 1) * 512], start=(c == 0), stop=(c == DC - 1)), reads=[TW, self.Th[hb]], writes=[Tp])
            kb.op('act', lambda e: e.activation(out=glx[0:16, hb * 512:(hb + 1) * 512], in_=ps[0:16, :], func=AF.Copy), reads=[Tp], writes=[Tglx])
        for st in range(8):
            tk = slice(st * 128, (st + 1) * 128); hb = st // 4; r = st % 2
            ps, Tp = self.psum_next()
            kb.op('pe', lambda e: e.matmul(ps[:, 0:256], glx[0:33, tk], self.wgx[0:33, l, :], start=True, stop=True), reads=[Tglx, self.Twgx], writes=[Tp])
            kb.op('act', lambda e: e.activation(out=e1[r][:], in_=ps[:, 0:256], func=AF.Exp, scale=-1.0), reads=[Tp], writes=[Te1[r]])
            ps, Tp = self.psum_next()
            for c in range(DC):
                kb.op('pe', lambda e: e.matmul(ps[:], self.hbuf[:, c, tk], WinG[:, c, 512:1024], start=(c == 0), stop=(c == DC - 1)), reads=[TW, self.Th[hb]], writes=[Tp])
            kb.op('act', lambda e: e.activation(out=vt[:, st, :], in_=ps[:], func=AF.Copy), reads=[Tp], writes=[Tvt[st]])
            kb.op('act', lambda e: e.activation(out=nl[r][:], in_=e1[r][:], func=AF.Ln, bias=self.epsc[:, 1:2]), reads=[Te1[r], Tc], writes=[Tnl[r]])
            ps, Tp = self.psum_next()
            for c in range(2):
                kb.op('pe', lambda e: e.matmul(ps[:, c * 128:(c + 1) * 128], nl[r][:, c * 128:(c + 1) * 128], self.cmask[:], start=True, stop=True), reads=[Tnl[r], Tc], writes=[Tp])
            kb.op('act', lambda e: e.activation(out=Epos[:, :, tk], in_=ps[:, 0:256].rearrange("p (c t) -> p c t", c=2), func=AF.Exp, scale=-1.0 / 16), reads=[Tp], writes=[TEp[st]])
            kb.op('act', lambda e: e.activation(out=Eneg[:, :, tk], in_=ps[:, 0:256].rearrange("p (c t) -> p c t", c=2), func=AF.Exp, scale=1.0 / 16), reads=[Tp], writes=[TEp[st]])
            ps, Tp = self.psum_next()
            kb.op('pe', lambda e: e.matmul(ps[:, 0:256], self.triR[:], nl[r][:], start=True, stop=True), reads=[Tnl[r], Tc], writes=[Tp])
            kb.op('act', lambda e: e.activation(out=Erc[r][:], in_=ps[:, 0:256], func=AF.Exp, scale=-1.0 / 16), reads=[Tp], writes=[TErc[r]])
            ps, Tp = self.psum_next()
            for c in range(DC):
                kb.op('pe', lambda e: e.matmul(ps[:, 0:256], self.hbuf[:, c, tk], WinG[:, c, 256:512], start=(c == 0), stop=(c == DC - 1)), reads=[TW, self.Th[hb]], writes=[Tp])
            kb.op('dve', lambda e: e.tensor_tensor(out=ke[:, st, :], in0=ps[:, 0:256], in1=Erc[r][:], op=ALU.mult), reads=[Tp, TErc[r]], writes=[Tke[st]])
        for hb in range(2):
            hs = slice(hb * 512, (hb + 1) * 512)
            TE = TEp[hb * 4:(hb + 1) * 4]
            for c in range(2):
                ps, Tp = self.psum_next()
                for kc in range(DC):
                    kb.op('pe', lambda e: e.matmul(ps[:], WinG[:, kc, c * 128:(c + 1) * 128], self.hbuf[:, kc, hs], start=(kc == 0), stop=(kc == DC - 1)), reads=[TW, self.Th[hb]], writes=[Tp])
                kb.op('dve', lambda e: e.scalar_tensor_tensor(out=qd[:, c, hs], in0=ps[:], scalar=0.125, in1=Epos[:, c, hs], op0=ALU.mult, op1=ALU.mult), reads=[Tp] + TE, writes=[Tqk[hb]])
                ps, Tp = self.psum_next()
                for kc in range(DC):
                    kb.op('pe', lambda e: e.matmul(ps[:], WinG[:, kc, 256 + c * 128:256 + (c + 1) * 128], self.hbuf[:, kc, hs], start=(kc == 0), stop=(kc == DC - 1)), reads=[TW, self.Th[hb]], writes=[Tp])
                kb.op('dve', lambda e: e.tensor_tensor(out=ki[:, c, hs], in0=ps[:], in1=Eneg[:, c, hs], op=ALU.mult), reads=[Tp] + TE, writes=[Tqk[hb]])
            for c in range(4):
                ps, Tp = self.psum_next()
                for kc in range(DC):
                    kb.op('pe', lambda e: e.matmul(ps[:], WinG[:, kc, 1024 + c * 128:1024 + (c + 1) * 128], self.hbuf[:, kc, hs], start=(kc == 0), stop=(kc == DC - 1)), reads=[TW, self.Th[hb]], writes=[Tp])
                kb.op('act', lambda e: e.activation(out=gs[:, c, hs], in_=ps[:], func=AF.Silu), reads=[Tp], writes=[Tgs[hb]])
        pending = None
        for st in range(8):
            tk = slice(st * 128, (st + 1) * 128); hb = st // 4; r = st % 2
            psb = [self.psum_next(), self.psum_next()]
            for hd in range(4):
                c = hd // 2; par = hd % 2; pr = slice(par * 64, par * 64 + 64)
                ps, Tp = psb[par]
                kb.op('pe', lambda e: e.matmul(ps[:, c * 128:(c + 1) * 128], ki[pr, c, tk], qd[pr, c, tk], start=True, stop=True), reads=[Tqk[hb]], writes=[Tp])
            for par in range(2):
                ps, Tp = psb[par]
                dst = sT[r][:].rearrange("p (c two) t -> p two c t", two=2)[:, par, :, :]
                kb.op('dve', lambda e: e.tensor_tensor(out=dst, in0=ps[:, 0:256].rearrange("p (a b) -> p a b", b=128), in1=self.cmask[:].unsqueeze(1).to_broadcast([P, 2, 128]), op=ALU.mult), reads=[Tp, Tc], writes=[TsT[r]])
            def upd_state(half, dst):
                rows = slice(half * 64, half * 64 + 64)
                psu, Tpu = self.psum_next()
                for hd in range(4):
                    c = hd // 2; pr = slice((hd % 2) * 64, (hd % 2) * 64 + 64)
                    kb.op('pe', lambda e: e.matmul(psu[pr, c * 128:(c + 1) * 128], ke[rows, st, hd * 64:(hd + 1) * 64], vt[rows, st, hd * 128:(hd + 1) * 128], start=True, stop=True), reads=[Tke[st], Tvt[st]], writes=[Tpu])
                tend = st * 128 + half * 64 + 63
                for c in range(2):
                    kb.op('dve', lambda e: e.scalar_tensor_tensor(out=self.gst_f[:, c, :], in0=self.gst_f[:, c, :], scalar=Epos[:, c, tend:tend + 1], in1=psu[:, c * 128:(c + 1) * 128], op0=ALU.mult, op1=ALU.add), reads=[self.Tgf, TEp[st], Tpu], writes=[self.Tgf])
                kb.op('act', lambda e: e.activation(out=self.gst_b[dst][:].rearrange("p a b -> p (a b)"), in_=self.gst_f[:].rearrange("p a b -> p (a b)"), func=AF.Copy), reads=[self.Tgf], writes=[self.Tgb[dst]])
            upd_state(0, 1)
            pob = [self.psum_next(), self.psum_next()]
            for hd in range(4):
                c = hd // 2; par = hd % 2; pr = slice(par * 64, par * 64 + 64)
                pso, Tpo = pob[par]
                oc = slice(c * 128, (c + 1) * 128)
                kb.op('pe', lambda e: e.matmul(pso[:, oc], sT[r][:, hd, :], vt[:, st, hd * 128:(hd + 1) * 128], start=True, stop=False, skip_group_check=True), reads=[TsT[r], Tvt[st]], writes=[Tpo])
                kb.op('pe', lambda e: e.matmul(pso[0:64, oc], qd[pr, c, st * 128:st * 128 + 64], self.gst_b[0][pr, c, :], start=False, stop=False, skip_group_check=True), reads=[Tqk[hb], self.Tgb[0]], writes=[Tpo])
                kb.op('pe', lambda e: e.matmul(pso[64:128, oc], qd[pr, c, st * 128 + 64:st * 128 + 128], self.gst_b[1][pr, c, :], start=False, stop=True, skip_group_check=True), reads=[Tqk[hb], self.Tgb[1]], writes=[Tpo])
            upd_state(1, 0)
            for hd in range(4):
                c = hd // 2; par = hd % 2; pso, Tpo = pob[par]
                kb.op('act', lambda e: e.activation(out=junk[:], in_=pso[:, c * 128:(c + 1) * 128], func=AF.Square, accum_out=ss[r][:, hd:hd + 1]), reads=[Tpo], writes=[Tjunk, Tss[r]])
            kb.op('act', lambda e: e.activation(out=ss[r][:], in_=ss[r][:], func=AF.Sqrt, scale=1.0 / 128, bias=self.epsc[:, 0:1]), reads=[Tss[r], Tc], writes=[Tss[r]])
            kb.op('dve', lambda e: e.reciprocal(ss[r][:], ss[r][:]), reads=[Tss[r]], writes=[Tss[r]])
            for par in range(2):
                pso, Tpo = pob[par]
                dst = on[r][:].rearrange("p (c two) t -> p two c t", two=2)[:, par, :, :]
                sc = ss[r][:].rearrange("p (c two) -> p two c", two=2)[:, par, :].unsqueeze(2).to_broadcast([P, 2, 128])
                kb.op('dve', lambda e: e.tensor_tensor(out=dst, in0=pso[:, 0:256].rearrange("p (a b) -> p a b", b=128), in1=sc, op=ALU.mult), reads=[Tpo, Tss[r]], writes=[Ton[r]])
            def tail(st=st, tk=tk, hb=hb, r=r):
                pst, Tpt = self.psum_next()
                pstb = pst[:].bitcast(BF16)
                for hd in range(4):
                    kb.op('pe', lambda e: e.transpose(pstb[:, hd * 128:(hd + 1) * 128], on[r][:, hd, :], self.ident_bf[:]), reads=[Ton[r], Tc], writes=[Tpt])
                for hd in range(4):
                    kb.op('dve', lambda e: e.scalar_tensor_tensor(out=self.ycat[:, 4 + hd, tk], in0=pstb[:, hd * 128:(hd + 1) * 128], scalar=self.cols[:, C_GLAN + l * 4 + hd:C_GLAN + l * 4 + hd + 1], in1=gs[:, hd, tk], op0=ALU.mult, op1=ALU.mult), reads=[Tpt, Tc, Tgs[hb]], writes=[self.Tyc[hb][4 + hd]])
            if pending is not None:
                pending()
            pending = tail
        pending()

    def s5(self, es, l, tile):
        nc = self.nc; kb = self.kb
        sb = lambda n, s, d: self.sb(es, "s_" + n, s, d)
        Tc = self.Tconst
        slot = self.cur_slot
        self.wing_valid = None
        flat = slot[:].rearrange("p a b -> p (a b)")
        WSs = self.WSs
        WinS = WSs[:, 0:4096].rearrange("p (c n) -> p c n", c=DC); TW = self.ptok("WinS")
        Wglu = WSs[:, 4096:6144].rearrange("p (c n) -> p c n", c=4); TWg = self.ptok("Wglu")
        mats = [sb("mat%d" % i, [P, G, 128], BF16) for i in range(3)]; Tm = [self.ptok("mat%d" % i) for i in range(3)]
        for i in range(3):
            kb.dma('sp', mats[i][:].rearrange("p a b -> p (a b)"), self.s5m[l, i], reads=[self.Ts5m[l][i]], writes=[Tm[i]], owner=Tm[i])
        WB, Toep, WC = mats
        ring = [sb("ring%d" % i, [P, G, 128], BF16) for i in range(2)]; Tring = [self.ptok("ring0"), self.ptok("ring1")]
        bufA = sb("bufA", [P, 4, TT], BF16); TA = [Tok("bufA%d" % i) for i in range(4)]
        UT = sb("UT", [P, G, 128], BF16); TUT = [Tok("UT%d" % i) for i in range(8)]
        Sf = flat[:, 0:8256].bitcast(F32).rearrange("p (g j) -> p g j", j=129)
        Sb_ = flat[:, 8256:12384].rearrange("p (g j) -> p g j", j=129)
        TSf = [Tok("Sf%d" % i) for i in range(8)]; TSb = [Tok("Sb%d" % i) for i in range(8)]
        yg = ring[0]; Tyg = [Tok("yg%d" % i) for i in range(4)]
        y2 = self.hbuf
        if tile == 0:
            kb.op('dve', lambda e: e.memset(self.carry[:], 0.0), writes=[self.Tcarry])
        for cc in range(4):
            for hb in range(2):
                ps, Tp = self.psum_next()
                for c in range(DC):
                    kb.op('pe', lambda e: e.matmul(ps[:], WinS[:, c, cc * 128:(cc + 1) * 128], self.hbuf[:, c, hb * 512:(hb + 1) * 512], start=(c == 0), stop=(c == DC - 1)), reads=[TW, self.Th[hb]], writes=[Tp])
                eng = self.evac_eng()
                if eng == 'act':
                    kb.op('act', lambda e: e.activation(out=bufA[:, cc, hb * 512:(hb + 1) * 512], in_=ps[:], func=AF.Copy), reads=[Tp], writes=[TA[cc]])
                else:
                    kb.op('dve', lambda e: e.tensor_copy(bufA[:, cc, hb * 512:(hb + 1) * 512], ps[:]), reads=[Tp], writes=[TA[cc]])
        U8 = ring[1][:].rearrange("p g (s h) -> p g s h", s=8)
        for s_ in range(8):
            ps, Tp = self.psum_next()
            psb = ps[:].bitcast(BF16)
            for cc in range(4):
                mv = bufA[:, cc, :].rearrange("p (j s) -> p s j", s=8)[:, s_, :]
                kb.op('pe', lambda e: e.transpose(psb[:, cc * 128:(cc + 1) * 128], mv, self.ident_bf[:]), reads=[TA[cc], Tc], writes=[Tp])
            eng = self.evac_eng()
            if eng == 'act':
                kb.op('act', lambda e: e.activation(out=U8[:, :, s_, :], in_=psb[:, 0:512].rearrange("p (g h) -> p g h", h=16), func=AF.Copy), reads=[Tp], writes=[Tring[1]])
            else:
                kb.op('dve', lambda e: e.tensor_copy(U8[:, :, s_, :], psb[:, 0:512].rearrange("p (g h) -> p g h", h=16)), reads=[Tp], writes=[Tring[1]])
        for b in range(8):
            ps, Tp = self.psum_next()
            psb = ps[:].bitcast(BF16)
            for gi in range(4):
                g = 4 * b + gi
                kb.op('pe', lambda e: e.transpose(psb[:, gi * 128:(gi + 1) * 128], ring[1][:, g, :], self.ident_bf[:]), reads=[Tring[1], Tc], writes=[Tp])
            eng = self.evac_eng()
            dst = UT[:, 4 * b:4 * b + 4, :].rearrange("p a b -> p (a b)")
            if eng == 'act':
                kb.op('act', lambda e: e.activation(out=dst, in_=psb[:, 0:512], func=AF.Copy), reads=[Tp], writes=[TUT[b]])
            else:
                kb.op('dve', lambda e: e.tensor_copy(dst, psb[:, 0:512]), reads=[Tp], writes=[TUT[b]])
        for b in range(8):
            ps, Tp = self.psum_next()
            for gi in range(4):
                g = 4 * b + gi
                kb.op('pe', lambda e: e.matmul(ps[:, gi * 128:(gi + 1) * 128], WB[:, g, :], UT[:, g, :], start=True, stop=True), reads=[Tm[0], TUT[b]], writes=[Tp])
            g4 = slice(4 * b, 4 * b + 4)
            kb.op('dve', lambda e: e.tensor_copy(Sf[:, g4, 1:129], ps[:].rearrange("p (a b) -> p a b", b=128)), reads=[Tp], writes=[TSf[b]])
            kb.op('dve', lambda e: e.tensor_copy(Sf[:, g4, 0:1], self.carry[:, g4].unsqueeze(2)), reads=[self.Tcarry], writes=[TSf[b]])
            kb.op('act', lambda e: e.activation(out=Sb_[:, g4, :], in_=Sf[:, g4, :], func=AF.Copy), reads=[TSf[b]], writes=[TSb[b]])
        for k in range(8):
            d = 1 << k; n = 129 - d
            rg = ring[k % 2]; Tr = Tring[k % 2]
            kb.dma('sp', rg[:].rearrange("p a b -> p (a b)"), self.s5m[l, 3 + k], reads=[self.Ts5m[l][3 + k]], writes=[Tr], owner=Tr)
            for b in range(8):
                ps, Tp = self.psum_next()
                g4 = slice(4 * b, 4 * b + 4)
                for gi in range(4):
                    g = 4 * b + gi
                    kb.op('pe', lambda e: e.matmul(ps[:, gi * 128:gi * 128 + n], rg[:, g, :], Sb_[:, g, 0:n], start=True, stop=True), reads=[Tr, TSb[b]], writes=[Tp])
                kb.op('dve', lambda e: e.tensor_tensor(out=Sf[:, g4, d:129], in0=Sf[:, g4, d:129], in1=ps[:].rearrange("p (a b) -> p a b", b=128)[:, :, 0:n], op=ALU.add), reads=[Tp, TSf[b]], writes=[TSf[b]])
                kb.op('act', lambda e: e.activation(out=Sb_[:, g4, d:129], in_=Sf[:, g4, d:129], func=AF.Copy), reads=[TSf[b]], writes=[TSb[b]])
        kb.op('dve', lambda e: e.tensor_copy(self.carry[:].unsqueeze(2), Sf[:, :, 128:129]), reads=TSf, writes=[self.Tcarry])
        for b in range(8):
            ps, Tp = self.psum_next()
            for gi in range(4):
                g = 4 * b + gi
                kb.op('pe', lambda e: e.matmul(ps[:, gi * 128:(gi + 1) * 128], Toep[:, g, :], UT[:, g, :], start=True, stop=False), reads=[Tm[1], TUT[b]], writes=[Tp])
                kb.op('pe', lambda e: e.matmul(ps[:, gi * 128:(gi + 1) * 128], WC[:, g, :], Sb_[:, g, 0:128], start=False, stop=True), reads=[Tm[2], TSb[b]], writes=[Tp])
            kb.op('act', lambda e: e.activation(out=yg[:, 4 * b:4 * b + 4, :].rearrange("p a b -> p (a b)"), in_=ps[:], func=AF.Gelu_apprx_tanh), reads=[Tp], writes=[Tyg[b // 2], Tring[0]])
        Y8 = UT[:].rearrange("p g j -> p (g j)").rearrange("p (t c) -> p t c", t=8)
        for b in range(8):
            ps, Tp = self.psum_next()
            psb = ps[:].bitcast(BF16)
            for gi in range(4):
                g = 4 * b + gi
                kb.op('pe', lambda e: e.transpose(psb[:, gi * 128:(gi + 1) * 128], yg[:, g, :], self.ident_bf[:]), reads=[Tyg[b // 2], Tc], writes=[Tp])
            dst = Y8[:, :, 64 * b:64 * b + 64].rearrange("p t (g h) -> p g t h", g=4)
            src = psb[:, 0:512].rearrange("p (g t h) -> p g t h", g=4, t=8)
            eng = self.evac_eng()
            if eng == 'act':
                kb.op('act', lambda e: e.activation(out=dst, in_=src, func=AF.Copy), reads=[Tp], writes=TUT)
            else:
                kb.op('dve', lambda e: e.tensor_copy(dst, src), reads=[Tp], writes=TUT)
        for cc in range(4):
            for th in range(2):
                ps, Tp = self.psum_next()
                psb = ps[:].bitcast(BF16)
                for ti in range(4):
                    t0 = th * 4 + ti
                    kb.op('pe', lambda e: e.transpose(psb[:, ti * 128:(ti + 1) * 128], Y8[:, t0, cc * 128:(cc + 1) * 128], self.ident_bf[:]), reads=TUT + [Tc], writes=[Tp])
                dst = bufA[:, cc, :].rearrange("p (j s) -> p s j", s=8)[:, th * 4:th * 4 + 4, :]
                src = psb[:, 0:512].rearrange("p (a b) -> p a b", b=128)
                eng = self.evac_eng()
                if eng == 'act':
                    kb.op('act', lambda e: e.activation(out=dst, in_=src, func=AF.Copy), reads=[Tp], writes=[TA[cc]])
                else:
                    kb.op('dve', lambda e: e.tensor_copy(dst, src), reads=[Tp], writes=[TA[cc]])
        if 'yg' in self.dbg_out and tile == 0:
            kb.dma('pool', self.dbg_out['yg'], bufA[:], reads=TA, owner=TA[0])
        sq = UT[:, 0:16, :].rearrange("p (a b) c -> p a (b c)", a=4); Tsq = TUT[0:4]
        rs = UT[:, 16:24, :].rearrange("p a b -> p (a b)").bitcast(F32); Trs = TUT[4:6]
        gt = [UT[:, 24:28, :].rearrange("p a b -> p (a b)"), UT[:, 28:32, :].rearrange("p a b -> p (a b)")]; Tgt = [TUT[6], TUT[7]]
        for hb in range(2):
            hs = slice(hb * 512, (hb + 1) * 512)
            for co in range(4):
                ps, Tp = self.psum_next()
                for ci in range(4):
                    kb.op('pe', lambda e: e.matmul(ps[:], Wglu[:, ci, co * 128:(co + 1) * 128], bufA[:, ci, hs], start=(ci == 0), stop=(ci == 3)), reads=[TWg] + TA, writes=[Tp])
                r = co % 2
                kb.op('act', lambda e: e.activation(out=gt[r], in_=ps[:], func=AF.Sigmoid, bias=self.cols[:, C_BGLU + l * 4 + co:C_BGLU + l * 4 + co + 1]), reads=[Tp, Tc], writes=[Tgt[r]])
                kb.op('dve', lambda e: e.tensor_tensor(out=y2[:, co, hs], in0=bufA[:, co, hs], in1=gt[r], op=ALU.mult), reads=[TA[co], Tgt[r]], writes=[self.Th[hb]])
        for hb in range(2):
            hs = slice(hb * 512, (hb + 1) * 512)
            kb.op('act', lambda e: e.activation(out=sq, in_=y2[:, 0:4, hs], func=AF.Square), reads=[self.Th[hb]], writes=Tsq)
            ps, Tp = self.psum_next()
            for c in range(4):
                kb.op('pe', lambda e: e.matmul(ps[:], self.ones_bf[:], sq[:, c, :], start=(c == 0), stop=(c == 3)), reads=Tsq + [Tc], writes=[Tp])
            kb.op('act', lambda e: e.activation(out=rs, in_=ps[:], func=AF.Sqrt, scale=1.0 / 512, bias=self.epsc[:, 0:1]), reads=[Tp, Tc], writes=Trs)
            kb.op('dve', lambda e: e.reciprocal(rs, rs), reads=Trs, writes=Trs)
            for c in range(4):
                kb.op('dve', lambda e: e.scalar_tensor_tensor(out=self.ycat[:, c, hs], in0=y2[:, c, hs], scalar=self.cols[:, C_S5N + l * 4 + c:C_S5N + l * 4 + c + 1], in1=rs, op0=ALU.mult, op1=ALU.mult), reads=[self.Th[hb], Tc] + Trs, writes=[self.Tyc[hb][c]])

    def wout(self, es, l, tile):
        kb = self.kb
        self.wing_slot(es)
        Wout = self.sb(es, "Wout", [P, DC, DC, 128], BF16)
        TWo = [self.ptok("Wout%d" % c) for c in range(DC)]
        for co in range(DC):
            kb.dma('pool', Wout[:, co, :, :], self.w_out[l, co], writes=[TWo[co]])
        if tile == 0:
            self.load_wing(l)
        if 'ycat' in self.dbg_out and tile == 0:
            dtmp = self.sb(es, "dtmp2", [P, DC, TT], F32); Td = Tok("dtmp2")
            kb.op('dve', lambda e: e.tensor_copy(dtmp[:], self.ycat[:]), reads=[t for hb in range(2) for t in self.Tyc[hb]], writes=[Td])
            kb.dma('sp', self.dbg_out['ycat'], dtmp[:], reads=[Td])
            kb.barrier([Td], eng='dve')
        for hb in range(2):
            blk = tile * 2 + hb; t0 = blk * 512
            for co in range(DC):
                ps, Tp = self.psum_next()
                for ci in range(DC):
                    kb.op('pe', lambda e: e.matmul(ps[:], Wout[:, co, ci, :], self.ycat[:, ci, hb * 512:(hb + 1) * 512], start=(ci == 0), stop=(ci == DC - 1)), reads=[TWo[co], self.Tyc[hb][ci]], writes=[Tp])
                kb.op('dve', lambda e: e.tensor_tensor(out=self.xres[:, co, t0:t0 + 512], in0=ps[:], in1=self.xres[:, co, t0:t0 + 512], op=ALU.add), reads=[Tp, self.Tx[blk][co]], writes=[self.Tx[blk][co]])

    def ffn(self, l, tile):
        kb = self.kb
        with contextlib.ExitStack() as es:
            self.wing_slot(es)
            scr = self.norm_scratch(es)
            for hb in range(2):
                self.rmsnorm_block(scr, tile * 2 + hb, C_NFFN + l * 8, lambda c: self.hbuf[:, c, hb * 512:(hb + 1) * 512], [self.Th[hb]])
            kb.full_barrier()
        with contextlib.ExitStack() as es:
            sb = lambda n, s, d: self.sb(es, "f_" + n, s, d)
            self.wing_slot(es)
            if tile == 1 and self.next_layer is not None:
                self.load_wing(self.next_layer)
            act = sb("act", [P, FC, TT], BF16); Tact = [[Tok("act%d_%d" % (f, hb)) for hb in range(2)] for f in range(FC)]
            NR1 = 4; NR2 = 2
            W1 = [sb("W1_%d" % i, [P, DC, 2, 128], BF16) for i in range(NR1)]; TW1 = [self.ptok("W1_%d" % i) for i in range(NR1)]
            W2 = [sb("W2_%d" % i, [P, FC, 128], BF16) for i in range(NR2)]; TW2 = [self.ptok("W2_%d" % i) for i in range(NR2)]
            sg = [sb("sg%d" % i, [P, 512], BF16) for i in range(2)]; Tsg = [Tok("sg0"), Tok("sg1")]
            for f in range(FC):
                w = W1[f % NR1]; Tw = TW1[f % NR1]
                kb.dma('pool', w[:], self.w_f1[l, f], writes=[Tw])
                for hb in range(2):
                    hs = slice(hb * 512, (hb + 1) * 512)
                    psg, Tpg = self.psum_next()
                    for c in range(DC):
                        kb.op('pe', lambda e: e.matmul(psg[:], w[:, c, 0, :], self.hbuf[:, c, hs], start=(c == 0), stop=(c == DC - 1)), reads=[Tw, self.Th[hb]], writes=[Tpg])
                    psu, Tpu = self.psum_next()
                    for c in range(DC):
                        kb.op('pe', lambda e: e.matmul(psu[:], w[:, c, 1, :], self.hbuf[:, c, hs], start=(c == 0), stop=(c == DC - 1)), reads=[Tw, self.Th[hb]], writes=[Tpu])
                    r = (2 * f + hb) % 2
                    kb.op('act', lambda e: e.activation(out=sg[r][:], in_=psg[:], func=AF.Silu), reads=[Tpg], writes=[Tsg[r]])
                    kb.op('dve', lambda e: e.tensor_tensor(out=act[:, f, hs], in0=psu[:], in1=sg[r][:], op=ALU.mult), reads=[Tpu, Tsg[r]], writes=[Tact[f][hb]])
            for co in range(DC):
                w = W2[co % NR2]; Tw = TW2[co % NR2]
                kb.dma('pool', w[:], self.w_f2[l, co], writes=[Tw])
                for hb in range(2):
                    blk = tile * 2 + hb; t0 = blk * 512
                    ps, Tp = self.psum_next()
                    for f in range(FC):
                        kb.op('pe', lambda e: e.matmul(ps[:], w[:, f, :], act[:, f, hb * 512:(hb + 1) * 512], start=(f == 0), stop=(f == FC - 1)), reads=[Tw, Tact[f][hb]], writes=[Tp])
                    kb.op('dve', lambda e: e.tensor_tensor(out=self.xres[:, co, t0:t0 + 512], in0=ps[:], in1=self.xres[:, co, t0:t0 + 512], op=ALU.add), reads=[Tp, self.Tx[blk][co]], writes=[self.Tx[blk][co]])
            kb.full_barrier()
            for e_ in ('pool',):
                kb.barrier(TW1 + TW2, eng=e_)

    def final_norm(self, s):
        kb = self.kb
        with contextlib.ExitStack() as es:
            self.wing_slot(es)
            scr = self.norm_scratch(es)
            ob = [self.sb(es, "fo%d" % i, [P, DC, 512], F32) for i in range(2)]
            To = [self.ptok("fo0"), self.ptok("fo1")]
            self.Tout = To
            for blk in range(4):
                r = blk % 2
                self.rmsnorm_block(scr, blk, C_NFIN, lambda c: ob[r][:, c, :], [To[r]])
                kb.dma('sp', self.yT[s, :, :, blk * 512:(blk + 1) * 512], ob[r][:], reads=[To[r]], owner=To[r])
            kb.full_barrier()
            for e_ in ('sp', 'act', 'dve', 'pool', 'pe'):
                kb.barrier(To, eng=e_)


def _consts():
    c = np.zeros((P, NCONST), np.float32)
    idx = np.arange(P)
    c[:, K_ID:K_ID + 128] = np.eye(P, dtype=np.float32)
    s = idx[:, None]; t = idx[None, :]
    same = (s // 64) == (t // 64)
    c[:, K_CM:K_CM + 128] = (same & (s <= t)).astype(np.float32)
    c[:, K_TR:K_TR + 128] = (same & (s > t)).astype(np.float32)
    c[:, K_TM:K_TM + 128] = ((t // 16) >= (s // 16)).astype(np.float32)
    c[:, K_SW:K_SW + 128] = (((s % 64) == (t % 64)) & ((s // 64) != (t // 64))).astype(np.float32)
    st = np.zeros((P, 8, 240), np.float32)
    for gl in range(8):
        for h in range(16):
            st[gl * 16 + h, gl, 112 + h] = 1.0
    c[:, K_ST:K_ST + 1920] = st.reshape(P, 1920)
    c[:, K_NV:K_NV + 32] = np.array(NVALS, np.float32)[None, :]
    c[0:64, K_SG] = 1.0; c[64:128, K_SG] = -1.0
    c[0:64, K_SG + 1] = -1.0; c[64:128, K_SG + 1] = 1.0
    return c


def prep_shared(inp):
    f = lambda a: np.ascontiguousarray(np.asarray(a, dtype=np.float32))
    sh = {}
    sh["w_in"] = f(inp["w_in"].reshape(NL, DC, P, DIN).transpose(0, 2, 1, 3))
    sh["w_glu"] = f(inp["s5_w_glu"].reshape(NL, 4, P, 512).transpose(0, 2, 1, 3))
    sh["w_out"] = f(inp["w_out"].reshape(NL, DC, P, DC, 128).transpose(0, 3, 2, 1, 4))
    sh["w_f1"] = f(inp["w_ffn_in"].reshape(NL, DC, P, 2, FC, 128).transpose(0, 4, 2, 1, 3, 5))
    sh["w_f2"] = f(inp["w_ffn_out"].reshape(NL, FC, P, DC, 128).transpose(0, 3, 2, 1, 4))
    cols = np.zeros((P, NCOL), np.float32)
    cols[:, C_NMIX:C_NMIX + 32] = inp["norm_mix"].reshape(NL, DC, P).transpose(2, 0, 1).reshape(P, 32)
    cols[:, C_NFFN:C_NFFN + 32] = inp["norm_ffn"].reshape(NL, DC, P).transpose(2, 0, 1).reshape(P, 32)
    cols[:, C_NFIN:C_NFIN + 8] = inp["norm_final"].reshape(DC, P).T
    cols[:, C_BGLU:C_BGLU + 16] = inp["s5_b_glu"].reshape(NL, 4, P).transpose(2, 0, 1).reshape(P, 16)
    cols[:, C_S5N:C_S5N + 16] = inp["s5_out_norm"].reshape(NL, 4, P).transpose(2, 0, 1).reshape(P, 16)
    cols[:, C_GLAN:C_GLAN + 16] = inp["gla_out_norm"].reshape(NL, 4, P).transpose(2, 0, 1).reshape(P, 16)
    sh["cols"] = cols
    wgx = np.zeros((33, NL, 256), np.float32)
    wgx[0:16] = inp["gla_w_gate"].transpose(1, 0, 2)
    wgx[32] = inp["gla_b_gate"]
    sh["wgx"] = wgx
    s5p = np.zeros((P, NL, NS5), np.float32)
    dup = lambda a: np.concatenate([a, a], 0)
    lre = inp["s5_lam_re"].transpose(2, 0, 1); lim = inp["s5_lam_im"].transpose(2, 0, 1)
    s5p[:, :, 0:32] = dup(lre); s5p[:, :, 32:64] = dup(lim)
    s5p[:, :, 64:96] = np.broadcast_to(inp["s5_log_step"][None], (P, NL, G))
    bre = inp["s5_b_re"].transpose(2, 0, 1, 3).reshape(64, NL, 512); bim = inp["s5_b_im"].transpose(2, 0, 1, 3).reshape(64, NL, 512)
    s5p[:, :, 96:608] = np.concatenate([bre, bim], 0); s5p[:, :, 608:1120] = np.concatenate([bim, bre], 0)
    cre = inp["s5_c_re"].transpose(3, 0, 1, 2).reshape(64, NL, 512); cim = inp["s5_c_im"].transpose(3, 0, 1, 2).reshape(64, NL, 512)
    s5p[:, :, 1120:1632] = dup(cre); s5p[:, :, 1632:2144] = dup(cim)
    sh["s5p"] = s5p
    dd = inp["s5_d"].reshape(NL, G, 16).transpose(2, 0, 1)
    sh["dcol"] = f(np.tile(dd, (8, 1, 1)))
    sh["consts"] = _consts()
    return sh


_PROG = {}


def kernel(**inputs):
    x = np.asarray(inputs["x"], dtype=np.float32)
    B = x.shape[0]; ncores = 8; nseq = B // ncores
    sh = prep_shared(inputs)
    if "full" not in _PROG:
        _PROG["full"] = MK(nseq=nseq)
    mk = _PROG["full"]
    in_maps = []
    for c in range(ncores):
        xs = x[c * nseq:(c + 1) * nseq]
        xT = np.ascontiguousarray(xs.reshape(nseq, L, DC, P).transpose(0, 3, 2, 1))
        m = dict(sh); m["xT"] = xT
        in_maps.append(m)
    res = run_bass_kernel_spmd(mk.nc, in_maps, core_ids=list(range(ncores)))
    out = np.empty((B, L, D), np.float32)
    for c in range(ncores):
        yT = res.results[c]["yT"]
        out[c * nseq:(c + 1) * nseq] = yT.transpose(0, 3, 2, 1).reshape(nseq, L, D)
    return out
```

```python
import contextlib, itertools, math
import numpy as np
import concourse.bass as bass
import concourse.mybir as mybir
from concourse.bass_utils import run_bass_kernel_spmd

F32 = mybir.dt.float32; BF16 = mybir.dt.bfloat16; I32 = mybir.dt.int32
AF = mybir.ActivationFunctionType; ALU = mybir.AluOpType
P = 128; L = 2048; D = 1024; DC = 8; TT = 1024; DIN = 2064; DFF = 2816; FC = 22
NL = 4; G = 32; EPS = 1e-6
TWO_PI = 2.0 * math.pi
C_NMIX = 0; C_NFFN = 32; C_NFIN = 64; C_BGLU = 72; C_S5N = 88; C_GLAN = 104; NCOL = 120
K_ID = 0; K_CM = 128; K_TR = 256; K_TM = 384; K_SW = 512; K_ST = 640; K_NV = 640 + 1920; K_SG = K_NV + 32; NCONST = K_SG + 2
NS5 = 96 + 4 * 512
NVALS = [7, 6, 5, 4, 3, 2, 1, 0, -1, -2, -3, -4, -5, -6, -7, -8, 1, 2, 3, 4, 5, 6, 7, 8, 8, 16, 32, 64, 128, 256, 512, 1024]


class Tok:
    __slots__ = ('name', 'w', 'rd', 'sem', 'semv')

    def __init__(self, name):
        self.name = name; self.w = None; self.rd = {}; self.sem = None; self.semv = 0


class KB:
    def __init__(self, nc, es):
        self.nc = nc; self.es = es
        self.h = {'pe': nc.tensor, 'act': nc.scalar, 'dve': nc.vector, 'pool': nc.gpsimd, 'sp': nc.sync}
        self.sem = {}; self.cnt = {}; self.seen = {}
        self.pesems = set(); self.epoch = 0
        for e in self.h:
            self.seen[e] = {}
        self._new_sems()
        self.nwait = 0; self.nins = 0
        self.dsems = []; self.dlast = {}

    def _new_sems(self):
        for e in self.h:
            self.sem[e] = self.es.enter_context(self.nc.semaphore('s%d_%s' % (self.epoch, e))); self.cnt[e] = 0
        self.pesems.add(self.sem['pe'])
        self.epoch += 1

    def new_epoch(self):
        self.full_barrier()
        self._new_sems()

    def _deps(self, reads, writes):
        need = {}
        for b in reads:
            d = b.w
            if d is not None and need.get(d[0], 0) < d[1]: need[d[0]] = d[1]
        for b in writes:
            d = b.w
            if d is not None and need.get(d[0], 0) < d[1]: need[d[0]] = d[1]
            for k, v in b.rd.items():
                if need.get(k, 0) < v: need[k] = v
        return need

    def _wait(self, eng, need):
        seen = self.seen[eng]
        for k, v in need.items():
            if eng == 'pe' and k in self.pesems: continue
            if seen.get(k, 0) >= v: continue
            self.h[eng].wait_ge(k, v); seen[k] = v; self.nwait += 1

    def op(self, eng, fn, reads=(), writes=()):
        self._wait(eng, self._deps(reads, writes))
        ins = fn(self.h[eng])
        self.cnt[eng] += 1; c = self.cnt[eng]; sm = self.sem[eng]
        ins.then_inc(sm, 1); self.nins += 1
        for b in reads: b.rd[sm] = c
        for b in writes:
            b.w = (sm, c); b.rd = {}
        return ins

    def dma(self, eng, out, in_, reads=(), writes=(), owner=None):
        self._wait(eng, self._deps(reads, writes))
        own = owner or (writes[0] if writes else reads[0])
        if own.sem is None:
            own.sem = self.es.enter_context(self.nc.semaphore('d%d' % len(self.dsems))); own.semv = 0
            self.dsems.append(own.sem)
        ins = self.h[eng].dma_start(out=out, in_=in_)
        own.semv += 16
        ins.then_inc(own.sem, 16); self.nins += 1
        self.dlast[own.sem] = own.semv
        for b in reads: b.rd[own.sem] = own.semv
        for b in writes:
            b.w = (own.sem, own.semv); b.rd = {}
        return ins

    def barrier(self, toks, eng='sp'):
        need = {}
        for b in toks:
            for k, v in ([b.w] if b.w else []) + list(b.rd.items()):
                if need.get(k, 0) < v: need[k] = v
        self._wait(eng, need)

    def full_barrier(self, dmas=True):
        for e in self.h:
            need = {self.sem[k]: self.cnt[k] for k in self.h if self.cnt[k] > 0 and k != e}
            if dmas:
                need.update(self.dlast)
            self._wait(e, need)


class MK:
    def __init__(self, nseq=4, layers=(0, 1, 2, 3), final=True, prologue=True, dbg=None, stop_after=None):
        self.nseq = nseq; self.layers = list(layers); self.final = final; self.dbg = dbg or {}
        self.do_prologue = prologue; self.stop_after = stop_after
        nc = self.nc = bass.Bass("TRN2", target_bir_lowering=False)
        dt = nc.dram_tensor
        self.xT = dt("xT", [nseq, P, DC, L], F32, kind="ExternalInput").ap()
        self.w_in = dt("w_in", [NL, P, DC, DIN], F32, kind="ExternalInput").ap()
        self.w_glu = dt("w_glu", [NL, P, 4, 512], F32, kind="ExternalInput").ap()
        self.w_out = dt("w_out", [NL, DC, P, DC, 128], F32, kind="ExternalInput").ap()
        self.w_f1 = dt("w_f1", [NL, FC, P, DC, 2, 128], F32, kind="ExternalInput").ap()
        self.w_f2 = dt("w_f2", [NL, DC, P, FC, 128], F32, kind="ExternalInput").ap()
        self.cols_d = dt("cols", [P, NCOL], F32, kind="ExternalInput").ap()
        self.wgx_d = dt("wgx", [33, NL, 256], F32, kind="ExternalInput").ap()
        self.s5p_d = dt("s5p", [P, NL, NS5], F32, kind="ExternalInput").ap()
        self.dcol_d = dt("dcol", [P, NL, G], F32, kind="ExternalInput").ap()
        self.consts_d = dt("consts", [P, NCONST], F32, kind="ExternalInput").ap()
        self.yT = dt("yT", [nseq, P, DC, L], F32, kind="ExternalOutput").ap()
        if prologue:
            self.s5m = dt("s5m", [NL, 11, P, 4096], BF16, kind="Internal").ap()
        else:
            self.s5m = dt("s5m", [NL, 11, P, 4096], BF16, kind="ExternalInput").ap()
        self.dbg_out = {}
        for k, shp in self.dbg.items():
            self.dbg_out[k] = dt("dbg_" + k, list(shp), F32, kind="ExternalOutput").ap()
        self.build()

    def sb(self, es, name, shape, dtype):
        self.uid = getattr(self, 'uid', 0) + 1
        return es.enter_context(self.nc.sbuf_tensor("%s_%d" % (name, self.uid), list(shape), dtype))

    def ptok(self, name):
        d = self.__dict__.setdefault('_ptoks', {})
        if name not in d: d[name] = Tok(name)
        return d[name]

    def wing_slot(self, es):
        return self.cur_slot

    def load_wing(self, l):
        TW = self.ptok("WinG")
        for c in range(DC):
            self.kb.dma('pool', self.cur_slot[:, c, :], self.w_in[l, :, c, 512:2064], writes=[TW])
        self.wing_valid = l

    def psum_next(self):
        i = self.ps_i; self.ps_i = (i + 1) % 8
        return self.ps[i], self.Tps[i]

    def evac_eng(self):
        return next(self.ev)

    def build(self):
        nc = self.nc
        with contextlib.ExitStack() as es:
            kb = self.kb = KB(nc, es)
            self.ev = itertools.cycle(['act', 'dve'])
            self.ps = [es.enter_context(nc.psum_tensor("ps%d" % i, [P, 512], F32)) for i in range(8)]
            self.Tps = [Tok("ps%d" % i) for i in range(8)]
            self.ps_i = 0
            self.ident_bf = self.sb(es, "ident_bf", [P, P], BF16)
            self.ones_bf = self.sb(es, "ones_bf", [P, P], BF16)
            self.strips = self.sb(es, "strips", [P, 8, 240], BF16)
            self.cmask = self.sb(es, "cmask", [P, P], F32)
            self.triR = self.sb(es, "triR", [P, P], F32)
            self.cols = self.sb(es, "cols", [P, NCOL], F32)
            self.wgx = self.sb(es, "wgx", [33, NL, 256], BF16)
            self.epsc = self.sb(es, "epsc", [P, 2], F32)
            self.Tconst = Tok("const")
            T = self.Tconst
            with contextlib.ExitStack() as es2:
                cst = self.sb(es2, "cst", [P, NCONST], F32)
                Tc = Tok("cst")
                kb.dma('sp', cst[:], self.consts_d, writes=[Tc])
                kb.dma('sp', self.cols[:], self.cols_d, writes=[T])
                self.Twgx = Tok('wgx'); kb.dma('pool', self.wgx[:], self.wgx_d, writes=[self.Twgx])
                kb.op('dve', lambda e: e.tensor_copy(self.ident_bf[:], cst[:, K_ID:K_ID + 128]), reads=[Tc], writes=[T])
                kb.op('dve', lambda e: e.memset(self.ones_bf[:], 1.0), writes=[T])
                kb.op('dve', lambda e: e.memset(self.epsc[:, 0:1], EPS), writes=[T])
                kb.op('dve', lambda e: e.memset(self.epsc[:, 1:2], 1.0), writes=[T])
                kb.op('dve', lambda e: e.tensor_copy(self.strips[:].rearrange("p a b -> p (a b)"), cst[:, K_ST:K_ST + 1920]), reads=[Tc], writes=[T])
                kb.op('dve', lambda e: e.tensor_copy(self.cmask[:], cst[:, K_CM:K_CM + 128]), reads=[Tc], writes=[T])
                kb.op('dve', lambda e: e.tensor_copy(self.triR[:], cst[:, K_TR:K_TR + 128]), reads=[Tc], writes=[T])
                if self.do_prologue:
                    self.Ts5m = [[self.ptok("s5m%d" % l)] * 11 for l in range(NL)]
                    for l in self.layers:
                        self.prologue(l, cst, Tc)
                else:
                    self.Ts5m = [[self.ptok("s5m%d" % l)] * 11 for l in range(NL)]
                kb.full_barrier()
                allt = [self.Ts5m[l][0] for l in range(NL)]
                for e in ('sp', 'act', 'pool', 'pe', 'dve'):
                    kb.barrier(allt + [Tc, T, self.Twgx], eng=e)
            if self.stop_after != 'prologue':
                self.main(es)

    def prologue(self, l, cst, Tc):
        nc = self.nc; kb = self.kb
        with contextlib.ExitStack() as es:
            sb = lambda n, s, d=F32: self.sb(es, "pl_" + n, s, d)
            sp = sb("sp", [P, NS5]); dc = sb("dc", [P, G])
            Tsp = self.ptok("pl_sp")
            kb.dma('sp', sp[:], self.s5p_d[:, l, :], writes=[Tsp])
            kb.dma('sp', dc[:], self.dcol_d[:, l, :], writes=[Tsp])
            lr2 = sp[:, 0:32]; li2 = sp[:, 32:64]; ls2 = sp[:, 64:96]
            R1 = sp[:, 96:608]; R2 = sp[:, 608:1120]; C1 = sp[:, 1120:1632]; C2 = sp[:, 1632:2144]
            nv = cst[:, K_NV:K_NV + 32]
            sgA = cst[:, K_SG:K_SG + 1]; sgB = cst[:, K_SG + 1:K_SG + 2]
            sm = sb("sm", [P, 16, 32])
            Tsm = Tok("sm")
            tb = {k: sb("tb_" + k, [P, 32, 32]) for k in ("mag", "tr", "tf", "rc", "Cn", "Sn", "SnA", "SnB")}
            ti = sb("ti", [P, 32, 32], I32)
            Ttb = Tok("tb")
            V = lambda e: e
            def dve(fn, r, w): kb.op('dve', fn, reads=r, writes=w)
            def act(fn, r, w): kb.op('act', fn, reads=r, writes=w)
            step = sm[:, 0, :]; lrs = sm[:, 1, :]; lis = sm[:, 2, :]
            act(lambda e: e.activation(out=step, in_=ls2, func=AF.Exp), [Tsp], [Tsm])
            dve(lambda e: e.tensor_tensor(out=lrs, in0=lr2, in1=step, op=ALU.mult), [Tsp, Tsm], [Tsm])
            dve(lambda e: e.tensor_tensor(out=lis, in0=li2, in1=step, op=ALU.mult), [Tsp, Tsm], [Tsm])
            def bc_g(a):
                return a.unsqueeze(1).to_broadcast([P, 32, 32])
            def bc_n(a):
                return a.unsqueeze(2).to_broadcast([P, 32, 32])
            flat = lambda t: t[:].rearrange("p a b -> p (a b)")
            dve(lambda e: e.tensor_tensor(out=tb["mag"][:], in0=bc_g(lrs), in1=bc_n(nv), op=ALU.mult), [Tsm, Tc], [Ttb])
            act(lambda e: e.activation(out=flat(tb["mag"]), in_=flat(tb["mag"]), func=AF.Exp), [Ttb], [Ttb])
            dve(lambda e: e.scalar_tensor_tensor(out=tb["tr"][:], in0=bc_g(lis), scalar=1.0 / TWO_PI, in1=bc_n(nv), op0=ALU.mult, op1=ALU.mult), [Tsm, Tc], [Ttb])
            for name, ph in (("Cn", 0.25), ("Sn", 0.0)):
                dve(lambda e: e.tensor_scalar(out=flat(ti), in0=flat(tb["tr"]), scalar1=ph, scalar2=None, op0=ALU.add), [Ttb], [Ttb])
                dve(lambda e: e.tensor_copy(flat(tb["tf"]), flat(ti)), [Ttb], [Ttb])
                dve(lambda e: e.scalar_tensor_tensor(out=flat(tb["rc"]), in0=flat(tb["tr"]), scalar=ph, in1=flat(tb["tf"]), op0=ALU.add, op1=ALU.subtract), [Ttb], [Ttb])
                act(lambda e: e.activation(out=flat(tb[name]), in_=flat(tb["rc"]), func=AF.Sin, scale=6.283184), [Ttb], [Ttb])
                dve(lambda e: e.tensor_tensor(out=flat(tb[name]), in0=flat(tb[name]), in1=flat(tb["mag"]), op=ALU.mult), [Ttb], [Ttb])
            dve(lambda e: e.tensor_scalar(out=flat(tb["SnA"]), in0=flat(tb["Sn"]), scalar1=sgA, scalar2=None, op0=ALU.mult), [Ttb, Tc], [Ttb])
            dve(lambda e: e.tensor_scalar(out=flat(tb["SnB"]), in0=flat(tb["Sn"]), scalar1=sgB, scalar2=None, op0=ALU.mult), [Ttb, Tc], [Ttb])
            ar = tb["Cn"][:, 16, :]; ai = tb["Sn"][:, 16, :]
            s_ = lambda i: sm[:, i, :]
            tt = lambda o, a, b, op: dve(lambda e: e.tensor_tensor(out=o, in0=a, in1=b, op=op), [Tsm, Ttb, Tsp], [Tsm])
            dve(lambda e: e.tensor_scalar(out=s_(3), in0=ar, scalar1=-1.0, scalar2=None, op0=ALU.add), [Ttb], [Tsm])
            tt(s_(4), s_(3), lr2, ALU.mult); tt(s_(5), ai, li2, ALU.mult); tt(s_(4), s_(4), s_(5), ALU.add)
            tt(s_(5), ai, lr2, ALU.mult); tt(s_(6), s_(3), li2, ALU.mult); tt(s_(5), s_(5), s_(6), ALU.subtract)
            tt(s_(6), lr2, lr2, ALU.mult); tt(s_(7), li2, li2, ALU.mult); tt(s_(6), s_(6), s_(7), ALU.add)
            dve(lambda e: e.reciprocal(s_(6), s_(6)), [Tsm], [Tsm])
            tt(s_(8), s_(4), s_(6), ALU.mult)
            tt(s_(9), s_(5), s_(6), ALU.mult)
            dve(lambda e: e.tensor_scalar(out=s_(10), in0=s_(9), scalar1=sgB, scalar2=None, op0=ALU.mult), [Tsm, Tc], [Tsm])
            dve(lambda e: e.tensor_scalar(out=s_(11), in0=s_(9), scalar1=sgA, scalar2=None, op0=ALU.mult), [Tsm, Tc], [Tsm])
            X1 = sb("X1", [P, 32, 16]); X2 = sb("X2", [P, 32, 16]); Xt = sb("Xt", [P, 32, 16])
            TX = Tok("X")
            bh = lambda a: a.unsqueeze(2).to_broadcast([P, 32, 16])
            r3 = lambda a: a.rearrange("p (g h) -> p g h", h=16)
            dve(lambda e: e.tensor_tensor(out=X1[:], in0=r3(R1), in1=bh(s_(8)), op=ALU.mult), [Tsp, Tsm], [TX])
            dve(lambda e: e.tensor_tensor(out=Xt[:], in0=r3(R2), in1=bh(s_(10)), op=ALU.mult), [Tsp, Tsm], [TX])
            dve(lambda e: e.tensor_tensor(out=X1[:], in0=X1[:], in1=Xt[:], op=ALU.add), [TX], [TX])
            dve(lambda e: e.tensor_tensor(out=X2[:], in0=r3(R2), in1=bh(s_(8)), op=ALU.mult), [Tsp, Tsm], [TX])
            dve(lambda e: e.tensor_tensor(out=Xt[:], in0=r3(R1), in1=bh(s_(11)), op=ALU.mult), [Tsp, Tsm], [TX])
            dve(lambda e: e.tensor_tensor(out=X2[:], in0=X2[:], in1=Xt[:], op=ALU.add), [TX], [TX])
            T3 = sb("T3", [P, 8, 32]); T4 = sb("T4", [P, 8, 32]); TT34 = Tok("T34")
            dve(lambda e: e.tensor_copy(T3[0:64], tb["Cn"][0:64, 16:24, :]), [Ttb], [TT34])
            dve(lambda e: e.tensor_scalar(out=T3[64:128], in0=tb["Sn"][64:128, 16:24, :], scalar1=-1.0, scalar2=None, op0=ALU.mult), [Ttb], [TT34])
            dve(lambda e: e.tensor_scalar(out=T4[0:64], in0=tb["Sn"][0:64, 16:24, :], scalar1=-1.0, scalar2=None, op0=ALU.mult), [Ttb], [TT34])
            dve(lambda e: e.tensor_scalar(out=T4[64:128], in0=tb["Cn"][64:128, 16:24, :], scalar1=-1.0, scalar2=None, op0=ALU.mult), [Ttb], [TT34])
            big = {k: sb("big_" + k, [P, 32, 8, 16]) for k in ("WBe", "WBn", "WC", "m1", "m2")}
            Tbig = {k: Tok("big" + k) for k in big}
            def tab(t, i0):
                return t[:, i0:i0 + 8, :].rearrange("p s g -> p g s").unsqueeze(3).to_broadcast([P, 32, 8, 16])
            def xb(x):
                return x[:].unsqueeze(2).to_broadcast([P, 32, 8, 16])
            def combo(dst, ta, xa, tb_, xb_, i0, extra_r):
                dve(lambda e: e.tensor_tensor(out=big["m1"][:], in0=tab(ta, i0), in1=xb(xa), op=ALU.mult), extra_r, [Tbig["m1"]])
                dve(lambda e: e.tensor_tensor(out=big["m2"][:], in0=tab(tb_, i0), in1=xb(xb_), op=ALU.mult), extra_r, [Tbig["m2"]])
                dve(lambda e: e.tensor_tensor(out=big[dst][:], in0=big["m1"][:], in1=big["m2"][:], op=ALU.add), [Tbig["m1"], Tbig["m2"]], [Tbig[dst]])
            combo("WBe", tb["Cn"], X1, tb["SnB"], X2, 0, [Ttb, TX])
            combo("WBn", tb["Cn"], X1, tb["SnB"], X2, 8, [Ttb, TX])
            C1v = sb("C1v", [P, 32, 16]); C2v = sb("C2v", [P, 32, 16]); TC = Tok("C")
            dve(lambda e: e.tensor_copy(C1v[:], r3(C1)), [Tsp], [TC])
            dve(lambda e: e.tensor_copy(C2v[:], r3(C2)), [Tsp], [TC])
            combo("WC", T3, C1v, T4, C2v, 0, [TT34, TC])
            idf = cst[:, K_ID:K_ID + 128]; tmask = cst[:, K_TM:K_TM + 128]; swapm = cst[:, K_SW:K_SW + 128]
            outb = [sb("outb%d" % i, [P, 32, 128], BF16) for i in range(2)]
            Toutb = [Tok("outb%d" % i) for i in range(2)]
            tmpT = sb("tmpT", [P, 4, 128]); TtmpT = Tok("tmpT")
            g3 = lambda t, g: t[:, g, :, :].rearrange("p s h -> p (s h)")
            for b in range(8):
                ps, Tp = self.psum_next()
                for gi in range(4):
                    g = 4 * b + gi
                    kb.op('pe', lambda e: e.transpose(ps[:, gi * 128:(gi + 1) * 128], g3(big["WBe"], g), idf), reads=[Tbig["WBe"], Tc], writes=[Tp])
                kb.op('act', lambda e: e.activation(out=outb[0][:, 4 * b:4 * b + 4, :].rearrange("p a b -> p (a b)"), in_=ps[:], func=AF.Copy), reads=[Tp], writes=[Toutb[0]])
            kb.dma('sp', self.s5m[l, 0], outb[0][:].rearrange("p a b -> p (a b)"), reads=[Toutb[0]], writes=[self.Ts5m[l][0]], owner=self.Ts5m[l][0])
            for b in range(8):
                ps, Tp = self.psum_next()
                for gi in range(4):
                    g = 4 * b + gi
                    kb.op('pe', lambda e: e.matmul(ps[:, gi * 128:(gi + 1) * 128], g3(big["WBn"], g), g3(big["WC"], g), start=True, stop=True), reads=[Tbig["WBn"], Tbig["WC"]], writes=[Tp])
                dve(lambda e: e.tensor_tensor(out=tmpT[:], in0=ps[:].rearrange("p (a b) -> p a b", b=128), in1=tmask.unsqueeze(1).to_broadcast([P, 4, 128]), op=ALU.mult), [Tp, Tc], [TtmpT])
                for gi in range(4):
                    g = 4 * b + gi
                    dve(lambda e: e.scalar_tensor_tensor(out=outb[1][:, g, :], in0=idf, scalar=dc[:, g:g + 1], in1=tmpT[:, gi, :], op0=ALU.mult, op1=ALU.add), [TtmpT, Tc, Tsp], [Toutb[1]])
            kb.dma('sp', self.s5m[l, 1], outb[1][:].rearrange("p a b -> p (a b)"), reads=[Toutb[1]], writes=[self.Ts5m[l][1]], owner=self.Ts5m[l][1])
            dve(lambda e: e.tensor_copy(outb[0][:].rearrange("p a b -> p (a b)"), big["WC"][:].rearrange("p g s h -> p (g s h)")), [Tbig["WC"]], [Toutb[0]])
            kb.dma('sp', self.s5m[l, 2], outb[0][:].rearrange("p a b -> p (a b)"), reads=[Toutb[0]], writes=[self.Ts5m[l][2]], owner=self.Ts5m[l][2])
            m1 = big["m1"][:].rearrange("p g s h -> p g (s h)"); m2 = big["m2"][:].rearrange("p g s h -> p g (s h)")
            for k in range(8):
                colC = tb["Cn"][:, 24 + k, :].unsqueeze(2).to_broadcast([P, 32, 128])
                colS = tb["SnA"][:, 24 + k, :].unsqueeze(2).to_broadcast([P, 32, 128])
                ob = outb[(k + 1) % 2]; To = Toutb[(k + 1) % 2]
                dve(lambda e: e.tensor_tensor(out=m1, in0=idf.unsqueeze(1).to_broadcast([P, 32, 128]), in1=colC, op=ALU.mult), [Tc, Ttb], [Tbig["m1"]])
                kb.op('pool', lambda e: e.tensor_tensor(out=m2, in0=swapm.unsqueeze(1).to_broadcast([P, 32, 128]), in1=colS, op=ALU.mult), reads=[Tc, Ttb], writes=[Tbig["m2"]])
                dve(lambda e: e.tensor_tensor(out=ob[:], in0=m1, in1=m2, op=ALU.add), [Tbig["m1"], Tbig["m2"]], [To])
                kb.dma('sp', self.s5m[l, 3 + k], ob[:].rearrange("p a b -> p (a b)"), reads=[To], writes=[self.Ts5m[l][3 + k]], owner=self.Ts5m[l][3 + k])
            kb.full_barrier()
            for e_ in ('sp', 'dve', 'act', 'pool', 'pe'):
                kb.barrier(Toutb + [Tsp], eng=e_)

    def main(self, es):
        nc = self.nc; kb = self.kb
        self.xres = self.sb(es, "xres", [P, DC, L], F32)
        self.Tx = [[Tok("x%d_%d" % (b, c)) for c in range(DC)] for b in range(4)]
        self.hbuf = self.sb(es, "hbuf", [P, DC, TT], BF16)
        self.Th = [Tok("h0"), Tok("h1")]
        self.ycat = self.sb(es, "ycat", [P, DC, TT], BF16)
        self.Tyc = [[Tok("yc%d_%d" % (hb, c)) for c in range(DC)] for hb in range(2)]
        self.cur_slot = self.sb(es, "WinGslot", [P, DC, 1552], BF16)
        self.carry = self.sb(es, "carry", [P, G], F32); self.Tcarry = Tok("carry")
        self.gst_f = self.sb(es, "gst_f", [P, 2, 128], F32)
        self.gst_b = [self.sb(es, "gst_b%d" % i, [P, 2, 128], BF16) for i in range(2)]
        self.Tgf = Tok("gst_f"); self.Tgb = [Tok("gst_b0"), Tok("gst_b1")]
        for s in range(self.nseq):
            kb.new_epoch()
            for b in range(4):
                kb.dma('sp', self.xres[:, :, b * 512:(b + 1) * 512], self.xT[s, :, :, b * 512:(b + 1) * 512], writes=self.Tx[b], owner=self.Tx[b][0])
            self.stopped = (self.stop_after == 'load')
            for li, l in enumerate(self.layers):
                if li + 1 < len(self.layers): self.next_layer = self.layers[li + 1]
                elif s + 1 < self.nseq: self.next_layer = self.layers[0]
                else: self.next_layer = None
                for tile in range(2):
                    if not self.stopped: self.mixer(l, tile)
                for tile in range(2):
                    if not self.stopped: self.ffn(l, tile)
            if 'xres' in self.dbg_out:
                kb.dma('sp', self.dbg_out['xres'], self.xres[:], reads=[t for b in range(4) for t in self.Tx[b]], owner=self.Tx[0][0])
            if self.final:
                self.final_norm(s)
            else:
                for b in range(4):
                    kb.dma('sp', self.yT[s, :, :, b * 512:(b + 1) * 512], self.xres[:, :, b * 512:(b + 1) * 512], reads=self.Tx[b], owner=self.Tx[b][1])
        allt = [t for b in range(4) for t in self.Tx[b]] + getattr(self, 'Tout', [])
        kb.full_barrier()
        for e_ in ('sp', 'act', 'pool'):
            kb.barrier(allt, eng=e_)

    def rmsnorm_block(self, es_scr, blk, col0, dst_fn, Tdst):
        kb = self.kb
        sq, Tsq, rs, Trs = es_scr
        t0 = blk * 512
        Tx = self.Tx[blk]
        kb.op('act', lambda e: e.activation(out=sq[:], in_=self.xres[:, :, t0:t0 + 512], func=AF.Square), reads=Tx, writes=[Tsq])
        ps, Tp = self.psum_next()
        for c in range(DC):
            kb.op('pe', lambda e: e.matmul(ps[:], self.ones_bf[:], sq[:, c, :], start=(c == 0), stop=(c == DC - 1)), reads=[Tsq, self.Tconst], writes=[Tp])
        kb.op('act', lambda e: e.activation(out=rs[:], in_=ps[:], func=AF.Sqrt, scale=1.0 / D, bias=self.epsc[:, 0:1]), reads=[Tp, self.Tconst], writes=[Trs])
        kb.op('dve', lambda e: e.reciprocal(rs[:], rs[:]), reads=[Trs], writes=[Trs])
        for c in range(DC):
            kb.op('dve', lambda e: e.scalar_tensor_tensor(out=dst_fn(c), in0=self.xres[:, c, t0:t0 + 512], scalar=self.cols[:, col0 + c:col0 + c + 1], in1=rs[:], op0=ALU.mult, op1=ALU.mult), reads=[Tx[c], Trs, self.Tconst], writes=Tdst)

    def norm_scratch(self, es):
        sq = self.sb(es, "nsq", [P, DC, 512], BF16); rs = self.sb(es, "nrs", [P, 512], F32)
        return (sq, Tok("nsq"), rs, Tok("nrs"))

    def mixer(self, l, tile):
        nc = self.nc; kb = self.kb
        with contextlib.ExitStack() as es:
            self.wing_slot(es)
            if getattr(self, 'wing_valid', None) != l:
                self.load_wing(l)
            scr = self.norm_scratch(es)
            for hb in range(2):
                self.rmsnorm_block(scr, tile * 2 + hb, C_NMIX + l * 8, lambda c: self.hbuf[:, c, hb * 512:(hb + 1) * 512], [self.Th[hb]])
            kb.full_barrier()
        if self.stop_after == 'norm': self.stopped = True; return
        with contextlib.ExitStack() as esm:
            self.WSs = self.sb(esm, "WSs", [P, 6144], BF16)
            with contextlib.ExitStack() as es:
                self.gla(es, l, tile)
                kb.full_barrier()
            with contextlib.ExitStack() as es:
                self.s5(es, l, tile)
                kb.full_barrier()
        if self.stop_after == 's5': self.stopped = True; return
        with contextlib.ExitStack() as es:
            self.wout(es, l, tile)
            kb.full_barrier()
        if self.stop_after == 'wout': self.stopped = True; return

    def gla(self, es, l, tile):
        nc = self.nc; kb = self.kb
        sb = lambda n, s, d: self.sb(es, "g_" + n, s, d)
        WinG = self.wing_slot(es); TW = self.ptok("WinG")
        if getattr(self, 'wing_valid', None) != l:
            self.load_wing(l)
        WSs = self.WSs
        kb.dma('pool', WSs[:, 0:4096].rearrange("p (c n) -> p c n", c=DC), self.w_in[l, :, :, 0:512], writes=[self.ptok("WinS")])
        kb.dma('pool', WSs[:, 4096:6144].rearrange("p (c n) -> p c n", c=4), self.w_glu[l], writes=[self.ptok("Wglu")])
        glx = sb("glx", [33, TT], BF16); Tglx = Tok("glx")
        Epos = sb("Epos", [P, 2, TT], F32); Eneg = sb("Eneg", [P, 2, TT], F32)
        TEp = [Tok("Ep%d" % i) for i in range(8)]
        qd = sb("qd", [P, 2, TT], BF16); ki = sb("ki", [P, 2, TT], BF16); Tqk = [Tok("qk0"), Tok("qk1")]
        gs = sb("gs", [P, 4, TT], BF16); Tgs = [Tok("gs0"), Tok("gs1")]
        vt = sb("vt", [P, 8, 512], BF16); Tvt = [Tok("vt%d" % i) for i in range(8)]
        ke = sb("ke", [P, 8, 256], BF16); Tke = [Tok("ke%d" % i) for i in range(8)]
        Erc = [sb("Erc%d" % i, [P, 256], F32) for i in range(2)]; TErc = [Tok("Erc0"), Tok("Erc1")]
        e1 = [sb("e1_%d" % i, [P, 256], F32) for i in range(2)]; Te1 = [Tok("e1_0"), Tok("e1_1")]
        nl = [sb("nl_%d" % i, [P, 256], F32) for i in range(2)]; Tnl = [Tok("nl0"), Tok("nl1")]
        sT = [sb("sT%d" % i, [P, 4, 128], BF16) for i in range(2)]; TsT = [Tok("sT0"), Tok("sT1")]
        on = [sb("on%d" % i, [P, 4, 128], BF16) for i in range(2)]; Ton = [Tok("on0"), Tok("on1")]
        junk = sb("junk", [P, 128], BF16); Tjunk = Tok("junk")
        ss = [sb("ss%d" % i, [P, 4], F32) for i in range(2)]; Tss = [Tok("ss0"), Tok("ss1")]
        Tc = self.Tconst
        if tile == 0:
            kb.op('dve', lambda e: e.memset(self.gst_f[:], 0.0), writes=[self.Tgf])
            kb.op('dve', lambda e: e.memset(self.gst_b[0][:], 0.0), writes=[self.Tgb[0]])
        kb.op('dve', lambda e: e.memset(glx[:], 0.0), writes=[Tglx])
        kb.op('dve', lambda e: e.memset(glx[32:33, :], 1.0), writes=[Tglx])
        for hb in range(2):
            ps, Tp = self.psum_next()
            for c in range(DC):
                kb.op('pe', lambda e: e.matmul(ps[0:16, :], WinG[:, c, 1536:1552], self.hbuf[:, c, hb * 512:(hb + 1) * 512], start=(c == 0), stop=(c == DC - 1)), reads=[TW, self.Th[hb]], writes=[Tp])
            kb.op('act', lambda e: e.activation(out=glx[0:16, hb * 512:(hb + 1) * 512], in_=ps[0:16, :], func=AF.Copy), reads=[Tp], writes=[Tglx])
        for st in range(8):
            tk = slice(st * 128, (st + 1) * 128); hb = st // 4; r = st % 2
            ps, Tp = self.psum_next()
            kb.op('pe', lambda e: e.matmul(ps[:, 0:256], glx[0:33, tk], self.wgx[0:33, l, :], start=True, stop=True), reads=[Tglx, self.Twgx], writes=[Tp])
            kb.op('act', lambda e: e.activation(out=e1[r][:], in_=ps[:, 0:256], func=AF.Exp, scale=-1.0), reads=[Tp], writes=[Te1[r]])
            ps, Tp = self.psum_next()
            for c in range(DC):
                kb.op('pe', lambda e: e.matmul(ps[:], self.hbuf[:, c, tk], WinG[:, c, 512:1024], start=(c == 0), stop=(c == DC - 1)), reads=[TW, self.Th[hb]], writes=[Tp])
            kb.op('act', lambda e: e.activation(out=vt[:, st, :], in_=ps[:], func=AF.Copy), reads=[Tp], writes=[Tvt[st]])
            kb.op('act', lambda e: e.activation(out=nl[r][:], in_=e1[r][:], func=AF.Ln, bias=self.epsc[:, 1:2]), reads=[Te1[r], Tc], writes=[Tnl[r]])
            ps, Tp = self.psum_next()
            for c in range(2):
                kb.op('pe', lambda e: e.matmul(ps[:, c * 128:(c + 1) * 128], nl[r][:, c * 128:(c + 1) * 128], self.cmask[:], start=True, stop=True), reads=[Tnl[r], Tc], writes=[Tp])
            kb.op('act', lambda e: e.activation(out=Epos[:, :, tk], in_=ps[:, 0:256].rearrange("p (c t) -> p c t", c=2), func=AF.Exp, scale=-1.0 / 16), reads=[Tp], writes=[TEp[st]])
            kb.op('act', lambda e: e.activation(out=Eneg[:, :, tk], in_=ps[:, 0:256].rearrange("p (c t) -> p c t", c=2), func=AF.Exp, scale=1.0 / 16), reads=[Tp], writes=[TEp[st]])
            ps, Tp = self.psum_next()
            kb.op('pe', lambda e: e.matmul(ps[:, 0:256], self.triR[:], nl[r][:], start=True, stop=True), reads=[Tnl[r], Tc], writes=[Tp])
            kb.op('act', lambda e: e.activation(out=Erc[r][:], in_=ps[:, 0:256], func=AF.Exp, scale=-1.0 / 16), reads=[Tp], writes=[TErc[r]])
            ps, Tp = self.psum_next()
            for c in range(DC):
                kb.op('pe', lambda e: e.matmul(ps[:, 0:256], self.hbuf[:, c, tk], WinG[:, c, 256:512], start=(c == 0), stop=(c == DC - 1)), reads=[TW, self.Th[hb]], writes=[Tp])
            kb.op('dve', lambda e: e.tensor_tensor(out=ke[:, st, :], in0=ps[:, 0:256], in1=Erc[r][:], op=ALU.mult), reads=[Tp, TErc[r]], writes=[Tke[st]])
        for hb in range(2):
            hs = slice(hb * 512, (hb + 1) * 512)
            TE = TEp[hb * 4:(hb + 1) * 4]
            for c in range(2):
                ps, Tp = self.psum_next()
                for kc in range(DC):
                    kb.op('pe', lambda e: e.matmul(ps[:], WinG[:, kc, c * 128:(c + 1) * 128], self.hbuf[:, kc, hs], start=(kc == 0), stop=(kc == DC - 1)), reads=[TW, self.Th[hb]], writes=[Tp])
                kb.op('dve', lambda e: e.scalar_tensor_tensor(out=qd[:, c, hs], in0=ps[:], scalar=0.125, in1=Epos[:, c, hs], op0=ALU.mult, op1=ALU.mult), reads=[Tp] + TE, writes=[Tqk[hb]])
                ps, Tp = self.psum_next()
                for kc in range(DC):
                    kb.op('pe', lambda e: e.matmul(ps[:], WinG[:, kc, 256 + c * 128:256 + (c + 1) * 128], self.hbuf[:, kc, hs], start=(kc == 0), stop=(kc == DC - 1)), reads=[TW, self.Th[hb]], writes=[Tp])
                kb.op('dve', lambda e: e.tensor_tensor(out=ki[:, c, hs], in0=ps[:], in1=Eneg[:, c, hs], op=ALU.mult), reads=[Tp] + TE, writes=[Tqk[hb]])
            for c in range(4):
                ps, Tp = self.psum_next()
                for kc in range(DC):
                    kb.op('pe', lambda e: e.matmul(ps[:], WinG[:, kc, 1024 + c * 128:1024 + (c + 1) * 128], self.hbuf[:, kc, hs], start=(kc == 0), stop=(kc == DC - 1)), reads=[TW, self.Th[hb]], writes=[Tp])
                kb.op('act', lambda e: e.activation(out=gs[:, c, hs], in_=ps[:], func=AF.Silu), reads=[Tp], writes=[Tgs[hb]])
        pending = None
        for st in range(8):
            tk = slice(st * 128, (st + 1) * 128); hb = st // 4; r = st % 2
            psb = [self.psum_next(), self.psum_next()]
            for hd in range(4):
                c = hd // 2; par = hd % 2; pr = slice(par * 64, par * 64 + 64)
                ps, Tp = psb[par]
                kb.op('pe', lambda e: e.matmul(ps[:, c * 128:(c + 1) * 128], ki[pr, c, tk], qd[pr, c, tk], start=True, stop=True), reads=[Tqk[hb]], writes=[Tp])
            for par in range(2):
                ps, Tp = psb[par]
                dst = sT[r][:].rearrange("p (c two) t -> p two c t", two=2)[:, par, :, :]
                kb.op('dve', lambda e: e.tensor_tensor(out=dst, in0=ps[:, 0:256].rearrange("p (a b) -> p a b", b=128), in1=self.cmask[:].unsqueeze(1).to_broadcast([P, 2, 128]), op=ALU.mult), reads=[Tp, Tc], writes=[TsT[r]])
            def upd_state(half, dst):
                rows = slice(half * 64, half * 64 + 64)
                psu, Tpu = self.psum_next()
                for hd in range(4):
                    c = hd // 2; pr = slice((hd % 2) * 64, (hd % 2) * 64 + 64)
                    kb.op('pe', lambda e: e.matmul(psu[pr, c * 128:(c + 1) * 128], ke[rows, st, hd * 64:(hd + 1) * 64], vt[rows, st, hd * 128:(hd + 1) * 128], start=True, stop=True), reads=[Tke[st], Tvt[st]], writes=[Tpu])
                tend = st * 128 + half * 64 + 63
                for c in range(2):
                    kb.op('dve', lambda e: e.scalar_tensor_tensor(out=self.gst_f[:, c, :], in0=self.gst_f[:, c, :], scalar=Epos[:, c, tend:tend + 1], in1=psu[:, c * 128:(c + 1) * 128], op0=ALU.mult, op1=ALU.add), reads=[self.Tgf, TEp[st], Tpu], writes=[self.Tgf])
                kb.op('act', lambda e: e.activation(out=self.gst_b[dst][:].rearrange("p a b -> p (a b)"), in_=self.gst_f[:].rearrange("p a b -> p (a b)"), func=AF.Copy), reads=[self.Tgf], writes=[self.Tgb[dst]])
            upd_state(0, 1)
            pob = [self.psum_next(), self.psum_next()]
            for hd in range(4):
                c = hd // 2; par = hd % 2; pr = slice(par * 64, par * 64 + 64)
                pso, Tpo = pob[par]
                oc = slice(c * 128, (c + 1) * 128)
                kb.op('pe', lambda e: e.matmul(pso[:, oc], sT[r][:, hd, :], vt[:, st, hd * 128:(hd + 1) * 128], start=True, stop=False, skip_group_check=True), reads=[TsT[r], Tvt[st]], writes=[Tpo])
                kb.op('pe', lambda e: e.matmul(pso[0:64, oc], qd[pr, c, st * 128:st * 128 + 64], self.gst_b[0][pr, c, :], start=False, stop=False, skip_group_check=True), reads=[Tqk[hb], self.Tgb[0]], writes=[Tpo])
                kb.op('pe', lambda e: e.matmul(pso[64:128, oc], qd[pr, c, st * 128 + 64:st * 128 + 128], self.gst_b[1][pr, c, :], start=False, stop=True, skip_group_check=True), reads=[Tqk[hb], self.Tgb[1]], writes=[Tpo])
            upd_state(1, 0)
            for hd in range(4):
                c = hd // 2; par = hd % 2; pso, Tpo = pob[par]
                kb.op('act', lambda e: e.activation(out=junk[:], in_=pso[:, c * 128:(c + 1) * 128], func=AF.Square, accum_out=ss[r][:, hd:hd + 1]), reads=[Tpo], writes=[Tjunk, Tss[r]])
            kb.op('act', lambda e: e.activation(out=ss[r][:], in_=ss[r][:], func=AF.Sqrt, scale=1.0 / 128, bias=self.epsc[:, 0:1]), reads=[Tss[r], Tc], writes=[Tss[r]])
            kb.op('dve', lambda e: e.reciprocal(ss[r][:], ss[r][:]), reads=[Tss[r]], writes=[Tss[r]])
            for par in range(2):
                pso, Tpo = pob[par]
                dst = on[r][:].rearrange("p (c two) t -> p two c t", two=2)[:, par, :, :]
                sc = ss[r][:].rearrange("p (c two) -> p two c", two=2)[:, par, :].unsqueeze(2).to_broadcast([P, 2, 128])
                kb.op('dve', lambda e: e.tensor_tensor(out=dst, in0=pso[:, 0:256].rearrange("p (a b) -> p a b", b=128), in1=sc, op=ALU.mult), reads=[Tpo, Tss[r]], writes=[Ton[r]])
            def tail(st=st, tk=tk, hb=hb, r=r):
                pst, Tpt = self.psum_next()
                pstb = pst[:].bitcast(BF16)
                for hd in range(4):
                    kb.op('pe', lambda e: e.transpose(pstb[:, hd * 128:(hd + 1) * 128], on[r][:, hd, :], self.ident_bf[:]), reads=[Ton[r], Tc], writes=[Tpt])
                for hd in range(4):
                    kb.op('dve', lambda e: e.scalar_tensor_tensor(out=self.ycat[:, 4 + hd, tk], in0=pstb[:, hd * 128:(hd + 1) * 128], scalar=self.cols[:, C_GLAN + l * 4 + hd:C_GLAN + l * 4 + hd + 1], in1=gs[:, hd, tk], op0=ALU.mult, op1=ALU.mult), reads=[Tpt, Tc, Tgs[hb]], writes=[self.Tyc[hb][4 + hd]])
            if pending is not None:
                pending()
            pending = tail
        pending()

    def s5(self, es, l, tile):
        nc = self.nc; kb = self.kb
        sb = lambda n, s, d: self.sb(es, "s_" + n, s, d)
        Tc = self.Tconst
        slot = self.cur_slot
        self.wing_valid = None
        flat = slot[:].rearrange("p a b -> p (a b)")
        WSs = self.WSs
        WinS = WSs[:, 0:4096].rearrange("p (c n) -> p c n", c=DC); TW = self.ptok("WinS")
        Wglu = WSs[:, 4096:6144].rearrange("p (c n) -> p c n", c=4); TWg = self.ptok("Wglu")
        mats = [sb("mat%d" % i, [P, G, 128], BF16) for i in range(3)]; Tm = [self.ptok("mat%d" % i) for i in range(3)]
        for i in range(3):
            kb.dma('sp', mats[i][:].rearrange("p a b -> p (a b)"), self.s5m[l, i], reads=[self.Ts5m[l][i]], writes=[Tm[i]], owner=Tm[i])
        WB, Toep, WC = mats
        ring = [sb("ring%d" % i, [P, G, 128], BF16) for i in range(2)]; Tring = [self.ptok("ring0"), self.ptok("ring1")]
        bufA = sb("bufA", [P, 4, TT], BF16); TA = [Tok("bufA%d" % i) for i in range(4)]
        UT = sb("UT", [P, G, 128], BF16); TUT = [Tok("UT%d" % i) for i in range(8)]
        Sf = flat[:, 0:8256].bitcast(F32).rearrange("p (g j) -> p g j", j=129)
        Sb_ = flat[:, 8256:12384].rearrange("p (g j) -> p g j", j=129)
        TSf = [Tok("Sf%d" % i) for i in range(8)]; TSb = [Tok("Sb%d" % i) for i in range(8)]
        yg = ring[0]; Tyg = [Tok("yg%d" % i) for i in range(4)]
        y2 = self.hbuf
        if tile == 0:
            kb.op('dve', lambda e: e.memset(self.carry[:], 0.0), writes=[self.Tcarry])
        for cc in range(4):
            for hb in range(2):
                ps, Tp = self.psum_next()
                for c in range(DC):
                    kb.op('pe', lambda e: e.matmul(ps[:], WinS[:, c, cc * 128:(cc + 1) * 128], self.hbuf[:, c, hb * 512:(hb + 1) * 512], start=(c == 0), stop=(c == DC - 1)), reads=[TW, self.Th[hb]], writes=[Tp])
                eng = self.evac_eng()
                if eng == 'act':
                    kb.op('act', lambda e: e.activation(out=bufA[:, cc, hb * 512:(hb + 1) * 512], in_=ps[:], func=AF.Copy), reads=[Tp], writes=[TA[cc]])
                else:
                    kb.op('dve', lambda e: e.tensor_copy(bufA[:, cc, hb * 512:(hb + 1) * 512], ps[:]), reads=[Tp], writes=[TA[cc]])
        U8 = ring[1][:].rearrange("p g (s h) -> p g s h", s=8)
        for s_ in range(8):
            ps, Tp = self.psum_next()
            psb = ps[:].bitcast(BF16)
            for cc in range(4):
                mv = bufA[:, cc, :].rearrange("p (j s) -> p s j", s=8)[:, s_, :]
                kb.op('pe', lambda e: e.transpose(psb[:, cc * 128:(cc + 1) * 128], mv, self.ident_bf[:]), reads=[TA[cc], Tc], writes=[Tp])
            eng = self.evac_eng()
            if eng == 'act':
                kb.op('act', lambda e: e.activation(out=U8[:, :, s_, :], in_=psb[:, 0:512].rearrange("p (g h) -> p g h", h=16), func=AF.Copy), reads=[Tp], writes=[Tring[1]])
            else:
                kb.op('dve', lambda e: e.tensor_copy(U8[:, :, s_, :], psb[:, 0:512].rearrange("p (g h) -> p g h", h=16)), reads=[Tp], writes=[Tring[1]])
        for b in range(8):
            ps, Tp = self.psum_next()
            psb = ps[:].bitcast(BF16)
            for gi in range(4):
                g = 4 * b + gi
                kb.op('pe', lambda e: e.transpose(psb[:, gi * 128:(gi + 1) * 128], ring[1][:, g, :], self.ident_bf[:]), reads=[Tring[1], Tc], writes=[Tp])
            eng = self.evac_eng()
            dst = UT[:, 4 * b:4 * b + 4, :].rearrange("p a b -> p (a b)")
            if eng == 'act':
                kb.op('act', lambda e: e.activation(out=dst, in_=psb[:, 0:512], func=AF.Copy), reads=[Tp], writes=[TUT[b]])
            else:
                kb.op('dve', lambda e: e.tensor_copy(dst, psb[:, 0:512]), reads=[Tp], writes=[TUT[b]])
        for b in range(8):
            ps, Tp = self.psum_next()
            for gi in range(4):
                g = 4 * b + gi
                kb.op('pe', lambda e: e.matmul(ps[:, gi * 128:(gi + 1) * 128], WB[:, g, :], UT[:, g, :], start=True, stop=True), reads=[Tm[0], TUT[b]], writes=[Tp])
            g4 = slice(4 * b, 4 * b + 4)
            kb.op('dve', lambda e: e.tensor_copy(Sf[:, g4, 1:129], ps[:].rearrange("p (a b) -> p a b", b=128)), reads=[Tp], writes=[TSf[b]])
            kb.op('dve', lambda e: e.tensor_copy(Sf[:, g4, 0:1], self.carry[:, g4].unsqueeze(2)), reads=[self.Tcarry], writes=[TSf[b]])
            kb.op('act', lambda e: e.activation(out=Sb_[:, g4, :], in_=Sf[:, g4, :], func=AF.Copy), reads=[TSf[b]], writes=[TSb[b]])
        for k in range(8):
            d = 1 << k; n = 129 - d
            rg = ring[k % 2]; Tr = Tring[k % 2]
            kb.dma('sp', rg[:].rearrange("p a b -> p (a b)"), self.s5m[l, 3 + k], reads=[self.Ts5m[l][3 + k]], writes=[Tr], owner=Tr)
            for b in range(8):
                ps, Tp = self.psum_next()
                g4 = slice(4 * b, 4 * b + 4)
                for gi in range(4):
                    g = 4 * b + gi
                    kb.op('pe', lambda e: e.matmul(ps[:, gi * 128:gi * 128 + n], rg[:, g, :], Sb_[:, g, 0:n], start=True, stop=True), reads=[Tr, TSb[b]], writes=[Tp])
                kb.op('dve', lambda e: e.tensor_tensor(out=Sf[:, g4, d:129], in0=Sf[:, g4, d:129], in1=ps[:].rearrange("p (a b) -> p a b", b=128)[:, :, 0:n], op=ALU.add), reads=[Tp, TSf[b]], writes=[TSf[b]])
                kb.op('act', lambda e: e.activation(out=Sb_[:, g4, d:129], in_=Sf[:, g4, d:129], func=AF.Copy), reads=[TSf[b]], writes=[TSb[b]])
        kb.op('dve', lambda e: e.tensor_copy(self.carry[:].unsqueeze(2), Sf[:, :, 128:129]), reads=TSf, writes=[self.Tcarry])
        for b in range(8):
            ps, Tp = self.psum_next()
            for gi in range(4):
                g = 4 * b + gi
                kb.op('pe', lambda e: e.matmul(ps[:, gi * 128:(gi + 1) * 128], Toep[:, g, :], UT[:, g, :], start=True, stop=False), reads=[Tm[1], TUT[b]], writes=[Tp])
                kb.op('pe', lambda e: e.matmul(ps[:, gi * 128:(gi + 1) * 128], WC[:, g, :], Sb_[:, g, 0:128], start=False, stop=True), reads=[Tm[2], TSb[b]], writes=[Tp])
            kb.op('act', lambda e: e.activation(out=yg[:, 4 * b:4 * b + 4, :].rearrange("p a b -> p (a b)"), in_=ps[:], func=AF.Gelu_apprx_tanh), reads=[Tp], writes=[Tyg[b // 2], Tring[0]])
        Y8 = UT[:].rearrange("p g j -> p (g j)").rearrange("p (t c) -> p t c", t=8)
        for b in range(8):
            ps, Tp = self.psum_next()
            psb = ps[:].bitcast(BF16)
            for gi in range(4):
                g = 4 * b + gi
                kb.op('pe', lambda e: e.transpose(psb[:, gi * 128:(gi + 1) * 128], yg[:, g, :], self.ident_bf[:]), reads=[Tyg[b // 2], Tc], writes=[Tp])
            dst = Y8[:, :, 64 * b:64 * b + 64].rearrange("p t (g h) -> p g t h", g=4)
            src = psb[:, 0:512].rearrange("p (g t h) -> p g t h", g=4, t=8)
            eng = self.evac_eng()
            if eng == 'act':
                kb.op('act', lambda e: e.activation(out=dst, in_=src, func=AF.Copy), reads=[Tp], writes=TUT)
            else:
                kb.op('dve', lambda e: e.tensor_copy(dst, src), reads=[Tp], writes=TUT)
        for cc in range(4):
            for th in range(2):
                ps, Tp = self.psum_next()
                psb = ps[:].bitcast(BF16)
                for ti in range(4):
                    t0 = th * 4 + ti
                    kb.op('pe', lambda e: e.transpose(psb[:, ti * 128:(ti + 1) * 128], Y8[:, t0, cc * 128:(cc + 1) * 128], self.ident_bf[:]), reads=TUT + [Tc], writes=[Tp])
                dst = bufA[:, cc, :].rearrange("p (j s) -> p s j", s=8)[:, th * 4:th * 4 + 4, :]
                src = psb[:, 0:512].rearrange("p (a b) -> p a b", b=128)
                eng = self.evac_eng()
                if eng == 'act':
                    kb.op('act', lambda e: e.activation(out=dst, in_=src, func=AF.Copy), reads=[Tp], writes=[TA[cc]])
                else:
                    kb.op('dve', lambda e: e.tensor_copy(dst, src), reads=[Tp], writes=[TA[cc]])
        if 'yg' in self.dbg_out and tile == 0:
            kb.dma('pool', self.dbg_out['yg'], bufA[:], reads=TA, owner=TA[0])
        sq = UT[:, 0:16, :].rearrange("p (a b) c -> p a (b c)", a=4); Tsq = TUT[0:4]
        rs = UT[:, 16:24, :].rearrange("p a b -> p (a b)").bitcast(F32); Trs = TUT[4:6]
        gt = [UT[:, 24:28, :].rearrange("p a b -> p (a b)"), UT[:, 28:32, :].rearrange("p a b -> p (a b)")]; Tgt = [TUT[6], TUT[7]]
        for hb in range(2):
            hs = slice(hb * 512, (hb + 1) * 512)
            for co in range(4):
                ps, Tp = self.psum_next()
                for ci in range(4):
                    kb.op('pe', lambda e: e.matmul(ps[:], Wglu[:, ci, co * 128:(co + 1) * 128], bufA[:, ci, hs], start=(ci == 0), stop=(ci == 3)), reads=[TWg] + TA, writes=[Tp])
                r = co % 2
                kb.op('act', lambda e: e.activation(out=gt[r], in_=ps[:], func=AF.Sigmoid, bias=self.cols[:, C_BGLU + l * 4 + co:C_BGLU + l * 4 + co + 1]), reads=[Tp, Tc], writes=[Tgt[r]])
                kb.op('dve', lambda e: e.tensor_tensor(out=y2[:, co, hs], in0=bufA[:, co, hs], in1=gt[r], op=ALU.mult), reads=[TA[co], Tgt[r]], writes=[self.Th[hb]])
        for hb in range(2):
            hs = slice(hb * 512, (hb + 1) * 512)
            kb.op('act', lambda e: e.activation(out=sq, in_=y2[:, 0:4, hs], func=AF.Square), reads=[self.Th[hb]], writes=Tsq)
            ps, Tp = self.psum_next()
            for c in range(4):
                kb.op('pe', lambda e: e.matmul(ps[:], self.ones_bf[:], sq[:, c, :], start=(c == 0), stop=(c == 3)), reads=Tsq + [Tc], writes=[Tp])
            kb.op('act', lambda e: e.activation(out=rs, in_=ps[:], func=AF.Sqrt, scale=1.0 / 512, bias=self.epsc[:, 0:1]), reads=[Tp, Tc], writes=Trs)
            kb.op('dve', lambda e: e.reciprocal(rs, rs), reads=Trs, writes=Trs)
            for c in range(4):
                kb.op('dve', lambda e: e.scalar_tensor_tensor(out=self.ycat[:, c, hs], in0=y2[:, c, hs], scalar=self.cols[:, C_S5N + l * 4 + c:C_S5N + l * 4 + c + 1], in1=rs, op0=ALU.mult, op1=ALU.mult), reads=[self.Th[hb], Tc] + Trs, writes=[self.Tyc[hb][c]])

    def wout(self, es, l, tile):
        kb = self.kb
        self.wing_slot(es)
        Wout = self.sb(es, "Wout", [P, DC, DC, 128], BF16)
        TWo = [self.ptok("Wout%d" % c) for c in range(DC)]
        for co in range(DC):
            kb.dma('pool', Wout[:, co, :, :], self.w_out[l, co], writes=[TWo[co]])
        if tile == 0:
            self.load_wing(l)
        if 'ycat' in self.dbg_out and tile == 0:
            dtmp = self.sb(es, "dtmp2", [P, DC, TT], F32); Td = Tok("dtmp2")
            kb.op('dve', lambda e: e.tensor_copy(dtmp[:], self.ycat[:]), reads=[t for hb in range(2) for t in self.Tyc[hb]], writes=[Td])
            kb.dma('sp', self.dbg_out['ycat'], dtmp[:], reads=[Td])
            kb.barrier([Td], eng='dve')
        for hb in range(2):
            blk = tile * 2 + hb; t0 = blk * 512
            for co in range(DC):
                ps, Tp = self.psum_next()
                for ci in range(DC):
                    kb.op('pe', lambda e: e.matmul(ps[:], Wout[:, co, ci, :], self.ycat[:, ci, hb * 512:(hb + 1) * 512], start=(ci == 0), stop=(ci == DC - 1)), reads=[TWo[co], self.Tyc[hb][ci]], writes=[Tp])
                kb.op('dve', lambda e: e.tensor_tensor(out=self.xres[:, co, t0:t0 + 512], in0=ps[:], in1=self.xres[:, co, t0:t0 + 512], op=ALU.add), reads=[Tp, self.Tx[blk][co]], writes=[self.Tx[blk][co]])

    def ffn(self, l, tile):
        kb = self.kb
        NR1 = 4; NR2 = 2
        with contextlib.ExitStack() as esf:
            W1 = [self.sb(esf, "f_W1_%d" % i, [P, DC, 2, 128], BF16) for i in range(NR1)]
            TW1 = [self.ptok("W1_%d" % i) for i in range(NR1)]
            for f in range(NR1):
                kb.dma('pool', W1[f][:], self.w_f1[l, f], writes=[TW1[f]])
            with contextlib.ExitStack() as es:
                scr = self.norm_scratch(es)
                for hb in range(2):
                    self.rmsnorm_block(scr, tile * 2 + hb, C_NFFN + l * 8, lambda c: self.hbuf[:, c, hb * 512:(hb + 1) * 512], [self.Th[hb]])
                kb.full_barrier()
            with contextlib.ExitStack() as es:
                sb = lambda n, s, d: self.sb(es, "f_" + n, s, d)
                if tile == 1 and self.next_layer is not None:
                    self.load_wing(self.next_layer)
                act = sb("act", [P, FC, TT], BF16); Tact = [[Tok("act%d_%d" % (f, hb)) for hb in range(2)] for f in range(FC)]
                W2 = [sb("W2_%d" % i, [P, FC, 128], BF16) for i in range(NR2)]; TW2 = [self.ptok("W2_%d" % i) for i in range(NR2)]
                sg = [sb("sg%d" % i, [P, 512], BF16) for i in range(2)]; Tsg = [Tok("sg0"), Tok("sg1")]
                for f in range(FC):
                    w = W1[f % NR1]; Tw = TW1[f % NR1]
                    if f >= NR1:
                        kb.dma('pool', w[:], self.w_f1[l, f], writes=[Tw])
                    for hb in range(2):
                        hs = slice(hb * 512, (hb + 1) * 512)
                        psg, Tpg = self.psum_next()
                        for c in range(DC):
                            kb.op('pe', lambda e: e.matmul(psg[:], w[:, c, 0, :], self.hbuf[:, c, hs], start=(c == 0), stop=(c == DC - 1)), reads=[Tw, self.Th[hb]], writes=[Tpg])
                        psu, Tpu = self.psum_next()
                        for c in range(DC):
                            kb.op('pe', lambda e: e.matmul(psu[:], w[:, c, 1, :], self.hbuf[:, c, hs], start=(c == 0), stop=(c == DC - 1)), reads=[Tw, self.Th[hb]], writes=[Tpu])
                        r = (2 * f + hb) % 2
                        kb.op('act', lambda e: e.activation(out=sg[r][:], in_=psg[:], func=AF.Silu), reads=[Tpg], writes=[Tsg[r]])
                        kb.op('dve', lambda e: e.tensor_tensor(out=act[:, f, hs], in0=psu[:], in1=sg[r][:], op=ALU.mult), reads=[Tpu, Tsg[r]], writes=[Tact[f][hb]])
                for co in range(DC):
                    w = W2[co % NR2]; Tw = TW2[co % NR2]
                    kb.dma('pool', w[:], self.w_f2[l, co], writes=[Tw])
                    for hb in range(2):
                        blk = tile * 2 + hb; t0 = blk * 512
                        ps, Tp = self.psum_next()
                        for f in range(FC):
                            kb.op('pe', lambda e: e.matmul(ps[:], w[:, f, :], act[:, f, hb * 512:(hb + 1) * 512], start=(f == 0), stop=(f == FC - 1)), reads=[Tw, Tact[f][hb]], writes=[Tp])
                        kb.op('dve', lambda e: e.tensor_tensor(out=self.xres[:, co, t0:t0 + 512], in0=ps[:], in1=self.xres[:, co, t0:t0 + 512], op=ALU.add), reads=[Tp, self.Tx[blk][co]], writes=[self.Tx[blk][co]])
                kb.full_barrier()

    def final_norm(self, s):
        kb = self.kb
        with contextlib.ExitStack() as es:
            self.wing_slot(es)
            scr = self.norm_scratch(es)
            ob = [self.sb(es, "fo%d" % i, [P, DC, 512], F32) for i in range(2)]
            To = [self.ptok("fo0"), self.ptok("fo1")]
            self.Tout = To
            for blk in range(4):
                r = blk % 2
                self.rmsnorm_block(scr, blk, C_NFIN, lambda c: ob[r][:, c, :], [To[r]])
                kb.dma('sp', self.yT[s, :, :, blk * 512:(blk + 1) * 512], ob[r][:], reads=[To[r]], owner=To[r])
            kb.full_barrier()
            for e_ in ('sp', 'act', 'dve', 'pool', 'pe'):
                kb.barrier(To, eng=e_)


def _consts():
    c = np.zeros((P, NCONST), np.float32)
    idx = np.arange(P)
    c[:, K_ID:K_ID + 128] = np.eye(P, dtype=np.float32)
    s = idx[:, None]; t = idx[None, :]
    same = (s // 64) == (t // 64)
    c[:, K_CM:K_CM + 128] = (same & (s <= t)).astype(np.float32)
    c[:, K_TR:K_TR + 128] = (same & (s > t)).astype(np.float32)
    c[:, K_TM:K_TM + 128] = ((t // 16) >= (s // 16)).astype(np.float32)
    c[:, K_SW:K_SW + 128] = (((s % 64) == (t % 64)) & ((s // 64) != (t // 64))).astype(np.float32)
    st = np.zeros((P, 8, 240), np.float32)
    for gl in range(8):
        for h in range(16):
            st[gl * 16 + h, gl, 112 + h] = 1.0
    c[:, K_ST:K_ST + 1920] = st.reshape(P, 1920)
    c[:, K_NV:K_NV + 32] = np.array(NVALS, np.float32)[None, :]
    c[0:64, K_SG] = 1.0; c[64:128, K_SG] = -1.0
    c[0:64, K_SG + 1] = -1.0; c[64:128, K_SG + 1] = 1.0
    return c


def prep_shared(inp):
    f = lambda a: np.ascontiguousarray(np.asarray(a, dtype=np.float32))
    sh = {}
    sh["w_in"] = f(inp["w_in"].reshape(NL, DC, P, DIN).transpose(0, 2, 1, 3))
    sh["w_glu"] = f(inp["s5_w_glu"].reshape(NL, 4, P, 512).transpose(0, 2, 1, 3))
    sh["w_out"] = f(inp["w_out"].reshape(NL, DC, P, DC, 128).transpose(0, 3, 2, 1, 4))
    sh["w_f1"] = f(inp["w_ffn_in"].reshape(NL, DC, P, 2, FC, 128).transpose(0, 4, 2, 1, 3, 5))
    sh["w_f2"] = f(inp["w_ffn_out"].reshape(NL, FC, P, DC, 128).transpose(0, 3, 2, 1, 4))
    cols = np.zeros((P, NCOL), np.float32)
    cols[:, C_NMIX:C_NMIX + 32] = inp["norm_mix"].reshape(NL, DC, P).transpose(2, 0, 1).reshape(P, 32)
    cols[:, C_NFFN:C_NFFN + 32] = inp["norm_ffn"].reshape(NL, DC, P).transpose(2, 0, 1).reshape(P, 32)
    cols[:, C_NFIN:C_NFIN + 8] = inp["norm_final"].reshape(DC, P).T
    cols[:, C_BGLU:C_BGLU + 16] = inp["s5_b_glu"].reshape(NL, 4, P).transpose(2, 0, 1).reshape(P, 16)
    cols[:, C_S5N:C_S5N + 16] = inp["s5_out_norm"].reshape(NL, 4, P).transpose(2, 0, 1).reshape(P, 16)
    cols[:, C_GLAN:C_GLAN + 16] = inp["gla_out_norm"].reshape(NL, 4, P).transpose(2, 0, 1).reshape(P, 16)
    sh["cols"] = cols
    wgx = np.zeros((33, NL, 256), np.float32)
    wgx[0:16] = inp["gla_w_gate"].transpose(1, 0, 2)
    wgx[32] = inp["gla_b_gate"]
    sh["wgx"] = wgx
    s5p = np.zeros((P, NL, NS5), np.float32)
    dup = lambda a: np.concatenate([a, a], 0)
    lre = inp["s5_lam_re"].transpose(2, 0, 1); lim = inp["s5_lam_im"].transpose(2, 0, 1)
    s5p[:, :, 0:32] = dup(lre); s5p[:, :, 32:64] = dup(lim)
    s5p[:, :, 64:96] = np.broadcast_to(inp["s5_log_step"][None], (P, NL, G))
    bre = inp["s5_b_re"].transpose(2, 0, 1, 3).reshape(64, NL, 512); bim = inp["s5_b_im"].transpose(2, 0, 1, 3).reshape(64, NL, 512)
    s5p[:, :, 96:608] = np.concatenate([bre, bim], 0); s5p[:, :, 608:1120] = np.concatenate([bim, bre], 0)
    cre = inp["s5_c_re"].transpose(3, 0, 1, 2).reshape(64, NL, 512); cim = inp["s5_c_im"].transpose(3, 0, 1, 2).reshape(64, NL, 512)
    s5p[:, :, 1120:1632] = dup(cre); s5p[:, :, 1632:2144] = dup(cim)
    sh["s5p"] = s5p
    dd = inp["s5_d"].reshape(NL, G, 16).transpose(2, 0, 1)
    sh["dcol"] = f(np.tile(dd, (8, 1, 1)))
    sh["consts"] = _consts()
    return sh


_PROG = {}


def kernel(**inputs):
    x = np.asarray(inputs["x"], dtype=np.float32)
    B = x.shape[0]; ncores = 8; nseq = B // ncores
    sh = prep_shared(inputs)
    if "full" not in _PROG:
        _PROG["full"] = MK(nseq=nseq)
    mk = _PROG["full"]
    in_maps = []
    for c in range(ncores):
        xs = x[c * nseq:(c + 1) * nseq]
        xT = np.ascontiguousarray(xs.reshape(nseq, L, DC, P).transpose(0, 3, 2, 1))
        m = dict(sh); m["xT"] = xT
        in_maps.append(m)
    res = run_bass_kernel_spmd(mk.nc, in_maps, core_ids=list(range(ncores)))
    out = np.empty((B, L, D), np.float32)
    for c in range(ncores):
        yT = res.results[c]["yT"]
        out[c * nseq:(c + 1) * nseq] = yT.transpose(0, 3, 2, 1).reshape(nseq, L, D)
    return out
```

```python
import contextlib, itertools, math
import numpy as np
import concourse.bass as bass
import concourse.mybir as mybir
from concourse.bass_utils import run_bass_kernel_spmd

F32 = mybir.dt.float32; BF16 = mybir.dt.bfloat16; I32 = mybir.dt.int32
AF = mybir.ActivationFunctionType; ALU = mybir.AluOpType
P = 128; L = 2048; D = 1024; DC = 8; TT = 1024; DIN = 2064; DFF = 2816; FC = 22
NL = 4; G = 32; EPS = 1e-6
TWO_PI = 2.0 * math.pi
C_NMIX = 0; C_NFFN = 32; C_NFIN = 64; C_BGLU = 72; C_S5N = 88; C_GLAN = 104; NCOL = 120
K_ID = 0; K_CM = 128; K_TR = 256; K_TM = 384; K_SW = 512; K_ST = 640; K_NV = 640 + 1920; K_SG = K_NV + 32; NCONST = K_SG + 2
NS5 = 96 + 4 * 512
NVALS = [7, 6, 5, 4, 3, 2, 1, 0, -1, -2, -3, -4, -5, -6, -7, -8, 1, 2, 3, 4, 5, 6, 7, 8, 8, 16, 32, 64, 128, 256, 512, 1024]


class Tok:
    __slots__ = ('name', 'w', 'rd', 'sem', 'semv')

    def __init__(self, name):
        self.name = name; self.w = None; self.rd = {}; self.sem = None; self.semv = 0


class KB:
    def __init__(self, nc, es):
        self.nc = nc; self.es = es
        self.h = {'pe': nc.tensor, 'act': nc.scalar, 'dve': nc.vector, 'pool': nc.gpsimd, 'sp': nc.sync}
        self.sem = {}; self.cnt = {}; self.seen = {}
        self.pesems = set(); self.epoch = 0
        for e in self.h:
            self.seen[e] = {}
        self._new_sems()
        self.nwait = 0; self.nins = 0
        self.dsems = []; self.dlast = {}

    def _new_sems(self):
        for e in self.h:
            self.sem[e] = self.es.enter_context(self.nc.semaphore('s%d_%s' % (self.epoch, e))); self.cnt[e] = 0
        self.pesems.add(self.sem['pe'])
        self.epoch += 1

    def new_epoch(self):
        self.full_barrier()
        self._new_sems()

    def _deps(self, reads, writes):
        need = {}
        for b in reads:
            d = b.w
            if d is not None and need.get(d[0], 0) < d[1]: need[d[0]] = d[1]
        for b in writes:
            d = b.w
            if d is not None and need.get(d[0], 0) < d[1]: need[d[0]] = d[1]
            for k, v in b.rd.items():
                if need.get(k, 0) < v: need[k] = v
        return need

    def _wait(self, eng, need):
        seen = self.seen[eng]
        for k, v in need.items():
            if eng == 'pe' and k in self.pesems: continue
            if seen.get(k, 0) >= v: continue
            self.h[eng].wait_ge(k, v); seen[k] = v; self.nwait += 1

    def op(self, eng, fn, reads=(), writes=()):
        self._wait(eng, self._deps(reads, writes))
        ins = fn(self.h[eng])
        self.cnt[eng] += 1; c = self.cnt[eng]; sm = self.sem[eng]
        ins.then_inc(sm, 1); self.nins += 1
        for b in reads: b.rd[sm] = c
        for b in writes:
            b.w = (sm, c); b.rd = {}
        return ins

    def dma(self, eng, out, in_, reads=(), writes=(), owner=None):
        self._wait(eng, self._deps(reads, writes))
        own = owner or (writes[0] if writes else reads[0])
        if own.sem is None:
            own.sem = self.es.enter_context(self.nc.semaphore('d%d' % len(self.dsems))); own.semv = 0
            self.dsems.append(own.sem)
        ins = self.h[eng].dma_start(out=out, in_=in_)
        own.semv += 16
        ins.then_inc(own.sem, 16); self.nins += 1
        self.dlast[own.sem] = own.semv
        for b in reads: b.rd[own.sem] = own.semv
        for b in writes:
            b.w = (own.sem, own.semv); b.rd = {}
        return ins

    def barrier(self, toks, eng='sp'):
        need = {}
        for b in toks:
            for k, v in ([b.w] if b.w else []) + list(b.rd.items()):
                if need.get(k, 0) < v: need[k] = v
        self._wait(eng, need)

    def full_barrier(self, dmas=True):
        for e in self.h:
            need = {self.sem[k]: self.cnt[k] for k in self.h if self.cnt[k] > 0 and k != e}
            if dmas:
                need.update(self.dlast)
            self._wait(e, need)


class MK:
    def __init__(self, nseq=4, layers=(0, 1, 2, 3), final=True, prologue=True, dbg=None, stop_after=None):
        self.nseq = nseq; self.layers = list(layers); self.final = final; self.dbg = dbg or {}
        self.do_prologue = prologue; self.stop_after = stop_after
        nc = self.nc = bass.Bass("TRN2", target_bir_lowering=False)
        dt = nc.dram_tensor
        self.xT = dt("xT", [nseq, P, DC, L], F32, kind="ExternalInput").ap()
        self.w_in = dt("w_in", [NL, P, DC, DIN], F32, kind="ExternalInput").ap()
        self.w_glu = dt("w_glu", [NL, P, 4, 512], F32, kind="ExternalInput").ap()
        self.w_out = dt("w_out", [NL, DC, P, DC, 128], F32, kind="ExternalInput").ap()
        self.w_f1 = dt("w_f1", [NL, FC, P, DC, 2, 128], F32, kind="ExternalInput").ap()
        self.w_f2 = dt("w_f2", [NL, DC, P, FC, 128], F32, kind="ExternalInput").ap()
        self.cols_d = dt("cols", [P, NCOL], F32, kind="ExternalInput").ap()
        self.wgx_d = dt("wgx", [33, NL, 256], F32, kind="ExternalInput").ap()
        self.s5p_d = dt("s5p", [P, NL, NS5], F32, kind="ExternalInput").ap()
        self.dcol_d = dt("dcol", [P, NL, G], F32, kind="ExternalInput").ap()
        self.consts_d = dt("consts", [P, NCONST], F32, kind="ExternalInput").ap()
        self.yT = dt("yT", [nseq, P, DC, L], F32, kind="ExternalOutput").ap()
        if prologue:
            self.s5m = dt("s5m", [NL, 11, P, 4096], BF16, kind="Internal").ap()
        else:
            self.s5m = dt("s5m", [NL, 11, P, 4096], BF16, kind="ExternalInput").ap()
        self.dbg_out = {}
        for k, shp in self.dbg.items():
            self.dbg_out[k] = dt("dbg_" + k, list(shp), F32, kind="ExternalOutput").ap()
        self.build()

    def sb(self, es, name, shape, dtype):
        self.uid = getattr(self, 'uid', 0) + 1
        return es.enter_context(self.nc.sbuf_tensor("%s_%d" % (name, self.uid), list(shape), dtype))

    def ptok(self, name):
        d = self.__dict__.setdefault('_ptoks', {})
        if name not in d: d[name] = Tok(name)
        return d[name]

    def wing_slot(self, es):
        return self.cur_slot

    def load_wing(self, l):
        TW = self.ptok("WinG")
        for c in range(DC):
            self.kb.dma('pool', self.cur_slot[:, c, :], self.w_in[l, :, c, 512:2064], writes=[TW])
        self.wing_valid = l

    def psum_next(self):
        i = self.ps_i; self.ps_i = (i + 1) % 8
        return self.ps[i], self.Tps[i]

    def evac_eng(self):
        return next(self.ev)

    def build(self):
        nc = self.nc
        with contextlib.ExitStack() as es:
            kb = self.kb = KB(nc, es)
            self.ev = itertools.cycle(['act', 'dve'])
            self.ps = [es.enter_context(nc.psum_tensor("ps%d" % i, [P, 512], F32)) for i in range(8)]
            self.Tps = [Tok("ps%d" % i) for i in range(8)]
            self.ps_i = 0
            self.ident_bf = self.sb(es, "ident_bf", [P, P], BF16)
            self.ones_bf = self.sb(es, "ones_bf", [P, P], BF16)
            self.strips = self.sb(es, "strips", [P, 8, 240], BF16)
            self.cmask = self.sb(es, "cmask", [P, P], F32)
            self.triR = self.sb(es, "triR", [P, P], F32)
            self.cols = self.sb(es, "cols", [P, NCOL], F32)
            self.wgx = self.sb(es, "wgx", [33, NL, 256], BF16)
            self.epsc = self.sb(es, "epsc", [P, 2], F32)
            self.Tconst = Tok("const")
            T = self.Tconst
            with contextlib.ExitStack() as es2:
                cst = self.sb(es2, "cst", [P, NCONST], F32)
                Tc = Tok("cst")
                kb.dma('sp', cst[:], self.consts_d, writes=[Tc])
                kb.dma('sp', self.cols[:], self.cols_d, writes=[T])
                self.Twgx = Tok('wgx'); kb.dma('pool', self.wgx[:], self.wgx_d, writes=[self.Twgx])
                kb.op('dve', lambda e: e.tensor_copy(self.ident_bf[:], cst[:, K_ID:K_ID + 128]), reads=[Tc], writes=[T])
                kb.op('dve', lambda e: e.memset(self.ones_bf[:], 1.0), writes=[T])
                kb.op('dve', lambda e: e.memset(self.epsc[:, 0:1], EPS), writes=[T])
                kb.op('dve', lambda e: e.memset(self.epsc[:, 1:2], 1.0), writes=[T])
                kb.op('dve', lambda e: e.tensor_copy(self.strips[:].rearrange("p a b -> p (a b)"), cst[:, K_ST:K_ST + 1920]), reads=[Tc], writes=[T])
                kb.op('dve', lambda e: e.tensor_copy(self.cmask[:], cst[:, K_CM:K_CM + 128]), reads=[Tc], writes=[T])
                kb.op('dve', lambda e: e.tensor_copy(self.triR[:], cst[:, K_TR:K_TR + 128]), reads=[Tc], writes=[T])
                if self.do_prologue:
                    self.Ts5m = [[self.ptok("s5m%d" % l)] * 11 for l in range(NL)]
                    for l in self.layers:
                        self.prologue(l, cst, Tc)
                else:
                    self.Ts5m = [[self.ptok("s5m%d" % l)] * 11 for l in range(NL)]
                kb.full_barrier()
                allt = [self.Ts5m[l][0] for l in range(NL)]
                for e in ('sp', 'act', 'pool', 'pe', 'dve'):
                    kb.barrier(allt + [Tc, T, self.Twgx], eng=e)
            if self.stop_after != 'prologue':
                self.main(es)

    def prologue(self, l, cst, Tc):
        nc = self.nc; kb = self.kb
        with contextlib.ExitStack() as es:
            sb = lambda n, s, d=F32: self.sb(es, "pl_" + n, s, d)
            sp = sb("sp", [P, NS5]); dc = sb("dc", [P, G])
            Tsp = self.ptok("pl_sp")
            kb.dma('sp', sp[:], self.s5p_d[:, l, :], writes=[Tsp])
            kb.dma('sp', dc[:], self.dcol_d[:, l, :], writes=[Tsp])
            lr2 = sp[:, 0:32]; li2 = sp[:, 32:64]; ls2 = sp[:, 64:96]
            R1 = sp[:, 96:608]; R2 = sp[:, 608:1120]; C1 = sp[:, 1120:1632]; C2 = sp[:, 1632:2144]
            nv = cst[:, K_NV:K_NV + 32]
            sgA = cst[:, K_SG:K_SG + 1]; sgB = cst[:, K_SG + 1:K_SG + 2]
            sm = sb("sm", [P, 16, 32])
            Tsm = Tok("sm")
            tb = {k: sb("tb_" + k, [P, 32, 32]) for k in ("mag", "tr", "tf", "rc", "Cn", "Sn", "SnA", "SnB")}
            ti = sb("ti", [P, 32, 32], I32)
            Ttb = Tok("tb")
            V = lambda e: e
            def dve(fn, r, w): kb.op('dve', fn, reads=r, writes=w)
            def act(fn, r, w): kb.op('act', fn, reads=r, writes=w)
            step = sm[:, 0, :]; lrs = sm[:, 1, :]; lis = sm[:, 2, :]
            act(lambda e: e.activation(out=step, in_=ls2, func=AF.Exp), [Tsp], [Tsm])
            dve(lambda e: e.tensor_tensor(out=lrs, in0=lr2, in1=step, op=ALU.mult), [Tsp, Tsm], [Tsm])
            dve(lambda e: e.tensor_tensor(out=lis, in0=li2, in1=step, op=ALU.mult), [Tsp, Tsm], [Tsm])
            def bc_g(a):
                return a.unsqueeze(1).to_broadcast([P, 32, 32])
            def bc_n(a):
                return a.unsqueeze(2).to_broadcast([P, 32, 32])
            flat = lambda t: t[:].rearrange("p a b -> p (a b)")
            dve(lambda e: e.tensor_tensor(out=tb["mag"][:], in0=bc_g(lrs), in1=bc_n(nv), op=ALU.mult), [Tsm, Tc], [Ttb])
            act(lambda e: e.activation(out=flat(tb["mag"]), in_=flat(tb["mag"]), func=AF.Exp), [Ttb], [Ttb])
            dve(lambda e: e.scalar_tensor_tensor(out=tb["tr"][:], in0=bc_g(lis), scalar=1.0 / TWO_PI, in1=bc_n(nv), op0=ALU.mult, op1=ALU.mult), [Tsm, Tc], [Ttb])
            for name, ph in (("Cn", 0.25), ("Sn", 0.0)):
                dve(lambda e: e.tensor_scalar(out=flat(ti), in0=flat(tb["tr"]), scalar1=ph, scalar2=None, op0=ALU.add), [Ttb], [Ttb])
                dve(lambda e: e.tensor_copy(flat(tb["tf"]), flat(ti)), [Ttb], [Ttb])
                dve(lambda e: e.scalar_tensor_tensor(out=flat(tb["rc"]), in0=flat(tb["tr"]), scalar=ph, in1=flat(tb["tf"]), op0=ALU.add, op1=ALU.subtract), [Ttb], [Ttb])
                act(lambda e: e.activation(out=flat(tb[name]), in_=flat(tb["rc"]), func=AF.Sin, scale=6.283184), [Ttb], [Ttb])
                dve(lambda e: e.tensor_tensor(out=flat(tb[name]), in0=flat(tb[name]), in1=flat(tb["mag"]), op=ALU.mult), [Ttb], [Ttb])
            dve(lambda e: e.tensor_scalar(out=flat(tb["SnA"]), in0=flat(tb["Sn"]), scalar1=sgA, scalar2=None, op0=ALU.mult), [Ttb, Tc], [Ttb])
            dve(lambda e: e.tensor_scalar(out=flat(tb["SnB"]), in0=flat(tb["Sn"]), scalar1=sgB, scalar2=None, op0=ALU.mult), [Ttb, Tc], [Ttb])
            ar = tb["Cn"][:, 16, :]; ai = tb["Sn"][:, 16, :]
            s_ = lambda i: sm[:, i, :]
            tt = lambda o, a, b, op: dve(lambda e: e.tensor_tensor(out=o, in0=a, in1=b, op=op), [Tsm, Ttb, Tsp], [Tsm])
            dve(lambda e: e.tensor_scalar(out=s_(3), in0=ar, scalar1=-1.0, scalar2=None, op0=ALU.add), [Ttb], [Tsm])
            tt(s_(4), s_(3), lr2, ALU.mult); tt(s_(5), ai, li2, ALU.mult); tt(s_(4), s_(4), s_(5), ALU.add)
            tt(s_(5), ai, lr2, ALU.mult); tt(s_(6), s_(3), li2, ALU.mult); tt(s_(5), s_(5), s_(6), ALU.subtract)
            tt(s_(6), lr2, lr2, ALU.mult); tt(s_(7), li2, li2, ALU.mult); tt(s_(6), s_(6), s_(7), ALU.add)
            dve(lambda e: e.reciprocal(s_(6), s_(6)), [Tsm], [Tsm])
            tt(s_(8), s_(4), s_(6), ALU.mult)
            tt(s_(9), s_(5), s_(6), ALU.mult)
            dve(lambda e: e.tensor_scalar(out=s_(10), in0=s_(9), scalar1=sgB, scalar2=None, op0=ALU.mult), [Tsm, Tc], [Tsm])
            dve(lambda e: e.tensor_scalar(out=s_(11), in0=s_(9), scalar1=sgA, scalar2=None, op0=ALU.mult), [Tsm, Tc], [Tsm])
            X1 = sb("X1", [P, 32, 16]); X2 = sb("X2", [P, 32, 16]); Xt = sb("Xt", [P, 32, 16])
            TX = Tok("X")
            bh = lambda a: a.unsqueeze(2).to_broadcast([P, 32, 16])
            r3 = lambda a: a.rearrange("p (g h) -> p g h", h=16)
            dve(lambda e: e.tensor_tensor(out=X1[:], in0=r3(R1), in1=bh(s_(8)), op=ALU.mult), [Tsp, Tsm], [TX])
            dve(lambda e: e.tensor_tensor(out=Xt[:], in0=r3(R2), in1=bh(s_(10)), op=ALU.mult), [Tsp, Tsm], [TX])
            dve(lambda e: e.tensor_tensor(out=X1[:], in0=X1[:], in1=Xt[:], op=ALU.add), [TX], [TX])
            dve(lambda e: e.tensor_tensor(out=X2[:], in0=r3(R2), in1=bh(s_(8)), op=ALU.mult), [Tsp, Tsm], [TX])
            dve(lambda e: e.tensor_tensor(out=Xt[:], in0=r3(R1), in1=bh(s_(11)), op=ALU.mult), [Tsp, Tsm], [TX])
            dve(lambda e: e.tensor_tensor(out=X2[:], in0=X2[:], in1=Xt[:], op=ALU.add), [TX], [TX])
            T3 = sb("T3", [P, 8, 32]); T4 = sb("T4", [P, 8, 32]); TT34 = Tok("T34")
            dve(lambda e: e.tensor_copy(T3[0:64], tb["Cn"][0:64, 16:24, :]), [Ttb], [TT34])
            dve(lambda e: e.tensor_scalar(out=T3[64:128], in0=tb["Sn"][64:128, 16:24, :], scalar1=-1.0, scalar2=None, op0=ALU.mult), [Ttb], [TT34])
            dve(lambda e: e.tensor_scalar(out=T4[0:64], in0=tb["Sn"][0:64, 16:24, :], scalar1=-1.0, scalar2=None, op0=ALU.mult), [Ttb], [TT34])
            dve(lambda e: e.tensor_scalar(out=T4[64:128], in0=tb["Cn"][64:128, 16:24, :], scalar1=-1.0, scalar2=None, op0=ALU.mult), [Ttb], [TT34])
            big = {k: sb("big_" + k, [P, 32, 8, 16]) for k in ("WBe", "WBn", "WC", "m1", "m2")}
            Tbig = {k: Tok("big" + k) for k in big}
            def tab(t, i0):
                return t[:, i0:i0 + 8, :].rearrange("p s g -> p g s").unsqueeze(3).to_broadcast([P, 32, 8, 16])
            def xb(x):
                return x[:].unsqueeze(2).to_broadcast([P, 32, 8, 16])
            def combo(dst, ta, xa, tb_, xb_, i0, extra_r):
                dve(lambda e: e.tensor_tensor(out=big["m1"][:], in0=tab(ta, i0), in1=xb(xa), op=ALU.mult), extra_r, [Tbig["m1"]])
                dve(lambda e: e.tensor_tensor(out=big["m2"][:], in0=tab(tb_, i0), in1=xb(xb_), op=ALU.mult), extra_r, [Tbig["m2"]])
                dve(lambda e: e.tensor_tensor(out=big[dst][:], in0=big["m1"][:], in1=big["m2"][:], op=ALU.add), [Tbig["m1"], Tbig["m2"]], [Tbig[dst]])
            combo("WBe", tb["Cn"], X1, tb["SnB"], X2, 0, [Ttb, TX])
            combo("WBn", tb["Cn"], X1, tb["SnB"], X2, 8, [Ttb, TX])
            C1v = sb("C1v", [P, 32, 16]); C2v = sb("C2v", [P, 32, 16]); TC = Tok("C")
            dve(lambda e: e.tensor_copy(C1v[:], r3(C1)), [Tsp], [TC])
            dve(lambda e: e.tensor_copy(C2v[:], r3(C2)), [Tsp], [TC])
            combo("WC", T3, C1v, T4, C2v, 0, [TT34, TC])
            idf = cst[:, K_ID:K_ID + 128]; tmask = cst[:, K_TM:K_TM + 128]; swapm = cst[:, K_SW:K_SW + 128]
            outb = [sb("outb%d" % i, [P, 32, 128], BF16) for i in range(2)]
            Toutb = [Tok("outb%d" % i) for i in range(2)]
            tmpT = sb("tmpT", [P, 4, 128]); TtmpT = Tok("tmpT")
            g3 = lambda t, g: t[:, g, :, :].rearrange("p s h -> p (s h)")
            for b in range(8):
                ps, Tp = self.psum_next()
                for gi in range(4):
                    g = 4 * b + gi
                    kb.op('pe', lambda e: e.transpose(ps[:, gi * 128:(gi + 1) * 128], g3(big["WBe"], g), idf), reads=[Tbig["WBe"], Tc], writes=[Tp])
                kb.op('act', lambda e: e.activation(out=outb[0][:, 4 * b:4 * b + 4, :].rearrange("p a b -> p (a b)"), in_=ps[:], func=AF.Copy), reads=[Tp], writes=[Toutb[0]])
            kb.dma('sp', self.s5m[l, 0], outb[0][:].rearrange("p a b -> p (a b)"), reads=[Toutb[0]], writes=[self.Ts5m[l][0]], owner=self.Ts5m[l][0])
            for b in range(8):
                ps, Tp = self.psum_next()
                for gi in range(4):
                    g = 4 * b + gi
                    kb.op('pe', lambda e: e.matmul(ps[:, gi * 128:(gi + 1) * 128], g3(big["WBn"], g), g3(big["WC"], g), start=True, stop=True), reads=[Tbig["WBn"], Tbig["WC"]], writes=[Tp])
                dve(lambda e: e.tensor_tensor(out=tmpT[:], in0=ps[:].rearrange("p (a b) -> p a b", b=128), in1=tmask.unsqueeze(1).to_broadcast([P, 4, 128]), op=ALU.mult), [Tp, Tc], [TtmpT])
                for gi in range(4):
                    g = 4 * b + gi
                    dve(lambda e: e.scalar_tensor_tensor(out=outb[1][:, g, :], in0=idf, scalar=dc[:, g:g + 1], in1=tmpT[:, gi, :], op0=ALU.mult, op1=ALU.add), [TtmpT, Tc, Tsp], [Toutb[1]])
            kb.dma('sp', self.s5m[l, 1], outb[1][:].rearrange("p a b -> p (a b)"), reads=[Toutb[1]], writes=[self.Ts5m[l][1]], owner=self.Ts5m[l][1])
            dve(lambda e: e.tensor_copy(outb[0][:].rearrange("p a b -> p (a b)"), big["WC"][:].rearrange("p g s h -> p (g s h)")), [Tbig["WC"]], [Toutb[0]])
            kb.dma('sp', self.s5m[l, 2], outb[0][:].rearrange("p a b -> p (a b)"), reads=[Toutb[0]], writes=[self.Ts5m[l][2]], owner=self.Ts5m[l][2])
            m1 = big["m1"][:].rearrange("p g s h -> p g (s h)"); m2 = big["m2"][:].rearrange("p g s h -> p g (s h)")
            for k in range(8):
                colC = tb["Cn"][:, 24 + k, :].unsqueeze(2).to_broadcast([P, 32, 128])
                colS = tb["SnA"][:, 24 + k, :].unsqueeze(2).to_broadcast([P, 32, 128])
                ob = outb[(k + 1) % 2]; To = Toutb[(k + 1) % 2]
                dve(lambda e: e.tensor_tensor(out=m1, in0=idf.unsqueeze(1).to_broadcast([P, 32, 128]), in1=colC, op=ALU.mult), [Tc, Ttb], [Tbig["m1"]])
                kb.op('pool', lambda e: e.tensor_tensor(out=m2, in0=swapm.unsqueeze(1).to_broadcast([P, 32, 128]), in1=colS, op=ALU.mult), reads=[Tc, Ttb], writes=[Tbig["m2"]])
                dve(lambda e: e.tensor_tensor(out=ob[:], in0=m1, in1=m2, op=ALU.add), [Tbig["m1"], Tbig["m2"]], [To])
                kb.dma('sp', self.s5m[l, 3 + k], ob[:].rearrange("p a b -> p (a b)"), reads=[To], writes=[self.Ts5m[l][3 + k]], owner=self.Ts5m[l][3 + k])
            kb.full_barrier()
            for e_ in ('sp', 'dve', 'act', 'pool', 'pe'):
                kb.barrier(Toutb + [Tsp], eng=e_)

    def main(self, es):
        nc = self.nc; kb = self.kb
        self.xres = self.sb(es, "xres", [P, DC, L], F32)
        self.Tx = [[Tok("x%d_%d" % (b, c)) for c in range(DC)] for b in range(4)]
        self.hbuf = self.sb(es, "hbuf", [P, DC, TT], BF16)
        self.Th = [Tok("h0"), Tok("h1")]
        self.ycat = self.sb(es, "ycat", [P, DC, TT], BF16)
        self.Tyc = [[Tok("yc%d_%d" % (hb, c)) for c in range(DC)] for hb in range(2)]
        self.cur_slot = self.sb(es, "WinGslot", [P, DC, 1552], BF16)
        self.carry = self.sb(es, "carry", [P, G], F32); self.Tcarry = Tok("carry")
        self.gst_f = self.sb(es, "gst_f", [P, 2, 128], F32)
        self.gst_b = [self.sb(es, "gst_b%d" % i, [P, 2, 128], BF16) for i in range(2)]
        self.Tgf = Tok("gst_f"); self.Tgb = [Tok("gst_b0"), Tok("gst_b1")]
        for s in range(self.nseq):
            kb.new_epoch()
            for b in range(4):
                kb.dma('sp', self.xres[:, :, b * 512:(b + 1) * 512], self.xT[s, :, :, b * 512:(b + 1) * 512], writes=self.Tx[b], owner=self.Tx[b][0])
            self.stopped = (self.stop_after == 'load')
            for li, l in enumerate(self.layers):
                if li + 1 < len(self.layers): self.next_layer = self.layers[li + 1]
                elif s + 1 < self.nseq: self.next_layer = self.layers[0]
                else: self.next_layer = None
                for tile in range(2):
                    if not self.stopped: self.mixer(l, tile)
                for tile in range(2):
                    if not self.stopped: self.ffn(l, tile)
            if 'xres' in self.dbg_out:
                kb.dma('sp', self.dbg_out['xres'], self.xres[:], reads=[t for b in range(4) for t in self.Tx[b]], owner=self.Tx[0][0])
            if self.final:
                self.final_norm(s)
            else:
                for b in range(4):
                    kb.dma('sp', self.yT[s, :, :, b * 512:(b + 1) * 512], self.xres[:, :, b * 512:(b + 1) * 512], reads=self.Tx[b], owner=self.Tx[b][1])
        allt = [t for b in range(4) for t in self.Tx[b]] + getattr(self, 'Tout', [])
        kb.full_barrier()
        for e_ in ('sp', 'act', 'pool'):
            kb.barrier(allt, eng=e_)

    def rmsnorm_block(self, es_scr, blk, col0, dst_fn, Tdst):
        kb = self.kb
        sq, Tsq, rs, Trs = es_scr
        t0 = blk * 512
        Tx = self.Tx[blk]
        kb.op('act', lambda e: e.activation(out=sq[:], in_=self.xres[:, :, t0:t0 + 512], func=AF.Square), reads=Tx, writes=[Tsq])
        ps, Tp = self.psum_next()
        for c in range(DC):
            kb.op('pe', lambda e: e.matmul(ps[:], self.ones_bf[:], sq[:, c, :], start=(c == 0), stop=(c == DC - 1)), reads=[Tsq, self.Tconst], writes=[Tp])
        kb.op('act', lambda e: e.activation(out=rs[:], in_=ps[:], func=AF.Sqrt, scale=1.0 / D, bias=self.epsc[:, 0:1]), reads=[Tp, self.Tconst], writes=[Trs])
        kb.op('dve', lambda e: e.reciprocal(rs[:], rs[:]), reads=[Trs], writes=[Trs])
        for c in range(DC):
            kb.op('dve', lambda e: e.scalar_tensor_tensor(out=dst_fn(c), in0=self.xres[:, c, t0:t0 + 512], scalar=self.cols[:, col0 + c:col0 + c + 1], in1=rs[:], op0=ALU.mult, op1=ALU.mult), reads=[Tx[c], Trs, self.Tconst], writes=Tdst)

    def norm_scratch(self, es):
        sq = self.sb(es, "nsq", [P, DC, 512], BF16); rs = self.sb(es, "nrs", [P, 512], F32)
        return (sq, Tok("nsq"), rs, Tok("nrs"))

    def mixer(self, l, tile):
        nc = self.nc; kb = self.kb
        with contextlib.ExitStack() as es:
            self.wing_slot(es)
            if getattr(self, 'wing_valid', None) != l:
                self.load_wing(l)
            scr = self.norm_scratch(es)
            for hb in range(2):
                self.rmsnorm_block(scr, tile * 2 + hb, C_NMIX + l * 8, lambda c: self.hbuf[:, c, hb * 512:(hb + 1) * 512], [self.Th[hb]])
            kb.full_barrier()
        if self.stop_after == 'norm': self.stopped = True; return
        with contextlib.ExitStack() as esm:
            self.WSs = self.sb(esm, "WSs", [P, 6144], BF16)
            with contextlib.ExitStack() as es:
                self.gla(es, l, tile)
                kb.full_barrier()
            with contextlib.ExitStack() as es:
                self.s5(es, l, tile)
                kb.full_barrier()
        if self.stop_after == 's5': self.stopped = True; return
        with contextlib.ExitStack() as es:
            self.wout(es, l, tile)
            kb.full_barrier()
        if self.stop_after == 'wout': self.stopped = True; return

    def gla(self, es, l, tile):
        nc = self.nc; kb = self.kb
        sb = lambda n, s, d: self.sb(es, "g_" + n, s, d)
        WinG = self.wing_slot(es); TW = self.ptok("WinG")
        if getattr(self, 'wing_valid', None) != l:
            self.load_wing(l)
        WSs = self.WSs
        kb.dma('pool', WSs[:, 0:4096].rearrange("p (c n) -> p c n", c=DC), self.w_in[l, :, :, 0:512], writes=[self.ptok("WinS")])
        kb.dma('pool', WSs[:, 4096:6144].rearrange("p (c n) -> p c n", c=4), self.w_glu[l], writes=[self.ptok("Wglu")])
        glx = sb("glx", [33, TT], BF16); Tglx = Tok("glx")
        Epos = sb("Epos", [P, 2, TT], F32); Eneg = sb("Eneg", [P, 2, TT], F32)
        TEp = [Tok("Ep%d" % i) for i in range(8)]
        qd = sb("qd", [P, 2, TT], BF16); ki = sb("ki", [P, 2, TT], BF16); Tqk = [Tok("qk0"), Tok("qk1")]
        gs = sb("gs", [P, 4, TT], BF16); Tgs = [Tok("gs0"), Tok("gs1")]
        vt = sb("vt", [P, 8, 512], BF16); Tvt = [Tok("vt%d" % i) for i in range(8)]
        ke = sb("ke", [P, 8, 256], BF16); Tke = [Tok("ke%d" % i) for i in range(8)]
        Erc = [sb("Erc%d" % i, [P, 256], F32) for i in range(2)]; TErc = [Tok("Erc0"), Tok("Erc1")]
        e1 = [sb("e1_%d" % i, [P, 256], F32) for i in range(2)]; Te1 = [Tok("e1_0"), Tok("e1_1")]
        nl = [sb("nl_%d" % i, [P, 256], F32) for i in range(2)]; Tnl = [Tok("nl0"), Tok("nl1")]
        sT = [sb("sT%d" % i, [P, 4, 128], BF16) for i in range(2)]; TsT = [Tok("sT0"), Tok("sT1")]
        on = [sb("on%d" % i, [P, 4, 128], BF16) for i in range(2)]; Ton = [Tok("on0"), Tok("on1")]
        junk = sb("junk", [P, 128], BF16); Tjunk = Tok("junk")
        ss = [sb("ss%d" % i, [P, 4], F32) for i in range(2)]; Tss = [Tok("ss0"), Tok("ss1")]
        Tc = self.Tconst
        if tile == 0:
            kb.op('dve', lambda e: e.memset(self.gst_f[:], 0.0), writes=[self.Tgf])
            kb.op('dve', lambda e: e.memset(self.gst_b[0][:], 0.0), writes=[self.Tgb[0]])
        kb.op('dve', lambda e: e.memset(glx[:], 0.0), writes=[Tglx])
        kb.op('dve', lambda e: e.memset(glx[32:33, :], 1.0), writes=[Tglx])
        for hb in range(2):
            ps, Tp = self.psum_next()
            for c in range(DC):
                kb.op('pe', lambda e: e.matmul(ps[0:16, :], WinG[:, c, 1536:1552], self.hbuf[:, c, hb * 512:(hb + 1) * 512], start=(c == 0), stop=(c == DC - 1)), reads=[TW, self.Th[hb]], writes=[Tp])
            kb.op('act', lambda e: e.activation(out=glx[0:16, hb * 512:(hb + 1) * 512], in_=ps[0:16, :], func=AF.Copy), reads=[Tp], writes=[Tglx])
        for st in range(8):
            tk = slice(st * 128, (st + 1) * 128); hb = st // 4; r = st % 2
            ps, Tp = self.psum_next()
            kb.op('pe', lambda e: e.matmul(ps[:, 0:256], glx[0:33, tk], self.wgx[0:33, l, :], start=True, stop=True), reads=[Tglx, self.Twgx], writes=[Tp])
            kb.op('act', lambda e: e.activation(out=e1[r][:], in_=ps[:, 0:256], func=AF.Exp, scale=-1.0), reads=[Tp], writes=[Te1[r]])
            ps, Tp = self.psum_next()
            for c in range(DC):
                kb.op('pe', lambda e: e.matmul(ps[:], self.hbuf[:, c, tk], WinG[:, c, 512:1024], start=(c == 0), stop=(c == DC - 1)), reads=[TW, self.Th[hb]], writes=[Tp])
            kb.op('act', lambda e: e.activation(out=vt[:, st, :], in_=ps[:], func=AF.Copy), reads=[Tp], writes=[Tvt[st]])
            kb.op('act', lambda e: e.activation(out=nl[r][:], in_=e1[r][:], func=AF.Ln, bias=self.epsc[:, 1:2]), reads=[Te1[r], Tc], writes=[Tnl[r]])
            ps, Tp = self.psum_next()
            for c in range(2):
                kb.op('pe', lambda e: e.matmul(ps[:, c * 128:(c + 1) * 128], nl[r][:, c * 128:(c + 1) * 128], self.cmask[:], start=True, stop=True), reads=[Tnl[r], Tc], writes=[Tp])
            kb.op('act', lambda e: e.activation(out=Epos[:, :, tk], in_=ps[:, 0:256].rearrange("p (c t) -> p c t", c=2), func=AF.Exp, scale=-1.0 / 16), reads=[Tp], writes=[TEp[st]])
            kb.op('act', lambda e: e.activation(out=Eneg[:, :, tk], in_=ps[:, 0:256].rearrange("p (c t) -> p c t", c=2), func=AF.Exp, scale=1.0 / 16), reads=[Tp], writes=[TEp[st]])
            ps, Tp = self.psum_next()
            kb.op('pe', lambda e: e.matmul(ps[:, 0:256], self.triR[:], nl[r][:], start=True, stop=True), reads=[Tnl[r], Tc], writes=[Tp])
            kb.op('act', lambda e: e.activation(out=Erc[r][:], in_=ps[:, 0:256], func=AF.Exp, scale=-1.0 / 16), reads=[Tp], writes=[TErc[r]])
            ps, Tp = self.psum_next()
            for c in range(DC):
                kb.op('pe', lambda e: e.matmul(ps[:, 0:256], self.hbuf[:, c, tk], WinG[:, c, 256:512], start=(c == 0), stop=(c == DC - 1)), reads=[TW, self.Th[hb]], writes=[Tp])
            kb.op('dve', lambda e: e.tensor_tensor(out=ke[:, st, :], in0=ps[:, 0:256], in1=Erc[r][:], op=ALU.mult), reads=[Tp, TErc[r]], writes=[Tke[st]])
        for hb in range(2):
            hs = slice(hb * 512, (hb + 1) * 512)
            TE = TEp[hb * 4:(hb + 1) * 4]
            for c in range(2):
                ps, Tp = self.psum_next()
                for kc in range(DC):
                    kb.op('pe', lambda e: e.matmul(ps[:], WinG[:, kc, c * 128:(c + 1) * 128], self.hbuf[:, kc, hs], start=(kc == 0), stop=(kc == DC - 1)), reads=[TW, self.Th[hb]], writes=[Tp])
                kb.op('dve', lambda e: e.scalar_tensor_tensor(out=qd[:, c, hs], in0=ps[:], scalar=0.125, in1=Epos[:, c, hs], op0=ALU.mult, op1=ALU.mult), reads=[Tp] + TE, writes=[Tqk[hb]])
                ps, Tp = self.psum_next()
                for kc in range(DC):
                    kb.op('pe', lambda e: e.matmul(ps[:], WinG[:, kc, 256 + c * 128:256 + (c + 1) * 128], self.hbuf[:, kc, hs], start=(kc == 0), stop=(kc == DC - 1)), reads=[TW, self.Th[hb]], writes=[Tp])
                kb.op('dve', lambda e: e.tensor_tensor(out=ki[:, c, hs], in0=ps[:], in1=Eneg[:, c, hs], op=ALU.mult), reads=[Tp] + TE, writes=[Tqk[hb]])
            for c in range(4):
                ps, Tp = self.psum_next()
                for kc in range(DC):
                    kb.op('pe', lambda e: e.matmul(ps[:], WinG[:, kc, 1024 + c * 128:1024 + (c + 1) * 128], self.hbuf[:, kc, hs], start=(kc == 0), stop=(kc == DC - 1)), reads=[TW, self.Th[hb]], writes=[Tp])
                kb.op('act', lambda e: e.activation(out=gs[:, c, hs], in_=ps[:], func=AF.Silu), reads=[Tp], writes=[Tgs[hb]])
        def scores(st):
            tk = slice(st * 128, (st + 1) * 128); hb = st // 4; r = st % 2
            psb = [self.psum_next(), self.psum_next()]
            for hd in range(4):
                c = hd // 2; par = hd % 2; pr = slice(par * 64, par * 64 + 64)
                ps, Tp = psb[par]
                kb.op('pe', lambda e: e.matmul(ps[:, c * 128:(c + 1) * 128], ki[pr, c, tk], qd[pr, c, tk], start=True, stop=True), reads=[Tqk[hb]], writes=[Tp])
            for par in range(2):
                ps, Tp = psb[par]
                dst = sT[r][:].rearrange("p (c two) t -> p two c t", two=2)[:, par, :, :]
                kb.op('dve', lambda e: e.tensor_tensor(out=dst, in0=ps[:, 0:256].rearrange("p (a b) -> p a b", b=128), in1=self.cmask[:].unsqueeze(1).to_broadcast([P, 2, 128]), op=ALU.mult), reads=[Tp, Tc], writes=[TsT[r]])

        def upd_state(st, half, dst):
            rows = slice(half * 64, half * 64 + 64)
            psu, Tpu = self.psum_next()
            for hd in range(4):
                c = hd // 2; pr = slice((hd % 2) * 64, (hd % 2) * 64 + 64)
                kb.op('pe', lambda e: e.matmul(psu[pr, c * 128:(c + 1) * 128], ke[rows, st, hd * 64:(hd + 1) * 64], vt[rows, st, hd * 128:(hd + 1) * 128], start=True, stop=True), reads=[Tke[st], Tvt[st]], writes=[Tpu])
            tend = st * 128 + half * 64 + 63
            for c in range(2):
                kb.op('dve', lambda e: e.scalar_tensor_tensor(out=self.gst_f[:, c, :], in0=self.gst_f[:, c, :], scalar=Epos[:, c, tend:tend + 1], in1=psu[:, c * 128:(c + 1) * 128], op0=ALU.mult, op1=ALU.add), reads=[self.Tgf, TEp[st], Tpu], writes=[self.Tgf])
            kb.op('act', lambda e: e.activation(out=self.gst_b[dst][:].rearrange("p a b -> p (a b)"), in_=self.gst_f[:].rearrange("p a b -> p (a b)"), func=AF.Copy), reads=[self.Tgf], writes=[self.Tgb[dst]])

        def tail(st):
            tk = slice(st * 128, (st + 1) * 128); hb = st // 4; r = st % 2
            pst, Tpt = self.psum_next()
            pstb = pst[:].bitcast(BF16)
            for hd in range(4):
                kb.op('pe', lambda e: e.transpose(pstb[:, hd * 128:(hd + 1) * 128], on[r][:, hd, :], self.ident_bf[:]), reads=[Ton[r], Tc], writes=[Tpt])
            for hd in range(4):
                kb.op('dve', lambda e: e.scalar_tensor_tensor(out=self.ycat[:, 4 + hd, tk], in0=pstb[:, hd * 128:(hd + 1) * 128], scalar=self.cols[:, C_GLAN + l * 4 + hd:C_GLAN + l * 4 + hd + 1], in1=gs[:, hd, tk], op0=ALU.mult, op1=ALU.mult), reads=[Tpt, Tc, Tgs[hb]], writes=[self.Tyc[hb][4 + hd]])

        scores(0)
        for st in range(8):
            tk = slice(st * 128, (st + 1) * 128); hb = st // 4; r = st % 2
            upd_state(st, 0, 1)
            if st + 1 < 8:
                scores(st + 1)
            if st >= 1:
                tail(st - 1)
            pob = [self.psum_next(), self.psum_next()]
            for hd in range(4):
                c = hd // 2; par = hd % 2; pr = slice(par * 64, par * 64 + 64)
                pso, Tpo = pob[par]
                oc = slice(c * 128, (c + 1) * 128)
                kb.op('pe', lambda e: e.matmul(pso[:, oc], sT[r][:, hd, :], vt[:, st, hd * 128:(hd + 1) * 128], start=True, stop=False, skip_group_check=True), reads=[TsT[r], Tvt[st]], writes=[Tpo])
                kb.op('pe', lambda e: e.matmul(pso[0:64, oc], qd[pr, c, st * 128:st * 128 + 64], self.gst_b[0][pr, c, :], start=False, stop=False, skip_group_check=True), reads=[Tqk[hb], self.Tgb[0]], writes=[Tpo])
                kb.op('pe', lambda e: e.matmul(pso[64:128, oc], qd[pr, c, st * 128 + 64:st * 128 + 128], self.gst_b[1][pr, c, :], start=False, stop=True, skip_group_check=True), reads=[Tqk[hb], self.Tgb[1]], writes=[Tpo])
            upd_state(st, 1, 0)
            for hd in range(4):
                c = hd // 2; par = hd % 2; pso, Tpo = pob[par]
                kb.op('act', lambda e: e.activation(out=junk[:], in_=pso[:, c * 128:(c + 1) * 128], func=AF.Square, accum_out=ss[r][:, hd:hd + 1]), reads=[Tpo], writes=[Tjunk, Tss[r]])
            kb.op('act', lambda e: e.activation(out=ss[r][:], in_=ss[r][:], func=AF.Sqrt, scale=1.0 / 128, bias=self.epsc[:, 0:1]), reads=[Tss[r], Tc], writes=[Tss[r]])
            kb.op('dve', lambda e: e.reciprocal(ss[r][:], ss[r][:]), reads=[Tss[r]], writes=[Tss[r]])
            for par in range(2):
                pso, Tpo = pob[par]
                dst = on[r][:].rearrange("p (c two) t -> p two c t", two=2)[:, par, :, :]
                sc = ss[r][:].rearrange("p (c two) -> p two c", two=2)[:, par, :].unsqueeze(2).to_broadcast([P, 2, 128])
                kb.op('dve', lambda e: e.tensor_tensor(out=dst, in0=pso[:, 0:256].rearrange("p (a b) -> p a b", b=128), in1=sc, op=ALU.mult), reads=[Tpo, Tss[r]], writes=[Ton[r]])
        tail(7)

    def s5(self, es, l, tile):
        nc = self.nc; kb = self.kb
        sb = lambda n, s, d: self.sb(es, "s_" + n, s, d)
        Tc = self.Tconst
        slot = self.cur_slot
        self.wing_valid = None
        flat = slot[:].rearrange("p a b -> p (a b)")
        WSs = self.WSs
        WinS = WSs[:, 0:4096].rearrange("p (c n) -> p c n", c=DC); TW = self.ptok("WinS")
        Wglu = WSs[:, 4096:6144].rearrange("p (c n) -> p c n", c=4); TWg = self.ptok("Wglu")
        mats = [sb("mat%d" % i, [P, G, 128], BF16) for i in range(3)]; Tm = [self.ptok("mat%d" % i) for i in range(3)]
        for i in range(3):
            kb.dma('sp', mats[i][:].rearrange("p a b -> p (a b)"), self.s5m[l, i], reads=[self.Ts5m[l][i]], writes=[Tm[i]], owner=Tm[i])
        WB, Toep, WC = mats
        ring = [sb("ring%d" % i, [P, G, 128], BF16) for i in range(2)]; Tring = [self.ptok("ring0"), self.ptok("ring1")]
        bufA = sb("bufA", [P, 4, TT], BF16); TA = [Tok("bufA%d" % i) for i in range(4)]
        UT = sb("UT", [P, G, 128], BF16); TUT = [Tok("UT%d" % i) for i in range(8)]
        Sf = flat[:, 0:8256].bitcast(F32).rearrange("p (g j) -> p g j", j=129)
        Sb_ = flat[:, 8256:12384].rearrange("p (g j) -> p g j", j=129)
        TSf = [Tok("Sf%d" % i) for i in range(8)]; TSb = [Tok("Sb%d" % i) for i in range(8)]
        yg = ring[0]; Tyg = [Tok("yg%d" % i) for i in range(4)]
        y2 = self.hbuf
        if tile == 0:
            kb.op('dve', lambda e: e.memset(self.carry[:], 0.0), writes=[self.Tcarry])
        for cc in range(4):
            for hb in range(2):
                ps, Tp = self.psum_next()
                for c in range(DC):
                    kb.op('pe', lambda e: e.matmul(ps[:], WinS[:, c, cc * 128:(cc + 1) * 128], self.hbuf[:, c, hb * 512:(hb + 1) * 512], start=(c == 0), stop=(c == DC - 1)), reads=[TW, self.Th[hb]], writes=[Tp])
                eng = self.evac_eng()
                if eng == 'act':
                    kb.op('act', lambda e: e.activation(out=bufA[:, cc, hb * 512:(hb + 1) * 512], in_=ps[:], func=AF.Copy), reads=[Tp], writes=[TA[cc]])
                else:
                    kb.op('dve', lambda e: e.tensor_copy(bufA[:, cc, hb * 512:(hb + 1) * 512], ps[:]), reads=[Tp], writes=[TA[cc]])
        U8 = ring[1][:].rearrange("p g (s h) -> p g s h", s=8)
        for s_ in range(8):
            ps, Tp = self.psum_next()
            psb = ps[:].bitcast(BF16)
            for cc in range(4):
                mv = bufA[:, cc, :].rearrange("p (j s) -> p s j", s=8)[:, s_, :]
                kb.op('pe', lambda e: e.transpose(psb[:, cc * 128:(cc + 1) * 128], mv, self.ident_bf[:]), reads=[TA[cc], Tc], writes=[Tp])
            eng = self.evac_eng()
            if eng == 'act':
                kb.op('act', lambda e: e.activation(out=U8[:, :, s_, :], in_=psb[:, 0:512].rearrange("p (g h) -> p g h", h=16), func=AF.Copy), reads=[Tp], writes=[Tring[1]])
            else:
                kb.op('dve', lambda e: e.tensor_copy(U8[:, :, s_, :], psb[:, 0:512].rearrange("p (g h) -> p g h", h=16)), reads=[Tp], writes=[Tring[1]])
        for b in range(8):
            ps, Tp = self.psum_next()
            psb = ps[:].bitcast(BF16)
            for gi in range(4):
                g = 4 * b + gi
                kb.op('pe', lambda e: e.transpose(psb[:, gi * 128:(gi + 1) * 128], ring[1][:, g, :], self.ident_bf[:]), reads=[Tring[1], Tc], writes=[Tp])
            eng = self.evac_eng()
            dst = UT[:, 4 * b:4 * b + 4, :].rearrange("p a b -> p (a b)")
            if eng == 'act':
                kb.op('act', lambda e: e.activation(out=dst, in_=psb[:, 0:512], func=AF.Copy), reads=[Tp], writes=[TUT[b]])
            else:
                kb.op('dve', lambda e: e.tensor_copy(dst, psb[:, 0:512]), reads=[Tp], writes=[TUT[b]])
        for b in range(8):
            ps, Tp = self.psum_next()
            for gi in range(4):
                g = 4 * b + gi
                kb.op('pe', lambda e: e.matmul(ps[:, gi * 128:(gi + 1) * 128], WB[:, g, :], UT[:, g, :], start=True, stop=True), reads=[Tm[0], TUT[b]], writes=[Tp])
            g4 = slice(4 * b, 4 * b + 4)
            kb.op('dve', lambda e: e.tensor_copy(Sf[:, g4, 1:129], ps[:].rearrange("p (a b) -> p a b", b=128)), reads=[Tp], writes=[TSf[b]])
            kb.op('dve', lambda e: e.tensor_copy(Sf[:, g4, 0:1], self.carry[:, g4].unsqueeze(2)), reads=[self.Tcarry], writes=[TSf[b]])
            kb.op('act', lambda e: e.activation(out=Sb_[:, g4, :], in_=Sf[:, g4, :], func=AF.Copy), reads=[TSf[b]], writes=[TSb[b]])
        for k in range(8):
            d = 1 << k; n = 129 - d
            rg = ring[k % 2]; Tr = Tring[k % 2]
            kb.dma('sp', rg[:].rearrange("p a b -> p (a b)"), self.s5m[l, 3 + k], reads=[self.Ts5m[l][3 + k]], writes=[Tr], owner=Tr)
            for b in range(8):
                ps, Tp = self.psum_next()
                g4 = slice(4 * b, 4 * b + 4)
                for gi in range(4):
                    g = 4 * b + gi
                    kb.op('pe', lambda e: e.matmul(ps[:, gi * 128:gi * 128 + n], rg[:, g, :], Sb_[:, g, 0:n], start=True, stop=True), reads=[Tr, TSb[b]], writes=[Tp])
                kb.op('dve', lambda e: e.tensor_tensor(out=Sf[:, g4, d:129], in0=Sf[:, g4, d:129], in1=ps[:].rearrange("p (a b) -> p a b", b=128)[:, :, 0:n], op=ALU.add), reads=[Tp, TSf[b]], writes=[TSf[b]])
                kb.op('act', lambda e: e.activation(out=Sb_[:, g4, d:129], in_=Sf[:, g4, d:129], func=AF.Copy), reads=[TSf[b]], writes=[TSb[b]])
        kb.op('dve', lambda e: e.tensor_copy(self.carry[:].unsqueeze(2), Sf[:, :, 128:129]), reads=TSf, writes=[self.Tcarry])
        for b in range(8):
            ps, Tp = self.psum_next()
            for gi in range(4):
                g = 4 * b + gi
                kb.op('pe', lambda e: e.matmul(ps[:, gi * 128:(gi + 1) * 128], Toep[:, g, :], UT[:, g, :], start=True, stop=False), reads=[Tm[1], TUT[b]], writes=[Tp])
                kb.op('pe', lambda e: e.matmul(ps[:, gi * 128:(gi + 1) * 128], WC[:, g, :], Sb_[:, g, 0:128], start=False, stop=True), reads=[Tm[2], TSb[b]], writes=[Tp])
            kb.op('act', lambda e: e.activation(out=yg[:, 4 * b:4 * b + 4, :].rearrange("p a b -> p (a b)"), in_=ps[:], func=AF.Gelu_apprx_tanh), reads=[Tp], writes=[Tyg[b // 2], Tring[0]])
        Y8 = UT[:].rearrange("p g j -> p (g j)").rearrange("p (t c) -> p t c", t=8)
        for b in range(8):
            ps, Tp = self.psum_next()
            psb = ps[:].bitcast(BF16)
            for gi in range(4):
                g = 4 * b + gi
                kb.op('pe', lambda e: e.transpose(psb[:, gi * 128:(gi + 1) * 128], yg[:, g, :], self.ident_bf[:]), reads=[Tyg[b // 2], Tc], writes=[Tp])
            dst = Y8[:, :, 64 * b:64 * b + 64].rearrange("p t (g h) -> p g t h", g=4)
            src = psb[:, 0:512].rearrange("p (g t h) -> p g t h", g=4, t=8)
            eng = self.evac_eng()
            if eng == 'act':
                kb.op('act', lambda e: e.activation(out=dst, in_=src, func=AF.Copy), reads=[Tp], writes=TUT)
            else:
                kb.op('dve', lambda e: e.tensor_copy(dst, src), reads=[Tp], writes=TUT)
        for cc in range(4):
            for th in range(2):
                ps, Tp = self.psum_next()
                psb = ps[:].bitcast(BF16)
                for ti in range(4):
                    t0 = th * 4 + ti
                    kb.op('pe', lambda e: e.transpose(psb[:, ti * 128:(ti + 1) * 128], Y8[:, t0, cc * 128:(cc + 1) * 128], self.ident_bf[:]), reads=TUT + [Tc], writes=[Tp])
                dst = bufA[:, cc, :].rearrange("p (j s) -> p s j", s=8)[:, th * 4:th * 4 + 4, :]
                src = psb[:, 0:512].rearrange("p (a b) -> p a b", b=128)
                eng = self.evac_eng()
                if eng == 'act':
                    kb.op('act', lambda e: e.activation(out=dst, in_=src, func=AF.Copy), reads=[Tp], writes=[TA[cc]])
                else:
                    kb.op('dve', lambda e: e.tensor_copy(dst, src), reads=[Tp], writes=[TA[cc]])
        if 'yg' in self.dbg_out and tile == 0:
            kb.dma('pool', self.dbg_out['yg'], bufA[:], reads=TA, owner=TA[0])
        sq = UT[:, 0:16, :].rearrange("p (a b) c -> p a (b c)", a=4); Tsq = TUT[0:4]
        rs = UT[:, 16:24, :].rearrange("p a b -> p (a b)").bitcast(F32); Trs = TUT[4:6]
        gt = [UT[:, 24:28, :].rearrange("p a b -> p (a b)"), UT[:, 28:32, :].rearrange("p a b -> p (a b)")]; Tgt = [TUT[6], TUT[7]]
        for hb in range(2):
            hs = slice(hb * 512, (hb + 1) * 512)
            for co in range(4):
                ps, Tp = self.psum_next()
                for ci in range(4):
                    kb.op('pe', lambda e: e.matmul(ps[:], Wglu[:, ci, co * 128:(co + 1) * 128], bufA[:, ci, hs], start=(ci == 0), stop=(ci == 3)), reads=[TWg] + TA, writes=[Tp])
                r = co % 2
                kb.op('act', lambda e: e.activation(out=gt[r], in_=ps[:], func=AF.Sigmoid, bias=self.cols[:, C_BGLU + l * 4 + co:C_BGLU + l * 4 + co + 1]), reads=[Tp, Tc], writes=[Tgt[r]])
                kb.op('dve', lambda e: e.tensor_tensor(out=y2[:, co, hs], in0=bufA[:, co, hs], in1=gt[r], op=ALU.mult), reads=[TA[co], Tgt[r]], writes=[self.Th[hb]])
        for hb in range(2):
            hs = slice(hb * 512, (hb + 1) * 512)
            kb.op('act', lambda e: e.activation(out=sq, in_=y2[:, 0:4, hs], func=AF.Square), reads=[self.Th[hb]], writes=Tsq)
            ps, Tp = self.psum_next()
            for c in range(4):
                kb.op('pe', lambda e: e.matmul(ps[:], self.ones_bf[:], sq[:, c, :], start=(c == 0), stop=(c == 3)), reads=Tsq + [Tc], writes=[Tp])
            kb.op('act', lambda e: e.activation(out=rs, in_=ps[:], func=AF.Sqrt, scale=1.0 / 512, bias=self.epsc[:, 0:1]), reads=[Tp, Tc], writes=Trs)
            kb.op('dve', lambda e: e.reciprocal(rs, rs), reads=Trs, writes=Trs)
            for c in range(4):
                kb.op('dve', lambda e: e.scalar_tensor_tensor(out=self.ycat[:, c, hs], in0=y2[:, c, hs], scalar=self.cols[:, C_S5N + l * 4 + c:C_S5N + l * 4 + c + 1], in1=rs, op0=ALU.mult, op1=ALU.mult), reads=[self.Th[hb], Tc] + Trs, writes=[self.Tyc[hb][c]])

    def wout(self, es, l, tile):
        kb = self.kb
        self.wing_slot(es)
        Wout = self.sb(es, "Wout", [P, DC, DC, 128], BF16)
        TWo = [self.ptok("Wout%d" % c) for c in range(DC)]
        for co in range(DC):
            kb.dma('pool', Wout[:, co, :, :], self.w_out[l, co], writes=[TWo[co]])
        if tile == 0:
            self.load_wing(l)
        if 'ycat' in self.dbg_out and tile == 0:
            dtmp = self.sb(es, "dtmp2", [P, DC, TT], F32); Td = Tok("dtmp2")
            kb.op('dve', lambda e: e.tensor_copy(dtmp[:], self.ycat[:]), reads=[t for hb in range(2) for t in self.Tyc[hb]], writes=[Td])
            kb.dma('sp', self.dbg_out['ycat'], dtmp[:], reads=[Td])
            kb.barrier([Td], eng='dve')
        for hb in range(2):
            blk = tile * 2 + hb; t0 = blk * 512
            for co in range(DC):
                ps, Tp = self.psum_next()
                for ci in range(DC):
                    kb.op('pe', lambda e: e.matmul(ps[:], Wout[:, co, ci, :], self.ycat[:, ci, hb * 512:(hb + 1) * 512], start=(ci == 0), stop=(ci == DC - 1)), reads=[TWo[co], self.Tyc[hb][ci]], writes=[Tp])
                kb.op('dve', lambda e: e.tensor_tensor(out=self.xres[:, co, t0:t0 + 512], in0=ps[:], in1=self.xres[:, co, t0:t0 + 512], op=ALU.add), reads=[Tp, self.Tx[blk][co]], writes=[self.Tx[blk][co]])

    def ffn(self, l, tile):
        kb = self.kb
        NR1 = 4; NR2 = 2
        with contextlib.ExitStack() as esf:
            W1 = [self.sb(esf, "f_W1_%d" % i, [P, DC, 2, 128], BF16) for i in range(NR1)]
            TW1 = [self.ptok("W1_%d" % i) for i in range(NR1)]
            for f in range(NR1):
                kb.dma('pool', W1[f][:], self.w_f1[l, f], writes=[TW1[f]])
            with contextlib.ExitStack() as es:
                scr = self.norm_scratch(es)
                for hb in range(2):
                    self.rmsnorm_block(scr, tile * 2 + hb, C_NFFN + l * 8, lambda c: self.hbuf[:, c, hb * 512:(hb + 1) * 512], [self.Th[hb]])
                kb.full_barrier()
            with contextlib.ExitStack() as es:
                sb = lambda n, s, d: self.sb(es, "f_" + n, s, d)
                if tile == 1 and self.next_layer is not None:
                    self.load_wing(self.next_layer)
                act = sb("act", [P, FC, TT], BF16); Tact = [[Tok("act%d_%d" % (f, hb)) for hb in range(2)] for f in range(FC)]
                W2 = [sb("W2_%d" % i, [P, FC, 128], BF16) for i in range(NR2)]; TW2 = [self.ptok("W2_%d" % i) for i in range(NR2)]
                sg = [sb("sg%d" % i, [P, 512], BF16) for i in range(2)]; Tsg = [Tok("sg0"), Tok("sg1")]
                for f in range(FC):
                    w = W1[f % NR1]; Tw = TW1[f % NR1]
                    if f >= NR1:
                        kb.dma('pool', w[:], self.w_f1[l, f], writes=[Tw])
                    for hb in range(2):
                        hs = slice(hb * 512, (hb + 1) * 512)
                        psg, Tpg = self.psum_next()
                        for c in range(DC):
                            kb.op('pe', lambda e: e.matmul(psg[:], w[:, c, 0, :], self.hbuf[:, c, hs], start=(c == 0), stop=(c == DC - 1)), reads=[Tw, self.Th[hb]], writes=[Tpg])
                        psu, Tpu = self.psum_next()
                        for c in range(DC):
                            kb.op('pe', lambda e: e.matmul(psu[:], w[:, c, 1, :], self.hbuf[:, c, hs], start=(c == 0), stop=(c == DC - 1)), reads=[Tw, self.Th[hb]], writes=[Tpu])
                        r = (2 * f + hb) % 2
                        kb.op('act', lambda e: e.activation(out=sg[r][:], in_=psg[:], func=AF.Silu), reads=[Tpg], writes=[Tsg[r]])
                        kb.op('dve', lambda e: e.tensor_tensor(out=act[:, f, hs], in0=psu[:], in1=sg[r][:], op=ALU.mult), reads=[Tpu, Tsg[r]], writes=[Tact[f][hb]])
                for co in range(DC):
                    w = W2[co % NR2]; Tw = TW2[co % NR2]
                    kb.dma('pool', w[:], self.w_f2[l, co], writes=[Tw])
                    for hb in range(2):
                        blk = tile * 2 + hb; t0 = blk * 512
                        ps, Tp = self.psum_next()
                        for f in range(FC):
                            kb.op('pe', lambda e: e.matmul(ps[:], w[:, f, :], act[:, f, hb * 512:(hb + 1) * 512], start=(f == 0), stop=(f == FC - 1)), reads=[Tw, Tact[f][hb]], writes=[Tp])
                        kb.op('dve', lambda e: e.tensor_tensor(out=self.xres[:, co, t0:t0 + 512], in0=ps[:], in1=self.xres[:, co, t0:t0 + 512], op=ALU.add), reads=[Tp, self.Tx[blk][co]], writes=[self.Tx[blk][co]])
                kb.full_barrier()

    def final_norm(self, s):
        kb = self.kb
        with contextlib.ExitStack() as es:
            self.wing_slot(es)
            scr = self.norm_scratch(es)
            ob = [self.sb(es, "fo%d" % i, [P, DC, 512], F32) for i in range(2)]
            To = [self.ptok("fo0"), self.ptok("fo1")]
            self.Tout = To
            for blk in range(4):
                r = blk % 2
                self.rmsnorm_block(scr, blk, C_NFIN, lambda c: ob[r][:, c, :], [To[r]])
                kb.dma('sp', self.yT[s, :, :, blk * 512:(blk + 1) * 512], ob[r][:], reads=[To[r]], owner=To[r])
            kb.full_barrier()
            for e_ in ('sp', 'act', 'dve', 'pool', 'pe'):
                kb.barrier(To, eng=e_)


def _consts():
    c = np.zeros((P, NCONST), np.float32)
    idx = np.arange(P)
    c[:, K_ID:K_ID + 128] = np.eye(P, dtype=np.float32)
    s = idx[:, None]; t = idx[None, :]
    same = (s // 64) == (t // 64)
    c[:, K_CM:K_CM + 128] = (same & (s <= t)).astype(np.float32)
    c[:, K_TR:K_TR + 128] = (same & (s > t)).astype(np.float32)
    c[:, K_TM:K_TM + 128] = ((t // 16) >= (s // 16)).astype(np.float32)
    c[:, K_SW:K_SW + 128] = (((s % 64) == (t % 64)) & ((s // 64) != (t // 64))).astype(np.float32)
    st = np.zeros((P, 8, 240), np.float32)
    for gl in range(8):
        for h in range(16):
            st[gl * 16 + h, gl, 112 + h] = 1.0
    c[:, K_ST:K_ST + 1920] = st.reshape(P, 1920)
    c[:, K_NV:K_NV + 32] = np.array(NVALS, np.float32)[None, :]
    c[0:64, K_SG] = 1.0; c[64:128, K_SG] = -1.0
    c[0:64, K_SG + 1] = -1.0; c[64:128, K_SG + 1] = 1.0
    return c


def prep_shared(inp):
    f = lambda a: np.ascontiguousarray(np.asarray(a, dtype=np.float32))
    sh = {}
    sh["w_in"] = f(inp["w_in"].reshape(NL, DC, P, DIN).transpose(0, 2, 1, 3))
    sh["w_glu"] = f(inp["s5_w_glu"].reshape(NL, 4, P, 512).transpose(0, 2, 1, 3))
    sh["w_out"] = f(inp["w_out"].reshape(NL, DC, P, DC, 128).transpose(0, 3, 2, 1, 4))
    sh["w_f1"] = f(inp["w_ffn_in"].reshape(NL, DC, P, 2, FC, 128).transpose(0, 4, 2, 1, 3, 5))
    sh["w_f2"] = f(inp["w_ffn_out"].reshape(NL, FC, P, DC, 128).transpose(0, 3, 2, 1, 4))
    cols = np.zeros((P, NCOL), np.float32)
    cols[:, C_NMIX:C_NMIX + 32] = inp["norm_mix"].reshape(NL, DC, P).transpose(2, 0, 1).reshape(P, 32)
    cols[:, C_NFFN:C_NFFN + 32] = inp["norm_ffn"].reshape(NL, DC, P).transpose(2, 0, 1).reshape(P, 32)
    cols[:, C_NFIN:C_NFIN + 8] = inp["norm_final"].reshape(DC, P).T
    cols[:, C_BGLU:C_BGLU + 16] = inp["s5_b_glu"].reshape(NL, 4, P).transpose(2, 0, 1).reshape(P, 16)
    cols[:, C_S5N:C_S5N + 16] = inp["s5_out_norm"].reshape(NL, 4, P).transpose(2, 0, 1).reshape(P, 16)
    cols[:, C_GLAN:C_GLAN + 16] = inp["gla_out_norm"].reshape(NL, 4, P).transpose(2, 0, 1).reshape(P, 16)
    sh["cols"] = cols
    wgx = np.zeros((33, NL, 256), np.float32)
    wgx[0:16] = inp["gla_w_gate"].transpose(1, 0, 2)
    wgx[32] = inp["gla_b_gate"]
    sh["wgx"] = wgx
    s5p = np.zeros((P, NL, NS5), np.float32)
    dup = lambda a: np.concatenate([a, a], 0)
    lre = inp["s5_lam_re"].transpose(2, 0, 1); lim = inp["s5_lam_im"].transpose(2, 0, 1)
    s5p[:, :, 0:32] = dup(lre); s5p[:, :, 32:64] = dup(lim)
    s5p[:, :, 64:96] = np.broadcast_to(inp["s5_log_step"][None], (P, NL, G))
    bre = inp["s5_b_re"].transpose(2, 0, 1, 3).reshape(64, NL, 512); bim = inp["s5_b_im"].transpose(2, 0, 1, 3).reshape(64, NL, 512)
    s5p[:, :, 96:608] = np.concatenate([bre, bim], 0); s5p[:, :, 608:1120] = np.concatenate([bim, bre], 0)
    cre = inp["s5_c_re"].transpose(3, 0, 1, 2).reshape(64, NL, 512); cim = inp["s5_c_im"].transpose(3, 0, 1, 2).reshape(64, NL, 512)
    s5p[:, :, 1120:1632] = dup(cre); s5p[:, :, 1632:2144] = dup(cim)
    sh["s5p"] = s5p
    dd = inp["s5_d"].reshape(NL, G, 16).transpose(2, 0, 1)
    sh["dcol"] = f(np.tile(dd, (8, 1, 1)))
    sh["consts"] = _consts()
    return sh


_PROG = {}


def kernel(**inputs):
    x = np.asarray(inputs["x"], dtype=np.float32)
    B = x.shape[0]; ncores = 8; nseq = B // ncores
    sh = prep_shared(inputs)
    if "full" not in _PROG:
        _PROG["full"] = MK(nseq=nseq)
    mk = _PROG["full"]
    in_maps = []
    for c in range(ncores):
        xs = x[c * nseq:(c + 1) * nseq]
        xT = np.ascontiguousarray(xs.reshape(nseq, L, DC, P).transpose(0, 3, 2, 1))
        m = dict(sh); m["xT"] = xT
        in_maps.append(m)
    res = run_bass_kernel_spmd(mk.nc, in_maps, core_ids=list(range(ncores)))
    out = np.empty((B, L, D), np.float32)
    for c in range(ncores):
        yT = res.results[c]["yT"]
        out[c * nseq:(c + 1) * nseq] = yT.transpose(0, 3, 2, 1).reshape(nseq, L, D)
    return out
```
